# Optimizing a Trainium2 kernel written in Bass

```python
import math
import jax
import jax.numpy as jnp
from jax import lax
import numpy as np

D_MODEL = 1024
BATCH = 8
SEQ = 8192
DEPTH = 4

DN_HEADS = 8
DN_DK = 64
DN_DV = 64
DN_CONV = 4
DN_CHUNK = 64
DF_HEADS = 4
DF_D = 64
DF_QBLK = 128
REL_BUCKETS = 32
REL_MAX_DIST = 128
D_FF = 2816
N_EXPERTS = 8
TOP_K = 2
D_FF_EXPERT = 3584
MOE_BLK = 512
LN_EPS = 1e-5
RMS_EPS = 1e-6
ALPHA = (2 * DEPTH) ** 0.25
BETA_INIT = (8 * DEPTH) ** -0.25

DN_QK_W = DN_HEADS * DN_DK
DN_V_W = DN_HEADS * DN_DV
DN_CONV_CH = 2 * DN_QK_W + DN_V_W
DF_QK_W = DF_HEADS * 2 * DF_D
DF_V_W = DF_HEADS * 2 * DF_D
MIX_W = DN_V_W + DF_V_W
_IN_SIZES = (DN_CONV_CH, DN_V_W, DN_HEADS, DN_HEADS, DF_QK_W, DF_QK_W, DF_V_W)
IN_W = sum(_IN_SIZES)
IN_OFFSETS = tuple(int(o) for o in np.cumsum(_IN_SIZES)[:-1])
N_DENSE = (DEPTH + 1) // 2
N_MOE = DEPTH // 2

kernel_name = 'hybrid_deltanet_diffattn_moe_deepnorm'

F32 = jnp.float32


def layer_norm(x, g, b):
    xf = x.astype(F32)
    mu = jnp.mean(xf, axis=-1, keepdims=True)
    var = jnp.mean(jnp.square(xf - mu), axis=-1, keepdims=True)
    return ((xf - mu) * lax.rsqrt(var + LN_EPS) * g + b).astype(x.dtype)


def rms_norm(x, w):
    xf = x.astype(F32)
    return xf * lax.rsqrt(jnp.mean(jnp.square(xf), axis=-1, keepdims=True) + RMS_EPS) * w.astype(F32)


def l2norm(x):
    return x * lax.rsqrt(jnp.sum(jnp.square(x), axis=-1, keepdims=True) + 1e-6)


def t5_causal_bucket(dist):
    max_exact = REL_BUCKETS // 2
    d = jnp.maximum(dist, 1).astype(F32)
    large = max_exact + (jnp.log(d / max_exact) / math.log(REL_MAX_DIST / max_exact)
                         * (REL_BUCKETS - max_exact)).astype(jnp.int32)
    large = jnp.minimum(large, REL_BUCKETS - 1)
    return jnp.where(dist < max_exact, dist, large)


def causal_depthwise_conv(x, w):
    return lax.conv_general_dilated(
        x, w[:, None, :].astype(x.dtype), window_strides=(1,), padding=[(DN_CONV - 1, 0)],
        dimension_numbers=('NWC', 'WIO', 'NWC'), feature_group_count=x.shape[-1])


def gated_delta_rule(q, k, v, g, beta):
    bsz, nh, seq, dk = q.shape
    dv = v.shape[-1]
    c = DN_CHUNK
    n = seq // c
    q = q.reshape(bsz, nh, n, c, dk)
    k = k.reshape(bsz, nh, n, c, dk)
    v = v.reshape(bsz, nh, n, c, dv)
    g = g.reshape(bsz, nh, n, c)
    beta = beta.reshape(bsz, nh, n, c)
    gc = jnp.cumsum(g, axis=-1)
    causal = jnp.tril(jnp.ones((c, c), bool))
    strict = jnp.tril(jnp.ones((c, c), bool), -1)
    decay = jnp.exp(jnp.where(causal, gc[..., :, None] - gc[..., None, :], -jnp.inf))
    kb = k * beta[..., None]
    lower = jnp.where(strict, jnp.einsum('bhnid,bhnjd->bhnij', kb, k) * decay, 0.0)
    tri = lower + jnp.eye(c, dtype=q.dtype)
    rhs = jnp.concatenate([v * beta[..., None], kb * jnp.exp(gc)[..., None]], axis=-1)
    sol = lax.linalg.triangular_solve(tri, rhs, left_side=True, lower=True, unit_diagonal=True)
    u, w = sol[..., :dv], sol[..., dv:]
    intra = jnp.where(causal, jnp.einsum('bhnid,bhnjd->bhnij', q, k) * decay, 0.0)
    q_dec = q * jnp.exp(gc)[..., None]
    k_dec = k * jnp.exp(gc[..., -1:] - gc)[..., None]
    g_last = jnp.exp(gc[..., -1])

    def step(state, inp):
        q_i, k_i, u_i, w_i, a_i, gl_i = inp
        v_new = u_i - jnp.einsum('bhcd,bhde->bhce', w_i, state)
        o = jnp.einsum('bhcd,bhde->bhce', q_i, state) + jnp.einsum('bhij,bhje->bhie', a_i, v_new)
        state = state * gl_i[..., None, None] + jnp.einsum('bhcd,bhce->bhde', k_i, v_new)
        return state, o

    xs = tuple(jnp.moveaxis(t, 2, 0) for t in (q_dec, k_dec, u, w, intra, g_last))
    state0 = jnp.zeros((bsz, nh, dk, dv), q.dtype)
    _, o = lax.scan(step, state0, xs)
    return jnp.moveaxis(o, 0, 2).reshape(bsz, nh, seq, dv)


def diff_attention(q, k, v, lam, bias_dist):
    seq = q.shape[2]
    scale = DF_D ** -0.5
    outs = []
    for s0 in range(0, seq, DF_QBLK):
        e = s0 + DF_QBLK
        qb = q[:, :, s0:e]
        kb = k[:, :, :e]
        vb = v[:, :, :e]
        logits = jnp.einsum('bhqmd,bhkmd->bhmqk', qb, kb, preferred_element_type=F32) * scale
        dist = (s0 + jnp.arange(DF_QBLK, dtype=jnp.int32))[:, None] - jnp.arange(e, dtype=jnp.int32)[None, :]
        bias = bias_dist[jnp.maximum(dist, 0)].transpose(2, 0, 1)
        logits = jnp.where(dist >= 0, logits + bias[None, :, None], -jnp.inf)
        p = jax.nn.softmax(logits, axis=-1)
        attn = p[:, :, 0] - lam * p[:, :, 1]
        outs.append(jnp.einsum('bhqk,bhkd->bhqd', attn, vb.astype(F32)))
    return jnp.concatenate(outs, axis=2)


def hybrid_mixer(x, w_in, w_out, conv_w, dn_a_log, dn_dt_bias, dn_norm_w, df_lambda, df_subln_w,
                 bias_dist, lambda_init):
    bsz, seq, _ = x.shape
    proj = x @ w_in
    qkv_dn, z, a, b, q_df, k_df, v_df = jnp.split(proj, IN_OFFSETS, axis=-1)

    qkv = jax.nn.silu(causal_depthwise_conv(qkv_dn, conv_w))
    q, k, v = jnp.split(qkv, [DN_QK_W, 2 * DN_QK_W], axis=-1)

    def heads(t, d):
        return t.reshape(bsz, seq, -1, d).transpose(0, 2, 1, 3).astype(F32)

    q = l2norm(heads(q, DN_DK)) * DN_DK ** -0.5
    k = l2norm(heads(k, DN_DK))
    v = heads(v, DN_DV)
    beta = jax.nn.sigmoid(b.astype(F32)).transpose(0, 2, 1)
    g = (-jnp.exp(dn_a_log.astype(F32))
         * jax.nn.softplus(a.astype(F32) + dn_dt_bias.astype(F32))).transpose(0, 2, 1)
    o_dn = gated_delta_rule(q, k, v, g, beta).transpose(0, 2, 1, 3)
    o_dn = rms_norm(o_dn, dn_norm_w) * jax.nn.silu(z.reshape(bsz, seq, DN_HEADS, DN_DV).astype(F32))
    o_dn = o_dn.reshape(bsz, seq, DN_V_W).astype(x.dtype)

    qd = q_df.reshape(bsz, seq, DF_HEADS, 2, DF_D).transpose(0, 2, 1, 3, 4)
    kd = k_df.reshape(bsz, seq, DF_HEADS, 2, DF_D).transpose(0, 2, 1, 3, 4)
    vd = v_df.reshape(bsz, seq, DF_HEADS, 2 * DF_D).transpose(0, 2, 1, 3)
    lf = df_lambda.astype(F32)
    lam = jnp.exp(jnp.sum(lf[0] * lf[1])) - jnp.exp(jnp.sum(lf[2] * lf[3])) + lambda_init
    o_df = diff_attention(qd, kd, vd, lam, bias_dist)
    o_df = rms_norm(o_df, df_subln_w) * (1.0 - lambda_init)
    o_df = o_df.transpose(0, 2, 1, 3).reshape(bsz, seq, DF_V_W).astype(x.dtype)

    return jnp.concatenate([o_dn, o_df], axis=-1) @ w_out


def swiglu(x, w_gate, w_up, w_down):
    return (jax.nn.silu(x @ w_gate) * (x @ w_up)) @ w_down


def moe_swiglu(x2, router, w_gate, w_up, w_down):
    n_tok, d = x2.shape
    logits = jnp.dot(x2, router, preferred_element_type=F32)
    top_logit, top_idx = lax.top_k(logits, TOP_K)
    gate = jax.nn.softmax(top_logit, axis=-1).astype(x2.dtype)
    n_asg = n_tok * TOP_K
    flat_e = top_idx.reshape(n_asg)
    order = jnp.argsort(flat_e)
    sorted_e = flat_e[order]
    sorted_tok = (order // TOP_K).astype(jnp.int32)
    sorted_gate = gate.reshape(n_asg)[order]
    counts = jnp.zeros((N_EXPERTS,), jnp.int32).at[flat_e].add(1)
    padded = (counts + MOE_BLK - 1) // MOE_BLK * MOE_BLK
    pad_end = jnp.cumsum(padded)
    pad_start = pad_end - padded
    cnt_start = jnp.cumsum(counts) - counts
    slot = pad_start[sorted_e] + jnp.arange(n_asg, dtype=jnp.int32) - cnt_start[sorted_e]
    n_slot = -(-n_asg // MOE_BLK) * MOE_BLK + N_EXPERTS * MOE_BLK
    n_blk = n_slot // MOE_BLK
    slot_tok = jnp.full((n_slot,), n_tok, jnp.int32).at[slot].set(sorted_tok)
    slot_gate = jnp.zeros((n_slot,), x2.dtype).at[slot].set(sorted_gate)
    blk_e = jnp.minimum(jnp.searchsorted(pad_end, jnp.arange(n_blk, dtype=jnp.int32) * MOE_BLK,
                                         side='right'), N_EXPERTS - 1)
    x_pad = jnp.concatenate([x2, jnp.zeros((1, d), x2.dtype)], axis=0)
    xb = x_pad[slot_tok].reshape(n_blk, MOE_BLK, d)

    def expert_block(args):
        xe, e = args
        return swiglu(xe, w_gate[e], w_up[e], w_down[e])

    yb = lax.map(expert_block, (xb, blk_e)).reshape(n_slot, d)
    out = jnp.zeros((n_tok + 1, d), x2.dtype).at[slot_tok].add(yb * slot_gate[:, None])
    return out[:n_tok]


def setup_inputs(seed: int = 0) -> dict:
    key = jax.random.key(seed)
    ks = jax.random.split(key, 24)

    def nrm(k, shape, s):
        return jax.random.normal(k, shape, F32) * s

    x = nrm(ks[0], (BATCH, SEQ, D_MODEL), 1.0)
    w_in = nrm(ks[1], (DEPTH, D_MODEL, IN_W), D_MODEL ** -0.5)
    w_out = nrm(ks[2], (DEPTH, MIX_W, D_MODEL), MIX_W ** -0.5 * BETA_INIT)
    conv_w = nrm(ks[3], (DEPTH, DN_CONV, DN_CONV_CH), DN_CONV ** -0.5)
    dn_a_log = jnp.log(jax.random.uniform(ks[4], (DEPTH, DN_HEADS), F32, 1.0, 16.0))
    dt = jnp.exp(jax.random.uniform(ks[5], (DEPTH, DN_HEADS), F32, math.log(1e-3), math.log(1e-1)))
    dn_dt_bias = dt + jnp.log(-jnp.expm1(-dt))
    dn_norm_w = 1.0 + nrm(ks[6], (DEPTH, DN_DV), 0.02)
    df_lambda = nrm(ks[7], (DEPTH, 4, DF_D), 0.1)
    df_subln_w = 1.0 + nrm(ks[8], (DEPTH, 2 * DF_D), 0.02)
    rel_bias = nrm(ks[9], (REL_BUCKETS, DF_HEADS), 0.5)
    ln1_g = 1.0 + nrm(ks[10], (DEPTH, D_MODEL), 0.02)
    ln1_b = nrm(ks[11], (DEPTH, D_MODEL), 0.02)
    ln2_g = 1.0 + nrm(ks[12], (DEPTH, D_MODEL), 0.02)
    ln2_b = nrm(ks[13], (DEPTH, D_MODEL), 0.02)
    ffn_w_gate = nrm(ks[14], (N_DENSE, D_MODEL, D_FF), D_MODEL ** -0.5)
    ffn_w_up = nrm(ks[15], (N_DENSE, D_MODEL, D_FF), D_MODEL ** -0.5)
    ffn_w_down = nrm(ks[16], (N_DENSE, D_FF, D_MODEL), D_FF ** -0.5 * BETA_INIT)
    moe_router = nrm(ks[17], (N_MOE, D_MODEL, N_EXPERTS), D_MODEL ** -0.5)
    moe_w_gate = nrm(ks[18], (N_MOE, N_EXPERTS, D_MODEL, D_FF_EXPERT), D_MODEL ** -0.5)
    moe_w_up = nrm(ks[19], (N_MOE, N_EXPERTS, D_MODEL, D_FF_EXPERT), D_MODEL ** -0.5)
    moe_w_down = nrm(ks[20], (N_MOE, N_EXPERTS, D_FF_EXPERT, D_MODEL), D_FF_EXPERT ** -0.5 * BETA_INIT)
    return {'x': x, 'w_in': w_in, 'w_out': w_out, 'conv_w': conv_w, 'dn_a_log': dn_a_log,
            'dn_dt_bias': dn_dt_bias, 'dn_norm_w': dn_norm_w, 'df_lambda': df_lambda,
            'df_subln_w': df_subln_w, 'rel_bias': rel_bias, 'ln1_g': ln1_g, 'ln1_b': ln1_b,
            'ln2_g': ln2_g, 'ln2_b': ln2_b, 'ffn_w_gate': ffn_w_gate, 'ffn_w_up': ffn_w_up,
            'ffn_w_down': ffn_w_down, 'moe_router': moe_router, 'moe_w_gate': moe_w_gate,
            'moe_w_up': moe_w_up, 'moe_w_down': moe_w_down}


def reference(x, w_in, w_out, conv_w, dn_a_log, dn_dt_bias, dn_norm_w, df_lambda, df_subln_w, rel_bias,
              ln1_g, ln1_b, ln2_g, ln2_b, ffn_w_gate, ffn_w_up, ffn_w_down, moe_router, moe_w_gate,
              moe_w_up, moe_w_down):
    bsz, seq, d = x.shape
    dist = jnp.arange(seq, dtype=jnp.int32)
    bias_dist = rel_bias[t5_causal_bucket(dist)].astype(F32)
    for layer in range(DEPTH):
        lambda_init = 0.8 - 0.6 * math.exp(-0.3 * layer)
        mix = hybrid_mixer(x, w_in[layer], w_out[layer], conv_w[layer], dn_a_log[layer], dn_dt_bias[layer],
                           dn_norm_w[layer], df_lambda[layer], df_subln_w[layer], bias_dist, lambda_init)
        x = layer_norm(ALPHA * x + mix, ln1_g[layer], ln1_b[layer])
        if layer % 2 == 0:
            i = layer // 2
            f = swiglu(x, ffn_w_gate[i], ffn_w_up[i], ffn_w_down[i])
        else:
            i = layer // 2
            f = moe_swiglu(x.reshape(bsz * seq, d), moe_router[i], moe_w_gate[i], moe_w_up[i],
                           moe_w_down[i]).reshape(bsz, seq, d)
        x = layer_norm(ALPHA * x + f, ln2_g[layer], ln2_b[layer])
    return x
```

```python
import math
import numpy as np
import ml_dtypes
from contextlib import ExitStack
import concourse.bass as bass
import concourse.mybir as mybir
from concourse.bass_utils import run_bass_kernel_spmd

F32 = mybir.dt.float32
BF16 = mybir.dt.bfloat16
AF = mybir.ActivationFunctionType
ALU = mybir.AluOpType
AX = mybir.AxisListType

PE, ACT, DVE, POOL, SP = "pe", "act", "dve", "pool", "sp"
ENGMAP = {PE: "tensor", ACT: "scalar", DVE: "vector", POOL: "gpsimd", SP: "sync"}
SEM_EPOCH = 30000

S = 8192
D = 1024
NT = S // 128
DEPTH = 4
IN_W = 3600
DFF = 2816
DFE = 3584
NE = 8
ALPHA = (2 * DEPTH) ** 0.25
LN_EPS = 1e-5


class Op:
    __slots__ = ("eng", "fn", "deps", "signal", "sem", "val", "is_dma", "grp", "ndep", "seq")

    def __init__(self, eng, fn, is_dma, grp):
        self.eng = eng
        self.fn = fn
        self.deps = []
        self.signal = False
        self.sem = None
        self.val = 0
        self.is_dma = is_dma
        self.grp = grp
        self.ndep = 0


class Phase:
    def __init__(self, nc, name):
        self.nc = nc
        self.name = name
        self.ops = []
        self.last_w = {}
        self.readers = {}
        self.stack = ExitStack()
        self.excl = set()
        self.alias = {}
        self.eng_seq = {}

    def sb(self, name, shape, dt):
        return self.stack.enter_context(self.nc.sbuf_tensor(f"{self.name}_{name}", list(shape), dt))

    def ps(self, name, shape, dt=F32):
        return self.stack.enter_context(self.nc.psum_tensor(f"{self.name}_{name}", list(shape), dt))

    def op(self, eng, fn, r=(), w=(), dma=False, grp=None):
        if self.alias:
            r = [self.alias.get(k, k) for k in r]
            w = [self.alias.get(k, k) for k in w]
        o = Op(eng, fn, dma, grp)
        o.seq = self.eng_seq.get(eng, 0)
        self.eng_seq[eng] = o.seq + 1
        deps = []
        seen = set()
        raw = set()
        for k in r:
            lw = self.last_w.get(k)
            if lw is not None:
                raw.add(id(lw))
                if id(lw) not in seen:
                    seen.add(id(lw))
                    deps.append(lw)
            if k in self.excl:
                for rd in self.readers.get(k, ()):
                    if rd.eng != eng and id(rd) not in seen:
                        seen.add(id(rd))
                        raw.add(id(rd))
                        deps.append(rd)
        for k in w:
            lw = self.last_w.get(k)
            if lw is not None and id(lw) not in seen:
                seen.add(id(lw))
                deps.append(lw)
            for rd in self.readers.get(k, ()):
                if id(rd) not in seen:
                    seen.add(id(rd))
                    deps.append(rd)
        for d in deps:
            if d.eng == eng and not d.is_dma and not dma:
                if eng == PE:
                    continue
                if id(d) not in raw and o.seq - d.seq > 1:
                    continue
            o.deps.append(d)
            d.ndep += 1
        for k in r:
            self.readers.setdefault(k, []).append(o)
        for k in w:
            self.last_w[k] = o
            self.readers[k] = []
        self.ops.append(o)
        return o

    def mm(self, out, lhsT, rhs, start, stop, r, w, **kw):
        return self.op(PE, lambda e: e.matmul(out, lhsT, rhs, start=start, stop=stop, **kw), r, w)

    def tr(self, out, in_, ident, r, w):
        return self.op(PE, lambda e: e.transpose(out, in_, ident), r, w)

    def act(self, out, in_, func, r, w, **kw):
        return self.op(ACT, lambda e: e.activation(out, in_, func, **kw), r, w)

    def copy(self, eng, out, in_, r, w):
        if eng == ACT:
            return self.op(ACT, lambda e: e.copy(out, in_), r, w)
        return self.op(eng, lambda e: e.tensor_copy(out, in_), r, w)

    def tt(self, eng, out, in0, in1, op, r, w):
        return self.op(eng, lambda e: e.tensor_tensor(out, in0, in1, op), r, w)

    def ts(self, eng, out, in0, s1, s2, op0, op1, r, w):
        return self.op(eng, lambda e: e.tensor_scalar(out, in0, s1, s2, op0, op1), r, w)

    def stt(self, out, in0, scalar, in1, op0, op1, r, w):
        return self.op(DVE, lambda e: e.scalar_tensor_tensor(out, in0, scalar, in1, op0, op1), r, w)

    def dma(self, q, out, in_, r, w, grp, **kw):
        return self.op(q, lambda e: e.dma_start(out, in_, **kw), r, w, dma=True, grp=grp)

    def emit(self):
        nc = self.nc
        leaf = [o for o in self.ops if o.is_dma and o.ndep == 0]
        last = {}
        for o in self.ops:
            if not o.is_dma:
                last[o.eng] = o
        leaf += list(last.values())
        if leaf:
            fin = Op(SP, lambda e: e.nop(), False, None)
            fin.deps = leaf
            self.ops.append(fin)
        for o in self.ops:
            for d in o.deps:
                d.signal = True
        sem_state = {}
        nsem = [0]

        sem_handles = []

        def new_sem():
            nsem[0] += 1
            h = nc.alloc_semaphore(name=f"{self.name}_s{nsem[0]}")
            sem_handles.append(h)
            return h

        for o in self.ops:
            if not o.signal:
                continue
            key = ("dma", o.grp) if o.is_dma else o.eng
            st = sem_state.get(key)
            inc = 16 if o.is_dma else 1
            if st is None or st[1] + inc > SEM_EPOCH:
                st = [new_sem(), 0]
                sem_state[key] = st
            st[1] += inc
            o.sem = st[0]
            o.val = st[1]
        self.n_sems = nsem[0]
        per_eng = {}
        for o in self.ops:
            per_eng.setdefault(o.eng, []).append(o)
        with nc.Block() as block:
            for ename, lst in per_eng.items():
                def body(e, lst=lst):
                    waited = {}
                    for o in lst:
                        need = {}
                        for d in o.deps:
                            k = id(d.sem)
                            if k not in need or need[k][1] < d.val:
                                need[k] = (d.sem, d.val)
                        for k, (s, v) in need.items():
                            if waited.get(k, 0) >= v:
                                continue
                            e.wait_ge(s, v)
                            waited[k] = v
                        ins = o.fn(e)
                        if o.signal:
                            ins.then_inc(o.sem, 16 if o.is_dma else 1)
                getattr(block, ENGMAP[ename])(body)
        if sem_handles:
            nc.clear_and_free_semaphores(sem_handles)
            nc.all_engine_barrier()
        nops = len(self.ops)
        self.ops = None
        self.last_w = None
        self.readers = None
        self.stack.close()
        return nops


class Ctx:
    pass


def rr(lst, state=[0]):
    state[0] += 1
    return lst[state[0] % len(lst)]


def phase_x0(C):
    nc = C.nc
    P = Phase(nc, "x0")
    ident = P.sb("ident", [128, 128], BF16)
    P.dma(SP, ident[:], C.ident_bf, [], ["ident"], "ident")
    xin = [P.sb(f"xin{i}", [128, D], F32) for i in range(2)]
    xb = [P.sb(f"xb{i}", [128, D], BF16) for i in range(2)]
    xt = [P.sb(f"xt{i}", [128, 8, 128], BF16) for i in range(2)]
    pt = [P.ps(f"pt{i}", [128, 8, 128], BF16) for i in range(2)]
    P.excl.update(["pt0", "pt1"])
    xT3 = C.xT.rearrange("(c p) t -> p c t", p=128)
    for t in range(NT):
        b = t % 2
        P.dma(SP, xin[b][:], C.x[t * 128:(t + 1) * 128, :], [], [f"xin{b}"], f"xin{b}")
        P.copy(DVE if t % 2 else POOL, xb[b][:], xin[b][:], [f"xin{b}"], [f"xb{b}"])
        for c in range(8):
            P.tr(pt[b][:, c, :], xb[b][:, c * 128:(c + 1) * 128], ident[:], [f"xb{b}", "ident"], [f"pt{b}"])
        P.copy(ACT if t % 2 else DVE, xt[b][:], pt[b][:], [f"pt{b}"], [f"xt{b}"])
        P.dma(POOL, xT3[:, :, t * 128:(t + 1) * 128], xt[b][:], [f"xt{b}"], [("xT", t)], f"xt{b}")
    return P.emit()


def load_cast_w(P, dst3, src2, nchunk, ncols, r_keys, wkey, stg, colstep, cnt):
    for c in range(nchunk):
        for c0 in range(0, ncols, colstep):
            n = min(colstep, ncols - c0)
            i = cnt[0] % len(stg)
            cnt[0] += 1
            P.dma(SP, stg[i][:, 0:n], src2[c * 128:(c + 1) * 128, c0:c0 + n], list(r_keys), [f"stg{i}"], f"stg{i}")
            eng = (DVE, POOL, ACT)[cnt[0] % 3]
            P.copy(eng, dst3[:, c, c0:c0 + n], stg[i][:, 0:n], [f"stg{i}"], [wkey])


def phase_inproj(C, layer):
    nc = C.nc
    P = Phase(nc, f"ip{layer}")
    W = P.sb("W", [128, 8, IN_W], BF16)
    stg = [P.sb(f"stg{i}", [128, 1800], F32) for i in range(3)]
    cnt = [0]
    load_cast_w(P, W, C.w_in[layer], 8, IN_W, [], "W", stg, 1800, cnt)
    xTb = [P.sb(f"xTb{i}", [128, 8, 512], BF16) for i in range(2)]
    qk_sb = [P.sb(f"qk{i}", [128, 512], BF16) for i in range(3)]
    qkv_sb = [P.sb(f"qkv{i}", [128, 1536], F32) for i in range(2)]
    z_sb = [P.sb(f"z{i}", [128, 512], F32) for i in range(2)]
    v_sb = [P.sb(f"v{i}", [128, 512], BF16) for i in range(2)]
    ps = [P.ps(f"ps{i}", [128, 512], F32) for i in range(8)]
    P.excl.update([f"ps{i}" for i in range(8)])
    xT3 = C.xT.rearrange("(c p) t -> p c t", p=128)
    pi = 0
    ev = 0
    nqk = 0
    for tb in range(S // 512):
        b = tb % 2
        P.dma(SP, xTb[b][:], xT3[:, :, tb * 512:(tb + 1) * 512], [("xT", tb * 4 + i) for i in range(4)],
              [f"xTb{b}"], f"xTb{b}")
        for g in range(8):
            col0 = 2064 + g * 128
            p = pi % 8
            pi += 1
            for c in range(8):
                P.mm(ps[p][:, :], W[:, c, col0:col0 + 128], xTb[b][:, c, :], c == 0, c == 7,
                     ["W", f"xTb{b}"], [f"ps{p}"])
            i = nqk % 3
            nqk += 1
            P.copy(ACT if ev % 2 else DVE, qk_sb[i][:], ps[p][:, :], [f"ps{p}"], [f"qk{i}"])
            ev += 1
            dst = C.QT[g] if g < 4 else C.KT[g - 4]
            P.dma(POOL, dst[:, tb * 512:(tb + 1) * 512], qk_sb[i][:], [f"qk{i}"], [("qkT", g, tb)], f"qk{i}")
        for tt in range(4):
            t = tb * 4 + tt
            tb2 = t % 2
            groups = [(0, 512, "qkv"), (512, 512, "qkv"), (1024, 512, "qkv"), (1536, 512, "z"),
                      (2048, 16, "ab"), (3088, 512, "v")]
            for (col0, n, kind) in groups:
                p = pi % 8
                pi += 1
                for c in range(8):
                    P.mm(ps[p][:, 0:n], xTb[b][:, c, tt * 128:(tt + 1) * 128], W[:, c, col0:col0 + n],
                         c == 0, c == 7, ["W", f"xTb{b}"], [f"ps{p}"])
                eng = ACT if ev % 2 else DVE
                ev += 1
                if kind == "qkv":
                    P.copy(eng, qkv_sb[tb2][:, col0:col0 + 512], ps[p][:, 0:512], [f"ps{p}"], [f"qkv{tb2}_{col0}"])
                elif kind == "z":
                    P.copy(eng, z_sb[tb2][:], ps[p][:, 0:512], [f"ps{p}"], [f"z{tb2}"])
                elif kind == "ab":
                    P.copy(eng, C.ab_sb[:, t, :], ps[p][:, 0:16], [f"ps{p}"], [("ab", t)])
                else:
                    P.copy(eng, v_sb[tb2][:], ps[p][:, 0:512], [f"ps{p}"], [f"v{tb2}"])
            P.dma(POOL, C.qkvpre[3 + t * 128:3 + (t + 1) * 128, :], qkv_sb[tb2][:],
                  [f"qkv{tb2}_0", f"qkv{tb2}_512", f"qkv{tb2}_1024"], [("qkvpre", t)], f"qkv{tb2}")
            P.dma(POOL, C.zbuf[t * 128:(t + 1) * 128, :], z_sb[tb2][:], [f"z{tb2}"], [("zbuf", t)], f"z{tb2}")
            P.dma(POOL, C.Vdf[t * 128:(t + 1) * 128, :], v_sb[tb2][:], [f"v{tb2}"], [("Vdf", t)], f"v{tb2}")
    return P.emit()


def phase_attn(C, layer):
    nc = C.nc
    P = Phase(nc, f"at{layer}")
    lambda_init = 0.8 - 0.6 * math.exp(-0.3 * layer)
    identf = P.sb("identf", [128, 128], F32)
    onesf = P.sb("onesf", [128, 128], F32)
    maskneg = P.sb("maskneg", [128, 128], F32)
    cfar = P.sb("cfar", [128, 4], F32)
    bt = P.sb("bt", [128, 8, 128], F32)
    badd = P.sb("badd", [128, 8, 128], F32)
    dl = P.sb("dl", [128, 256], F32)
    dlp = P.sb("dlp", [128, 128], F32)
    ls = P.sb("ls", [128, 2], F32)
    le = P.sb("le", [128, 2], F32)
    neglam = P.sb("neglam", [128, 1], F32)
    wcol = P.sb("wcol", [128, 1], F32)
    P.dma(SP, identf[:], C.ident_f32, [], ["identf"], "c0")
    P.dma(SP, onesf[:], C.ones_f32, [], ["onesf"], "c1")
    P.dma(SP, maskneg[:], C.maskneg, [], ["maskneg"], "c2")
    P.dma(SP, cfar[:], C.cfar, [], ["cfar"], "c3")
    P.dma(SP, bt[:], C.bias_tiles.rearrange("h r k q -> k (h r) q"), [], ["bt"], "c4")
    P.dma(SP, dl[:], C.df_lambda_bc[layer], [], ["dl"], "c5")
    P.dma(SP, wcol[:], C.df_subln_col[layer], [], ["wcol"], "c6")
    for h in range(4):
        for rel in range(2):
            i = h * 2 + rel
            P.ts(DVE, badd[:, i, :], bt[:, i, :], cfar[:, h:h + 1], 8.0, ALU.subtract, ALU.mult,
                 ["bt", "cfar"], [("badd", i)])
            if rel == 0:
                P.tt(DVE, badd[:, i, :], badd[:, i, :], maskneg[:], ALU.add, [("badd", i), "maskneg"], [("badd", i)])
    P.tt(DVE, dlp[:, 0:64], dl[:, 0:64], dl[:, 64:128], ALU.mult, ["dl"], ["dlp0"])
    P.tt(DVE, dlp[:, 64:128], dl[:, 128:192], dl[:, 192:256], ALU.mult, ["dl"], ["dlp1"])
    P.op(DVE, lambda e: e.tensor_reduce(ls[:], dlp[:].rearrange("p (a b) -> p a b", a=2), AX.X, ALU.add),
         ["dlp0", "dlp1"], ["ls"])
    P.act(le[:], ls[:], AF.Exp, ["ls"], ["le"])
    P.tt(DVE, neglam[:], le[:, 1:2], le[:, 0:1], ALU.subtract, ["le"], ["neglam"])
    P.ts(DVE, neglam[:], neglam[:], -lambda_init, None, ALU.add, ALU.bypass, ["neglam"], ["neglam"])
    P.ts(DVE, wcol[:], wcol[:], 1.0 - lambda_init, None, ALU.mult, ALU.bypass, ["wcol"], ["wcol"])

    KTh = [P.sb(f"KTh{i}", [128, S], BF16) for i in range(2)]
    QTh = [P.sb(f"QTh{i}", [128, S], BF16) for i in range(2)]
    Vh = [P.sb(f"Vh{i}", [128, NT, 128], BF16) for i in range(2)]
    pT = [P.sb(f"pT{i}", [128, 512], BF16) for i in range(4)]
    PSa = [P.sb(f"PSa{i}", [128, 512], F32) for i in range(2)]
    rden = [P.sb(f"rden{i}", [128, 512], F32) for i in range(2)]
    on = [P.sb(f"on{i}", [128, 512], F32) for i in range(2)]
    o_sb = P.sb("o_sb", [128, 512], F32)
    sq = P.sb("sq", [128, 512], F32)
    rstd = P.sb("rstd", [128, 512], F32)
    of = [P.sb(f"of{i}", [128, 512], BF16) for i in range(2)]
    accO = [P.ps(f"accO{i}", [128, 512], F32) for i in range(2)]
    sc = [P.ps(f"sc{i}", [128, 512], F32) for i in range(4)]
    aux = [P.ps(f"aux{i}", [128, 512], F32) for i in range(2)]
    P.excl.update(["accO0", "accO1", "sc0", "sc1", "sc2", "sc3", "aux0", "aux1"])
    V3 = C.Vdf.rearrange("(t p) c -> p t c", p=128)

    def load_head(h):
        b = h % 2
        P.dma(SP, KTh[b][:], C.KT[h], [], [f"KTh{b}"], f"KTh{b}")
        P.dma(SP, QTh[b][:], C.QT[h], [], [f"QTh{b}"], f"QTh{b}")
        for i in range(8):
            P.dma(SP, Vh[b][:, i * 8:(i + 1) * 8, :], V3[:, i * 8:(i + 1) * 8, h * 128:(h + 1) * 128], [],
                  [(f"Vh{b}", i)], f"Vh{b}_{i}")

    load_head(0)
    nsc = 0
    npt = 0
    nq = 0
    for h in range(4):
        b = h % 2
        if h + 1 < 4:
            load_head(h + 1)
        for Qb in range(S // 512):
            nk = 4 * Qb + 4
            for kt in range(nk):
                j = kt - 4 * Qb
                c0 = max(0, j) * 128
                for m in range(2):
                    s = nsc % 4
                    nsc += 1
                    adds = []
                    if j >= 0:
                        adds.append((j * 128, h * 2 + 0))
                    if j >= -1 and j + 1 <= 3:
                        adds.append(((j + 1) * 128, h * 2 + 1))
                    P.mm(sc[s][:, c0:512], KTh[b][m * 64:(m + 1) * 64, kt * 128:(kt + 1) * 128],
                         QTh[b][m * 64:(m + 1) * 64, Qb * 512 + c0:Qb * 512 + 512], True, len(adds) == 0,
                         [f"KTh{b}", f"QTh{b}"], [f"sc{s}"])
                    for ai, (cc, bi) in enumerate(adds):
                        P.mm(sc[s][:, cc:cc + 128], identf[:], badd[:, bi, :], False, ai == len(adds) - 1,
                             ["identf", ("badd", bi)], [f"sc{s}"])
                    pi = npt % 4
                    npt += 1
                    P.act(pT[pi][:, c0:512], sc[s][:, c0:512], AF.Exp, [f"sc{s}"], [f"pT{pi}"], scale=0.125)
                    P.mm(accO[m][:, c0:512], Vh[b][:, kt, :], pT[pi][:, c0:512], kt == 0, kt == nk - 1,
                         [(f"Vh{b}", kt // 8), f"pT{pi}"], [f"accO{m}"])
                    eng = DVE if m == 0 else POOL
                    if kt == 0:
                        P.copy(eng, PSa[m][:], pT[pi][:], [f"pT{pi}"], [f"PSa{m}"])
                    else:
                        P.tt(eng, PSa[m][:, c0:512], PSa[m][:, c0:512], pT[pi][:, c0:512], ALU.add,
                             [f"PSa{m}", f"pT{pi}"], [f"PSa{m}"])
            for m in range(2):
                P.mm(aux[m][:], onesf[:], PSa[m][:], True, True, ["onesf", f"PSa{m}"], [f"aux{m}"])
                P.op(DVE, lambda e, m=m: e.reciprocal(rden[m][:], aux[m][:]), [f"aux{m}"], [f"rden{m}"])
                P.tt(DVE, on[m][:], accO[m][:], rden[m][:], ALU.mult, [f"accO{m}", f"rden{m}"], [f"on{m}"])
            P.stt(o_sb[:], on[1][:], neglam[:, 0:1], on[0][:], ALU.mult, ALU.add, ["on0", "on1", "neglam"], ["o_sb"])
            P.tt(POOL, sq[:], o_sb[:], o_sb[:], ALU.mult, ["o_sb"], ["sq"])
            P.mm(aux[0][:], onesf[:], sq[:], True, True, ["onesf", "sq"], ["aux0"])
            P.ts(DVE, rstd[:], aux[0][:], 1.0 / 128.0, 1e-6, ALU.mult, ALU.add, ["aux0"], ["rstd"])
            P.act(rstd[:], rstd[:], AF.Ln, ["rstd"], ["rstd"])
            P.act(rstd[:], rstd[:], AF.Exp, ["rstd"], ["rstd"], scale=-0.5)
            P.tt(DVE, o_sb[:], o_sb[:], rstd[:], ALU.mult, ["o_sb", "rstd"], ["o_sb"])
            ob = nq % 2
            nq += 1
            P.ts(DVE, of[ob][:], o_sb[:], wcol[:, 0:1], None, ALU.mult, ALU.bypass, ["o_sb", "wcol"], [f"of{ob}"])
            P.dma(POOL, C.catT[512 + h * 128:512 + (h + 1) * 128, Qb * 512:(Qb + 1) * 512], of[ob][:],
                  [f"of{ob}"], [("catT_df", h, Qb)], f"of{ob}")
    return P.emit()


def ln_epilogue(P, C, t, src_halves, src_keys, res_src, gb, ident, bufs, pt, pt_key, out_dst=None, router=None):
    i = t % 2
    xr, y, xo, xb, xt = bufs["xr"][i], bufs["y"][i], bufs["xo"][i], bufs["xb"][i], bufs["xt"][i]
    kxr, ky, kxo, kxb, kxt = bufs["kxr"][i], bufs["ky"][i], bufs["kxo"][i], bufs["kxb"][i], bufs["kxt"][i]
    st, mv, rs, nmr = bufs["st"][i], bufs["mv"][i], bufs["rs"][i], bufs["nmr"][i]
    ks = f"lnsmall{i}"
    P.dma(SP, xr[:, 0:D], res_src[t * 128:(t + 1) * 128, :], [("xres", t)], [kxr], f"ln_{kxr}")
    for hf in range(2):
        P.stt(y[:, hf * 512:(hf + 1) * 512], xr[:, hf * 512:(hf + 1) * 512], ALPHA, src_halves[hf], ALU.mult, ALU.add,
              [kxr, src_keys[hf]], [ky])
        P.op(DVE, lambda e, hf=hf: e.bn_stats(st[:, hf, :], y[:, hf * 512:(hf + 1) * 512]), [ky], [ks + f"st{hf}"])
    P.op(DVE, lambda e: e.bn_aggr(mv[:], st[:].rearrange("p a b -> p (a b)")), [ks + "st0", ks + "st1"], [ks + "mv"])
    P.ts(DVE, rs[:], mv[:, 1:2], LN_EPS, None, ALU.add, ALU.bypass, [ks + "mv"], [ks + "rs"])
    P.act(rs[:], rs[:], AF.Ln, [ks + "rs"], [ks + "rs"])
    P.act(rs[:], rs[:], AF.Exp, [ks + "rs"], [ks + "rs"], scale=-0.5)
    P.ts(DVE, nmr[:], mv[:, 0:1], rs[:, 0:1], -1.0, ALU.mult, ALU.mult, [ks + "mv", ks + "rs"], [ks + "nmr"])
    P.ts(POOL, y[:, 0:D], y[:, 0:D], rs[:, 0:1], nmr[:, 0:1], ALU.mult, ALU.add, [ky, ks + "rs", ks + "nmr"],
         [ky])
    P.tt(POOL, y[:, 0:D], y[:, 0:D], gb[0][:], ALU.mult, [ky, "ln_g"], [ky])
    P.tt(DVE, xo[:, 0:D], y[:, 0:D], gb[1][:], ALU.add, [ky, "ln_b"], [kxo])
    if out_dst is not None:
        P.dma(POOL, out_dst[t * 128:(t + 1) * 128, :], xo[:, 0:D], [kxo], [("out", t)], f"ln_{kxo}")
        return
    P.dma(POOL, C.xres[t * 128:(t + 1) * 128, :], xo[:, 0:D], [kxo], [("xres", t)], f"ln_{kxo}")
    P.copy(ACT, xb[:, 0:D], xo[:, 0:D], [kxo], [kxb])
    for c in range(8):
        P.tr(pt[:, c, :], xb[:, c * 128:(c + 1) * 128], ident[:], [kxb, "ident"], [pt_key])
    P.copy(ACT, xt[:], pt, [pt_key], [kxt])
    xT3 = C.xT.rearrange("(c p) t -> p c t", p=128)
    P.dma(POOL, xT3[:, :, t * 128:(t + 1) * 128], xt[:], [kxt], [("xT", t)], f"ln_{kxt}")
    if router is not None:
        rbc, lg, junk = router
        for e in range(NE):
            P.op(DVE, lambda en, e=e: en.scalar_tensor_tensor(junk[:], xo[:, 0:D], 1.0, rbc[:, e, :], ALU.mult, ALU.mult,
                                                               accum_out=lg[i][:, e:e + 1]),
                 [kxo, "rbc"], [f"lg{i}_{e}", "junk"])
        lgk = [f"lg{i}_{e}" for e in range(NE)]
        sm = bufs["sm"][i]
        kk = f"sm{i}"
        P.op(DVE, lambda en: en.tensor_reduce(sm[:, 0:1], lg[i][:], AX.X, ALU.max), lgk, [kk + "m1"])
        P.ts(DVE, sm[:, 8:16], lg[i][:], sm[:, 0:1], None, ALU.is_equal, ALU.bypass, lgk + [kk + "m1"], [kk + "k1"])
        P.stt(sm[:, 24:32], sm[:, 8:16], -1e30, lg[i][:], ALU.mult, ALU.add, [kk + "k1"] + lgk, [kk + "l2"])
        P.op(DVE, lambda en: en.tensor_reduce(sm[:, 1:2], sm[:, 24:32], AX.X, ALU.max), [kk + "l2"], [kk + "m2"])
        P.ts(DVE, sm[:, 16:24], sm[:, 24:32], sm[:, 1:2], None, ALU.is_equal, ALU.bypass, [kk + "l2", kk + "m2"], [kk + "k2"])
        P.tt(DVE, sm[:, 2:3], sm[:, 1:2], sm[:, 0:1], ALU.subtract, [kk + "m1", kk + "m2"], [kk + "d"])
        P.act(sm[:, 2:3], sm[:, 2:3], AF.Exp, [kk + "d"], [kk + "d"])
        P.ts(DVE, sm[:, 3:4], sm[:, 2:3], 1.0, None, ALU.add, ALU.bypass, [kk + "d"], [kk + "g1"])
        P.op(DVE, lambda en: en.reciprocal(sm[:, 3:4], sm[:, 3:4]), [kk + "g1"], [kk + "g1"])
        P.tt(DVE, sm[:, 4:5], sm[:, 2:3], sm[:, 3:4], ALU.mult, [kk + "d", kk + "g1"], [kk + "g2"])
        P.ts(DVE, sm[:, 8:16], sm[:, 8:16], sm[:, 3:4], None, ALU.mult, ALU.bypass, [kk + "k1", kk + "g1"], [kk + "k1"])
        P.stt(C.gate_sb[:, t, :], sm[:, 16:24], sm[:, 4:5], sm[:, 8:16], ALU.mult, ALU.add,
              [kk + "k2", kk + "g2", kk + "k1"], [("gate", t)])


def ln_bufs(P):
    b = {}
    for nm, shape, dt in [("xr", [128, D], F32), ("y", [128, D], F32), ("xo", [128, D], F32), ("xb", [128, D], BF16),
                          ("xt", [128, 8, 128], BF16), ("st", [128, 2, 6], F32), ("mv", [128, 2], F32),
                          ("rs", [128, 1], F32), ("nmr", [128, 1], F32), ("sm", [128, 40], F32)]:
        b[nm] = [P.sb(f"ln_{nm}{i}", shape, dt) for i in range(2)]
        b["k" + nm] = [f"ln_{nm}{i}" for i in range(2)]
    return b


def phase_outproj(C, layer):
    nc = C.nc
    P = Phase(nc, f"op{layer}")
    moe = (layer % 2 == 1)
    ident = P.sb("ident", [128, 128], BF16)
    P.dma(SP, ident[:], C.ident_bf, [], ["ident"], "c0")
    gb = [P.sb("ln_g", [128, D], F32), P.sb("ln_b", [128, D], F32)]
    P.dma(SP, gb[0][:], C.ln1_g_bc[layer], [], ["ln_g"], "c1")
    P.dma(SP, gb[1][:], C.ln1_b_bc[layer], [], ["ln_b"], "c2")
    router = None
    if moe:
        rbc = P.sb("rbc", [128, NE, D], F32)
        P.dma(SP, rbc[:], C.router_bc[layer // 2], [], ["rbc"], "c3")
        lg = [P.sb(f"lg{i}", [128, NE], F32) for i in range(2)]
        junk = P.sb("junk", [128, D], F32)
        router = (rbc, lg, junk)
    Wo = P.sb("Wo", [128, 8, D], BF16)
    stg = [P.sb(f"stg{i}", [128, 1024], F32) for i in range(3)]
    load_cast_w(P, Wo, C.w_out[layer], 8, D, [], "Wo", stg, 1024, [0])
    bufs = ln_bufs(P)
    ct = [P.sb(f"ct{i}", [128, 8, 512], BF16) for i in range(2)]
    ps = [P.ps(f"ps{i}", [128, 512], F32) for i in range(4)]
    pt = [P.ps(f"pt{i}", [128, 8, 128], BF16) for i in range(2)]
    P.excl.update(["ps0", "ps1", "ps2", "ps3", "pt0", "pt1"])
    catT3 = C.catT.rearrange("(c p) t -> p c t", p=128)
    res_src = C.x if layer == 0 else C.xres
    for tb in range(S // 512):
        b = tb % 2
        P.dma(SP, ct[b][:], catT3[:, :, tb * 512:(tb + 1) * 512], [], [f"ct{b}"], f"ct{b}")
        for tt in range(4):
            t = tb * 4 + tt
            pp = (t % 2) * 2
            for hf in range(2):
                for c in range(8):
                    P.mm(ps[pp + hf][:], ct[b][:, c, tt * 128:(tt + 1) * 128], Wo[:, c, hf * 512:(hf + 1) * 512],
                         c == 0, c == 7, [f"ct{b}", "Wo"], [f"ps{pp + hf}"])
            ln_epilogue(P, C, t, [ps[pp][:], ps[pp + 1][:]], [f"ps{pp}", f"ps{pp + 1}"], res_src, gb, ident, bufs,
                        pt[t % 2][:], f"pt{t % 2}", router=router)
    return P.emit()


def phase_ffn(C, layer, last):
    nc = C.nc
    P = Phase(nc, f"ff{layer}")
    moe = (layer % 2 == 1)
    li = layer // 2
    if moe:
        E, F = NE, DFE
        wg_of = lambda e: C.moe_w_gate[li, e]
        wu_of = lambda e: C.moe_w_up[li, e]
        wd_of = lambda e: C.moe_w_down[li, e]
    else:
        E, F = 1, DFF
        wg_of = lambda e: C.ffn_w_gate[li]
        wu_of = lambda e: C.ffn_w_up[li]
        wd_of = lambda e: C.ffn_w_down[li]
    nfc = F // 128
    groups = [(f0, min(4, nfc - f0)) for f0 in range(0, nfc, 4)]
    SBT = 16
    ident = P.sb("ident", [128, 128], BF16)
    P.dma(SP, ident[:], C.ident_bf, [], ["ident"], "c0")
    gb = [P.sb("ln_g", [128, D], F32), P.sb("ln_b", [128, D], F32)]
    P.dma(SP, gb[0][:], C.ln2_g_bc[layer], [], ["ln_g"], "c1")
    P.dma(SP, gb[1][:], C.ln2_b_bc[layer], [], ["ln_b"], "c2")
    acc = P.sb("acc", [128, SBT, D], F32)
    xTs = P.sb("xTs", [128, 8, SBT * 128], BF16)
    Wg = [P.sb(f"Wg{i}", [128, 8, 512], BF16) for i in range(2)]
    Wu = [P.sb(f"Wu{i}", [128, 8, 512], BF16) for i in range(2)]
    Wd = [P.sb(f"Wd{i}", [128, 4, D], BF16) for i in range(2)]
    stg = [P.sb(f"stg{i}", [128, 2048], F32) for i in range(3)]
    hT = [P.sb(f"hT{i}", [128, 4, 512], BF16) for i in range(2)]
    sg = [P.sb(f"sg{i}", [128, 512], F32) for i in range(2)]
    bufs = {}
    for nm, shape, dt in [("xb", [128, D], BF16), ("xt", [128, 8, 128], BF16), ("st", [128, 2, 6], F32),
                          ("mv", [128, 2], F32), ("rs", [128, 1], F32), ("nmr", [128, 1], F32), ("sm", [128, 40], F32)]:
        bufs[nm] = [P.sb(f"ln_{nm}{i}", shape, dt) for i in range(2)]
        bufs["k" + nm] = [f"ln_{nm}{i}" for i in range(2)]
    bufs["xr"] = [stg[0], stg[0]]
    bufs["kxr"] = ["stg0", "stg0"]
    bufs["y"] = [stg[1], stg[1]]
    bufs["ky"] = ["stg1", "stg1"]
    bufs["xo"] = [stg[2], stg[2]]
    bufs["kxo"] = ["stg2", "stg2"]
    pg = [P.ps(f"pg{i}", [128, 512], F32) for i in range(2)]
    pu = [P.ps(f"pu{i}", [128, 512], F32) for i in range(2)]
    py = [P.ps(f"py{i}", [128, 512], F32) for i in range(4)]
    P.excl.update(["pg0", "pg1", "pu0", "pu1", "py0", "py1", "py2", "py3"])
    xT3 = C.xT.rearrange("(c p) t -> p c t", p=128)
    cnt = [0]
    ngu = 0
    nh = 0
    ny = 0
    wi = 0
    import os as _os
    dbg_sb = int(_os.environ.get("FF_SB", NT // SBT))
    dbg_ng = int(_os.environ.get("FF_NG", 10 ** 6))
    dbg_noln = bool(int(_os.environ.get("FF_NOLN", "0")))
    for sbk in range(min(NT // SBT, dbg_sb)):
        t0 = sbk * SBT
        for q4 in range(4):
            P.dma(SP, xTs[:, :, q4 * 512:(q4 + 1) * 512], xT3[:, :, t0 * 128 + q4 * 512:t0 * 128 + (q4 + 1) * 512],
                  [("xT", t0 + q4 * 4 + i) for i in range(4)], [("xTs", q4)], f"xTs{q4}")
        first = [True] * SBT
        for e in range(E):
            for (f0, nf) in groups[:dbg_ng]:
                wb = wi % 2
                wi += 1
                for c in range(8):
                    for (dst, src, key) in ((Wg[wb], wg_of(e), f"Wg{wb}"), (Wu[wb], wu_of(e), f"Wu{wb}")):
                        i = cnt[0] % 3
                        cnt[0] += 1
                        P.dma(SP, stg[i][:, 0:nf * 128], src[c * 128:(c + 1) * 128, f0 * 128:(f0 + nf) * 128], [],
                              [f"stg{i}"], f"stg{i}")
                        P.copy(POOL, dst[:, c, 0:nf * 128], stg[i][:, 0:nf * 128], [f"stg{i}"], [key])
                for fc in range(0, nf, 2):
                    n2 = min(2, nf - fc)
                    i = cnt[0] % 3
                    cnt[0] += 1
                    src = wd_of(e)[(f0 + fc) * 128:(f0 + fc + n2) * 128, :].rearrange("(a p) d -> p a d", p=128)
                    P.dma(SP, stg[i][:, 0:n2 * D].rearrange("p (a d) -> p a d", a=n2), src, [], [f"stg{i}"], f"stg{i}")
                    P.copy(POOL, Wd[wb][:, fc:fc + n2, :], stg[i][:, 0:n2 * D].rearrange("p (a d) -> p a d", a=n2),
                           [f"stg{i}"], [f"Wd{wb}"])
                for q4 in range(SBT // 4):
                    hb = nh % 2
                    nh += 1
                    for fc in range(nf):
                        gi = ngu % 2
                        ngu += 1
                        for c in range(8):
                            P.mm(pg[gi][:], Wg[wb][:, c, fc * 128:(fc + 1) * 128], xTs[:, c, q4 * 512:(q4 + 1) * 512],
                                 c == 0, c == 7, [f"Wg{wb}", ("xTs", q4)], [f"pg{gi}"])
                        for c in range(8):
                            P.mm(pu[gi][:], Wu[wb][:, c, fc * 128:(fc + 1) * 128], xTs[:, c, q4 * 512:(q4 + 1) * 512],
                                 c == 0, c == 7, [f"Wu{wb}", ("xTs", q4)], [f"pu{gi}"])
                        P.act(sg[gi][:], pg[gi][:], AF.Silu, [f"pg{gi}"], [f"sg{gi}"])
                        P.tt(DVE, hT[hb][:, fc, :], sg[gi][:], pu[gi][:], ALU.mult, [f"sg{gi}", f"pu{gi}"], [(f"hT{hb}", fc)])
                    for tt in range(4):
                        tl = q4 * 4 + tt
                        yb = (ny % 2) * 2
                        ny += 1
                        for hf in range(2):
                            for fc in range(nf):
                                P.mm(py[yb + hf][:], hT[hb][:, fc, tt * 128:(tt + 1) * 128], Wd[wb][:, fc, hf * 512:(hf + 1) * 512],
                                     fc == 0, fc == nf - 1, [(f"hT{hb}", fc), f"Wd{wb}"], [f"py{yb + hf}"])
                            dst = acc[:, tl, hf * 512:(hf + 1) * 512]
                            akey = ("acc", tl, hf)
                            if moe:
                                gsc = C.gate_sb[:, t0 + tl, e:e + 1]
                                if first[tl]:
                                    P.ts(DVE, dst, py[yb + hf][:], gsc, None, ALU.mult, ALU.bypass, [f"py{yb + hf}", ("gate", t0 + tl)], [akey])
                                else:
                                    P.stt(dst, py[yb + hf][:], gsc, dst, ALU.mult, ALU.add, [f"py{yb + hf}", akey, ("gate", t0 + tl)], [akey])
                            else:
                                if first[tl]:
                                    P.copy(DVE, dst, py[yb + hf][:], [f"py{yb + hf}"], [akey])
                                else:
                                    P.tt(DVE, dst, py[yb + hf][:], dst, ALU.add, [f"py{yb + hf}", akey], [akey])
                        first[tl] = False
        for tl in range(0 if dbg_noln else SBT):
            t = t0 + tl
            ptv = py[tl % 4][:].bitcast(BF16).rearrange("p (c t) -> p c t", c=8)
            ln_epilogue(P, C, t, [acc[:, tl, 0:512], acc[:, tl, 512:1024]], [("acc", tl, 0), ("acc", tl, 1)], C.xres, gb, ident,
                        bufs, ptv, f"py{tl % 4}", out_dst=(C.out if last else None))
    return P.emit()


def bc(ap, shape):
    return ap.to_broadcast(list(shape))


def phase_dn(C, layer):
    nc = C.nc
    P = Phase(nc, f"dn{layer}")
    identb = P.sb("identb", [128, 128], BF16)
    identf = P.sb("identf", [128, 128], F32)
    onesf = P.sb("onesf", [128, 128], F32)
    trif = P.sb("trif", [128, 128], F32)
    mcaus = P.sb("mcaus", [128, 128], F32)
    mneg = P.sb("mneg", [128, 128], F32)
    cw = P.sb("cw", [128, 4, 1536], F32)
    alog = P.sb("alog", [128, 8], F32)
    dtb = P.sb("dtb", [128, 8], F32)
    wn = P.sb("wn", [128, 64], F32)
    for i, (dst, src, key) in enumerate([(identb, C.ident_bf, "identb"), (identf, C.ident_f32, "identf"), (onesf, C.ones_f32, "onesf"),
                                         (trif, C.tri_f32, "trif"), (mcaus, C.mcausT, "mcaus"), (mneg, C.mnegT, "mneg"),
                                         (alog, C.a_log_bc[layer], "alog"), (dtb, C.dt_bias_bc[layer], "dtb"),
                                         (wn, C.dn_norm_bc[layer], "wn")]):
        P.dma(SP, dst[:], src, [], [key], f"c{i}")
    P.dma(SP, cw[:], C.conv_w_bc[layer].rearrange("p (j c) -> p j c", j=4), [], ["cw"], "c_cw")

    def g8(name):
        return P.sb(name, [128, NT, 8], F32)
    x8, gg8, beta8, gc8, glb8, eg8 = [g8(n) for n in ("x8", "gg8", "beta8", "gc8", "glb8", "eg8")]
    kd8, negeg8, egl8 = x8, gg8, glb8
    P.alias = {"kd8": "x8", "negeg8": "gg8", "egl8": "glb8"}
    eglS = P.sb("eglS", [128, NT, 4], F32)
    negA = P.sb("negA", [128, 8], F32)
    pp = [P.ps(f"pp{i}", [128, 512], F32) for i in range(4)]
    rA, rB, rC, rD = [P.ps(f"r{n}", [128, 512], F32) for n in "ABCD"]
    P.excl.update(["pp0", "pp1", "pp2", "pp3", "rK0", "rK1", "rC", "rD"])
    P.act(negA[:], alog[:], AF.Exp, ["alog"], ["negA"])
    P.ts(DVE, negA[:], negA[:], -1.0, None, ALU.mult, ALU.bypass, ["negA"], ["negA"])
    for h in range(8):
        P.ts(DVE, x8[:, :, h], C.ab_sb[:, :, h], dtb[:, h:h + 1], None, ALU.add, ALU.bypass, ["dtb"], ["x8"])
    P.act(x8[:], x8[:], AF.Exp, ["x8"], ["x8"])
    P.act(x8[:], x8[:], AF.Ln, ["x8"], ["x8"], bias=1.0)
    for h in range(8):
        P.ts(DVE, gg8[:, :, h], x8[:, :, h], negA[:, h:h + 1], None, ALU.mult, ALU.bypass, ["x8", "negA"], ["gg8"])
    P.act(beta8[:], C.ab_sb[:, :, 8:16], AF.Exp, [], ["beta8"], scale=-1.0)
    P.ts(DVE, beta8[:], beta8[:], 1.0, None, ALU.add, ALU.bypass, ["beta8"], ["beta8"])
    P.op(DVE, lambda e: e.reciprocal(beta8[:], beta8[:]), ["beta8"], ["beta8"])
    ggf = gg8[:].rearrange("p t h -> p (t h)")
    P.mm(pp[0][:], trif[:], ggf, True, True, ["trif", "gg8"], ["pp0"])
    P.mm(pp[1][:], onesf[:], ggf, True, True, ["onesf", "gg8"], ["pp1"])
    P.copy(DVE, gc8[:].rearrange("p t h -> p (t h)"), pp[0][:], ["pp0"], ["gc8"])
    P.copy(DVE, glb8[:].rearrange("p t h -> p (t h)"), pp[1][:], ["pp1"], ["glb8"])
    P.act(eg8[:], gc8[:], AF.Exp, ["gc8"], ["eg8"])
    P.ts(DVE, negeg8[:], eg8[:], -1.0, None, ALU.mult, ALU.bypass, ["eg8"], ["negeg8"])
    P.tt(DVE, kd8[:], glb8[:], gc8[:], ALU.subtract, ["glb8", "gc8"], ["kd8"])
    P.act(kd8[:], kd8[:], AF.Exp, ["kd8"], ["kd8"])
    P.act(egl8[:], glb8[:], AF.Exp, ["glb8"], ["egl8"])
    for par in range(2):
        P.copy(DVE, eglS[par * 64:(par + 1) * 64, :, :], egl8[par * 64:(par + 1) * 64, :, par::2], ["egl8"], ["eglS"])

    cv = [P.sb(f"cv{i}", [128, 4, 1536], F32) for i in range(2)]
    act = [P.sb(f"act{i}", [128, 1536], F32) for i in range(2)]
    ss = [P.sb(f"ss{i}", [128, 16], F32) for i in range(2)]
    qkb = [P.sb(f"qkb{i}", [128, 1024], BF16) for i in range(2)]
    dg = [P.sb(f"dg{i}", [128, 4, 128], F32) for i in range(2)]
    dsb = [P.sb(f"dsb{i}", [128, 4, 128], F32) for i in range(2)]
    DTs = [P.sb(f"DTs{i}", [128, 4, 128], F32) for i in range(2)]
    DTc = [P.sb(f"DTc{i}", [128, 4, 128], F32) for i in range(2)]
    Mb = [[P.sb(f"M{i}_{k}", [128, 8, 128], BF16) for k in range(2)] for i in range(2)]
    MTb = [[P.sb(f"MT{i}_{k}", [128, 8, 128], BF16) for k in range(2)] for i in range(2)]
    PTf = [P.sb(f"PTf{i}", [128, 8, 128], F32) for i in range(2)]
    PTw = [P.sb(f"PTw{i}", [128, 8, 128], BF16) for i in range(2)]
    qkT = [P.sb(f"qkT{i}", [128, 8, 128], BF16) for i in range(3)]
    PTb = [P.sb(f"PTb{i}", [128, 8, 128], BF16) for i in range(3)]
    inT = [P.sb(f"inT{i}", [128, 8, 128], BF16) for i in range(3)]
    v_f = [P.sb(f"v_f{i}", [128, 512], F32) for i in range(3)]
    kdec = [P.sb(f"kdec{i}", [128, 512], BF16) for i in range(3)]
    zt = [P.sb(f"zt{i}", [128, 512], F32) for i in range(3)]
    o_f = [P.sb(f"o_f{i}", [128, 512], F32) for i in range(3)]
    Sf = P.sb("Sf", [128, 4, 64], F32)
    Sb = P.sb("Sb", [128, 4, 64], BF16)
    tmp = P.sb("tmp", [128, 8, 64], F32)
    rp = P.sb("rp", [128, 8, 64], BF16)
    vn = P.sb("vn", [128, 8, 64], BF16)
    o2 = [P.sb(f"o2{i}", [128, 512], F32) for i in range(2)]
    ssn = [P.sb(f"ssn{i}", [128, 8], F32) for i in range(2)]
    o_bf = [P.sb(f"o_bf{i}", [128, 512], BF16) for i in range(2)]
    oT = [P.sb(f"oT{i}", [128, 4, 128], BF16) for i in range(2)]
    P.op(DVE, lambda e: e.memset(Sf[:], 0.0), [], ["Sf"])
    P.op(DVE, lambda e: e.memset(Sb[:], 0.0), [], ["Sb"])
    pp_free = [0, 1, 2, 3]

    def acquire(n):
        while len(pp_free) < n:
            yield
        return [pp_free.pop(0) for _ in range(n)]

    def release(*idx):
        pp_free.extend(idx)

    def hp(h):
        return (h % 2) * 64

    def prep(t):
        pb = t % 2
        hb = t % 3
        K = lambda s: f"{s}{pb}"
        H = lambda s: f"{s}{hb}"
        src = bass.AP(tensor=C.qkvpre.tensor, offset=t * 128 * 1536, ap=[[1536, 128], [1536, 4], [1, 1536]])
        P.dma(SP, cv[pb][:], src, [("qkvpre", t)], [K("cv")], K("cv"))
        yield
        P.tt(POOL, cv[pb][:], cv[pb][:], cw[:], ALU.mult, [K("cv"), "cw"], [K("cv")])
        yield
        P.tt(DVE, cv[pb][:, 0:2, :], cv[pb][:, 0:2, :], cv[pb][:, 2:4, :], ALU.add, [K("cv")], [K("cv")])
        P.tt(DVE, act[pb][:], cv[pb][:, 0, :], cv[pb][:, 1, :], ALU.add, [K("cv")], [K("act")])
        yield
        P.act(act[pb][:], act[pb][:], AF.Silu, [K("act")], [K("act")])
        yield
        sqv = cv[pb][:, 2, 0:1024]
        P.tt(POOL, sqv, act[pb][:, 0:1024], act[pb][:, 0:1024], ALU.mult, [K("act")], [K("cv")])
        P.copy(POOL, v_f[hb][:], act[pb][:, 1024:1536], [K("act")], [H("v_f")])
        yield
        P.op(DVE, lambda e: e.tensor_reduce(ss[pb][:], sqv.rearrange("p (h d) -> p h d", h=16), AX.X, ALU.add),
             [K("cv")], [K("ss")])
        P.ts(DVE, ss[pb][:], ss[pb][:], 1e-6, None, ALU.add, ALU.bypass, [K("ss")], [K("ss")])
        yield
        P.act(ss[pb][:], ss[pb][:], AF.Ln, [K("ss")], [K("ss")])
        P.act(ss[pb][:], ss[pb][:], AF.Exp, [K("ss")], [K("ss")], scale=-0.5)
        yield
        P.ts(DVE, ss[pb][:, 0:8], ss[pb][:, 0:8], 0.125, None, ALU.mult, ALU.bypass, [K("ss")], [K("ss")])
        P.tt(DVE, qkb[pb][:].rearrange("p (h d) -> p h d", h=16), act[pb][:, 0:1024].rearrange("p (h d) -> p h d", h=16),
             bc(ss[pb][:].unsqueeze(2), [128, 16, 64]), ALU.mult, [K("act"), K("ss")], [K("qkb")])
        yield
        P.tt(POOL, kdec[hb][:].rearrange("p (h d) -> p h d", h=8), qkb[pb][:, 512:1024].rearrange("p (h d) -> p h d", h=8),
             bc(kd8[:, t, :].unsqueeze(2), [128, 8, 64]), ALU.mult, [K("qkb"), "kd8"], [H("kdec")])
        (pi,) = yield from acquire(1)
        ptv = pp[pi][:].bitcast(BF16).rearrange("p (a t) -> p a t", a=8)
        for a in range(8):
            P.tr(ptv[:, a, :], qkb[pb][:, a * 128:(a + 1) * 128], identb[:], [K("qkb"), "identb"], [f"pp{pi}"])
        yield
        P.copy(ACT, qkT[hb][:], ptv, [f"pp{pi}"], [H("qkT")])
        release(pi)
        yield
        for hg in range(2):
            iG, iQ, iR = yield from acquire(3)
            P.tt(POOL, dg[pb][:], bc(identf[:].unsqueeze(1), [128, 4, 128]),
                 bc(gc8[:, t, hg::2].unsqueeze(2), [128, 4, 128]), ALU.mult, ["identf", "gc8"], [K("dg")])
            for h4 in range(4):
                h = 2 * h4 + hg
                kT_h = qkT[hb][hp(h):hp(h) + 64, 4 + h // 2, :]
                qT_h = qkT[hb][hp(h):hp(h) + 64, h // 2, :]
                P.mm(pp[iG][:, h4 * 128:(h4 + 1) * 128], kT_h, kT_h, True, True, [H("qkT")], [f"pp{iG}"])
                P.mm(pp[iQ][:, h4 * 128:(h4 + 1) * 128], kT_h, qT_h, True, True, [H("qkT")], [f"pp{iQ}"])
            P.mm(pp[iR][:], onesf[:], dg[pb][:].rearrange("p h i -> p (h i)"), True, True,
                 ["onesf", K("dg")], [f"pp{iR}"])
            yield
            for h4 in range(4):
                h = 2 * h4 + hg
                P.ts(DVE, dsb[pb][:, h4, :], pp[iR][:, h4 * 128:(h4 + 1) * 128], gc8[:, t, h:h + 1], 0.0,
                     ALU.subtract, ALU.min, [f"pp{iR}", "gc8"], [K("dsb")])
            release(iR)
            yield
            hs = slice(hg * 4, (hg + 1) * 4)
            P.act(dsb[pb][:], dsb[pb][:], AF.Exp, [K("dsb")], [K("dsb")])
            yield
            P.tt(POOL, DTs[pb][:], dsb[pb][:], bc(mneg[:].unsqueeze(1), [128, 4, 128]), ALU.mult,
                 [K("dsb"), "mneg"], [K("DTs")])
            P.tt(POOL, DTc[pb][:], dsb[pb][:], bc(mcaus[:].unsqueeze(1), [128, 4, 128]), ALU.mult,
                 [K("dsb"), "mcaus"], [K("DTc")])
            yield
            for h4 in range(4):
                h = 2 * h4 + hg
                P.stt(MTb[pb][0][:, hg * 4 + h4, :], pp[iG][:, h4 * 128:(h4 + 1) * 128], beta8[:, t, h:h + 1],
                      DTs[pb][:, h4, :], ALU.mult, ALU.mult, [f"pp{iG}", "beta8", K("DTs")],
                      [K("MT") + f"0_{hg}"])
            P.tt(DVE, inT[hb][:, hs, :], pp[iQ][:].rearrange("p (h i) -> p h i", h=4), DTc[pb][:], ALU.mult,
                 [f"pp{iQ}", K("DTc")], [H("inT") + f"_{hg}"])
            release(iG, iQ)
            yield
        (pi,) = yield from acquire(1)
        ptv = pp[pi][:].bitcast(BF16).rearrange("p (a t) -> p a t", a=8)
        for h in range(8):
            P.tr(ptv[:, h, :], MTb[pb][0][:, h, :], identb[:], [K("MT") + f"0_{h // 4}", "identb"], [f"pp{pi}"])
        P.tt(POOL, PTf[pb][:], MTb[pb][0][:], bc(identf[:].unsqueeze(1), [128, 8, 128]), ALU.add,
             [K("MT") + "0_0", K("MT") + "0_1", "identf"], [K("PTf") + "_0", K("PTf") + "_1"])
        P.tt(POOL, PTw[pb][:], MTb[pb][0][:], bc(identf[:].unsqueeze(1), [128, 8, 128]), ALU.add,
             [K("MT") + "0_0", K("MT") + "0_1", "identf"], [K("PTw") + "_0", K("PTw") + "_1"])
        yield
        P.copy(ACT, Mb[pb][0][:, 0:4, :], ptv[:, 0:4, :], [f"pp{pi}"], [K("M") + "0_0"])
        P.copy(DVE, Mb[pb][0][:, 4:8, :], ptv[:, 4:8, :], [f"pp{pi}"], [K("M") + "0_1"])
        release(pi)
        yield
        for s in range(1, 7):
            cur, prv = s % 2, (s - 1) % 2
            for hg in range(2):
                hs = slice(hg * 4, (hg + 1) * 4)
                kM, kMT = K("M") + f"{cur}_{hg}", K("MT") + f"{cur}_{hg}"
                kMp, kMTp = K("M") + f"{prv}_{hg}", K("MT") + f"{prv}_{hg}"
                if s <= 5:
                    iM, iT = yield from acquire(2)
                else:
                    (iM,) = yield from acquire(1)
                for h4 in range(4):
                    h = hg * 4 + h4
                    P.mm(pp[iM][:, h4 * 128:(h4 + 1) * 128], MTb[pb][prv][:, h, :], Mb[pb][prv][:, h, :], True, True,
                         [kMp, kMTp], [f"pp{iM}"])
                if s <= 5:
                    for h4 in range(4):
                        h = hg * 4 + h4
                        P.mm(pp[iT][:, h4 * 128:(h4 + 1) * 128], Mb[pb][prv][:, h, :], MTb[pb][prv][:, h, :], True, True,
                             [kMp, kMTp], [f"pp{iT}"])
                yield
                P.copy(ACT, Mb[pb][cur][:, hs, :], pp[iM][:].rearrange("p (h i) -> p h i", h=4), [f"pp{iM}"], [kM])
                if s <= 5:
                    P.copy(DVE, MTb[pb][cur][:, hs, :], pp[iT][:].rearrange("p (h i) -> p h i", h=4), [f"pp{iT}"], [kMT])
                    release(iT)
                release(iM)
                yield
                (iA,) = yield from acquire(1)
                for h4 in range(4):
                    h = hg * 4 + h4
                    P.mm(pp[iA][:, h4 * 128:(h4 + 1) * 128], Mb[pb][cur][:, h, :], PTw[pb][:, h, :], True, True,
                         [kM, K("PTw") + f"_{hg}"], [f"pp{iA}"])
                yield
                P.tt(DVE, PTf[pb][:, hs, :], PTf[pb][:, hs, :], pp[iA][:].rearrange("p (h i) -> p h i", h=4), ALU.add,
                     [K("PTf") + f"_{hg}", f"pp{iA}"], [K("PTf") + f"_{hg}"])
                release(iA)
                yield
                if s < 6:
                    P.copy(ACT, PTw[pb][:, hs, :], PTf[pb][:, hs, :], [K("PTf") + f"_{hg}"], [K("PTw") + f"_{hg}"])
                else:
                    P.copy(ACT, PTb[hb][:, hs, :], PTf[pb][:, hs, :], [K("PTf") + f"_{hg}"], [H("PTb") + f"_{hg}"])
                yield

    def hi(h):
        return (h % 2) * 4 + h // 2

    def recur(t):
        hb = t % 3
        H = lambda s: f"{s}{hb}"
        rK = [rA, rB]
        rCv = rC[:].rearrange("p (h d) -> p h d", h=8)
        rDv = rD[:, 0:256].rearrange("p (a d) -> p a d", a=4)
        tmp4 = tmp[:].rearrange("p (a q) d -> p a q d", q=2)
        for h in range(8):
            par, a = h % 2, h // 2
            kT_h = qkT[hb][hp(h):hp(h) + 64, 4 + a, :]
            qT_h = qkT[hb][hp(h):hp(h) + 64, a, :]
            S_h = Sb[hp(h):hp(h) + 64, a, :]
            P.mm(rK[par][:, a * 64:(a + 1) * 64], kT_h, S_h, True, True, [H("qkT"), "Sb"], [f"rK{par}"])
            P.mm(rK[par][:, 256 + a * 64:256 + (a + 1) * 64], qT_h, S_h, True, True, [H("qkT"), "Sb"], [f"rK{par}"])
        yield
        for par in range(2):
            P.tt(DVE, tmp4[:, :, par, :], rK[par][:, 0:256].rearrange("p (a d) -> p a d", a=4),
                 bc(negeg8[:, t, par::2].unsqueeze(2), [128, 4, 64]), ALU.mult, [f"rK{par}", "negeg8"], ["tmp"])
        P.tt(DVE, rp[:], tmp[:], v_f[hb][:].rearrange("p (h d) -> p h d", h=8), ALU.add, ["tmp", H("v_f")], ["rp"])
        yield
        for h in range(8):
            P.mm(rCv[:, h, :], PTb[hb][:, hi(h), :], rp[:, h, :], True, True, [H("PTb") + f"_{h % 2}", "rp"], ["rC"])
        yield
        P.tt(DVE, vn[:], rCv, bc(beta8[:, t, :].unsqueeze(2), [128, 8, 64]), ALU.mult, ["rC", "beta8"], ["vn"])
        yield
        for h in range(8):
            P.mm(rDv[hp(h):hp(h) + 64, h // 2, :], kdec[hb][:, h * 64:(h + 1) * 64], vn[:, h, :], True, True,
                 [H("kdec"), "vn"], ["rD"])
        for h in range(8):
            P.mm(rCv[:, h, :], inT[hb][:, hi(h), :], vn[:, h, :], True, True, [H("inT") + f"_{h % 2}", "vn"], ["rC"])
        yield
        P.tt(DVE, Sf[:], Sf[:], bc(eglS[:, t, :].unsqueeze(2), [128, 4, 64]), ALU.mult, ["Sf", "eglS"], ["Sf"])
        P.tt(DVE, Sb[:], Sf[:], rDv, ALU.add, ["Sf", "rD"], ["Sb"])
        P.tt(DVE, Sf[:], Sf[:], rDv, ALU.add, ["Sf", "rD"], ["Sf"])
        for par in range(2):
            P.tt(DVE, tmp4[:, :, par, :], rK[par][:, 256:512].rearrange("p (a d) -> p a d", a=4),
                 bc(eg8[:, t, par::2].unsqueeze(2), [128, 4, 64]), ALU.mult, [f"rK{par}", "eg8"], ["tmp"])
        P.tt(DVE, o_f[hb][:].rearrange("p (h d) -> p h d", h=8), tmp[:], rCv, ALU.add, ["tmp", "rC"], [H("o_f")])
        yield

    def epi(t):
        hb = t % 3
        H = lambda s: f"{s}{hb}"
        ob = t % 2
        E = lambda s: f"{s}{ob}"
        P.dma(SP, zt[hb][:], C.zbuf[t * 128:(t + 1) * 128, :], [], [H("zt")], H("zt"))
        P.tt(POOL, o2[ob][:], o_f[hb][:], o_f[hb][:], ALU.mult, [H("o_f")], [E("o2")])
        yield
        P.act(zt[hb][:], zt[hb][:], AF.Silu, [H("zt")], [H("zt")])
        P.op(DVE, lambda e: e.tensor_reduce(ssn[ob][:], o2[ob][:].rearrange("p (h d) -> p h d", h=8), AX.X, ALU.add),
             [E("o2")], [E("ssn")])
        P.ts(DVE, ssn[ob][:], ssn[ob][:], 1.0 / 64.0, 1e-6, ALU.mult, ALU.add, [E("ssn")], [E("ssn")])
        yield
        P.act(ssn[ob][:], ssn[ob][:], AF.Ln, [E("ssn")], [E("ssn")])
        P.act(ssn[ob][:], ssn[ob][:], AF.Exp, [E("ssn")], [E("ssn")], scale=-0.5)
        yield
        P.tt(POOL, o2[ob][:].rearrange("p (h d) -> p h d", h=8), o_f[hb][:].rearrange("p (h d) -> p h d", h=8),
             bc(ssn[ob][:].unsqueeze(2), [128, 8, 64]), ALU.mult, [H("o_f"), E("ssn")], [E("o2")])
        yield
        P.tt(POOL, o2[ob][:].rearrange("p (h d) -> p h d", h=8), o2[ob][:].rearrange("p (h d) -> p h d", h=8),
             bc(wn[:].unsqueeze(1), [128, 8, 64]), ALU.mult, [E("o2"), "wn"], [E("o2")])
        yield
        P.tt(POOL, o_bf[ob][:], o2[ob][:], zt[hb][:], ALU.mult, [E("o2"), H("zt")], [E("o_bf")])
        yield
        (pi,) = yield from acquire(1)
        ptv = pp[pi][:].bitcast(BF16).rearrange("p (a t) -> p a t", a=8)
        for a in range(4):
            P.tr(ptv[:, a, :], o_bf[ob][:, a * 128:(a + 1) * 128], identb[:], [E("o_bf"), "identb"], [f"pp{pi}"])
        yield
        P.copy(ACT, oT[ob][:], ptv[:, 0:4, :], [f"pp{pi}"], [f"oT{ob}"])
        release(pi)
        dst = C.catT[0:512, t * 128:(t + 1) * 128].rearrange("(a p) t -> p a t", p=128)
        P.dma(POOL, dst, oT[ob][:], [f"oT{ob}"], [("catT_dn", t)], f"oT{ob}")
        yield

    preps = {}
    epis = []
    prep_done = set()
    next_prep = 0
    rec_t = 0
    rec_gen = None
    NTD = C.dn_tiles
    while rec_t < NTD or epis or preps:
        while next_prep < NTD and next_prep <= rec_t + 2 and len(preps) < 2:
            preps[next_prep] = prep(next_prep)
            next_prep += 1
        for tt_ in list(preps):
            try:
                C.dbg_stage = getattr(C, "dbg_stage", 0) + 1
                if C.dbg_stage > getattr(C, "dbg_maxstage", 10 ** 9):
                    raise StopIteration
                next(preps[tt_])
            except StopIteration:
                prep_done.add(tt_)
                del preps[tt_]
        if getattr(C, "dbg_norec", False) and not preps:
            break
        if rec_gen is None and rec_t < NTD and rec_t in prep_done:
            rec_gen = recur(rec_t)
        if rec_gen is not None:
            try:
                next(rec_gen)
            except StopIteration:
                rec_gen = None
                epis.append(epi(rec_t))
                rec_t += 1
        for g in list(epis):
            try:
                next(g)
            except StopIteration:
                epis.remove(g)
    if getattr(C, "dn_dump", None):
        dumps = {"d_qkT": (qkT[0], ["qkT0"]), "d_kdec": (kdec[0], ["kdec0"]), "d_vf": (v_f[0], ["v_f0"]),
                 "d_inT": (inT[0], ["inT0_0", "inT0_1"]), "d_PTb": (PTb[0], ["PTb0_0", "PTb0_1"]),
                 "d_MT0": (MTb[0][0], ["MT00_0", "MT00_1"]), "d_M0": (Mb[0][0], ["M00_0", "M00_1"]),
                 "d_gc8": (gc8, ["gc8"]), "d_beta8": (beta8, ["beta8"]), "d_eg8": (eg8, ["eg8"]), "d_Sf": (Sf, ["Sf"]),
                 "d_of": (o_f[0], ["o_f0"]), "d_gg8": (gg8, ["gg8"]), "d_kd8": (kd8, ["kd8"]), "d_glb8": (glb8, ["glb8"]),
                 "d_act": (act[0], ["act0"]), "d_qkb": (qkb[0], ["qkb0"]), "d_PTf": (PTf[0], ["PTf0_0", "PTf0_1"]),
                 "d_vn": (vn, ["vn"]), "d_rp": (rp, ["rp"])}
        for nm, (tile, keys) in dumps.items():
            if nm in C.dn_dump:
                P.dma(SP, C.dn_dump[nm], tile[:], keys, [nm], nm)
    return P.emit()


INPUT_SHAPES = {
    "x": ([S, D], F32), "w_in": ([DEPTH, D, IN_W], F32), "w_out": ([DEPTH, D, D], F32),
    "ffn_w_gate": ([2, D, DFF], F32), "ffn_w_up": ([2, D, DFF], F32), "ffn_w_down": ([2, DFF, D], F32),
    "moe_w_gate": ([2, NE, D, DFE], F32), "moe_w_up": ([2, NE, D, DFE], F32), "moe_w_down": ([2, NE, DFE, D], F32),
    "ident_bf": ([128, 128], BF16), "ident_f32": ([128, 128], F32), "ones_f32": ([128, 128], F32),
    "tri_f32": ([128, 128], F32), "mcausT": ([128, 128], F32), "mnegT": ([128, 128], F32), "maskneg": ([128, 128], F32),
    "conv_w_bc": ([DEPTH, 128, 6144], F32), "a_log_bc": ([DEPTH, 128, 8], F32), "dt_bias_bc": ([DEPTH, 128, 8], F32),
    "dn_norm_bc": ([DEPTH, 128, 64], F32), "df_lambda_bc": ([DEPTH, 128, 256], F32), "df_subln_col": ([DEPTH, 128, 1], F32),
    "ln1_g_bc": ([DEPTH, 128, D], F32), "ln1_b_bc": ([DEPTH, 128, D], F32), "ln2_g_bc": ([DEPTH, 128, D], F32),
    "ln2_b_bc": ([DEPTH, 128, D], F32), "router_bc": ([2, 128, NE, D], F32),
    "bias_tiles": ([4, 2, 128, 128], F32), "cfar": ([128, 4], F32),
}


DUMP_SHAPES = {"d_qkT": ([128, 8, 128], BF16), "d_kdec": ([128, 512], BF16), "d_vf": ([128, 512], F32),
               "d_inT": ([128, 8, 128], BF16), "d_PTb": ([128, 8, 128], BF16), "d_MT0": ([128, 8, 128], BF16),
               "d_M0": ([128, 8, 128], BF16), "d_gc8": ([128, NT, 8], F32), "d_beta8": ([128, NT, 8], F32),
               "d_eg8": ([128, NT, 8], F32), "d_Sf": ([128, 4, 64], F32), "d_of": ([128, 512], F32),
               "d_gg8": ([128, NT, 8], F32), "d_kd8": ([128, NT, 8], F32), "d_glb8": ([128, NT, 8], F32),
               "d_act": ([128, 1536], F32), "d_qkb": ([128, 1024], BF16), "d_PTf": ([128, 8, 128], F32),
               "d_vn": ([128, 8, 64], BF16), "d_rp": ([128, 8, 64], BF16)}


def build(n_layers=DEPTH, debug=(), stop_after=None, skip_inputs=(), dn_tiles=NT, only=None):
    nc = bass.Bass("TRN2", target_bir_lowering=False)
    C = Ctx()
    C.nc = nc
    C.debug = set(debug)
    C.dn_tiles = dn_tiles
    import os as _os
    C.dbg_maxstage = int(_os.environ.get('DN_MAXSTAGE', 10 ** 9))
    C.dbg_norec = bool(int(_os.environ.get('DN_NOREC', '0')))
    for name, (shape, dt) in INPUT_SHAPES.items():
        if name in skip_inputs:
            continue
        setattr(C, name, nc.dram_tensor(name, list(shape), dt, kind="ExternalInput").ap())

    def dscr(name, shape, dt):
        kind = "ExternalOutput" if name in C.debug else "Internal"
        return nc.dram_tensor(name, list(shape), dt, kind=kind).ap()

    C.out = nc.dram_tensor("out", [S, D], F32, kind="ExternalOutput").ap()
    C.xT = dscr("xT", [D, S], BF16)
    C.xres = dscr("xres", [S, D], F32)
    C.qkvpre = dscr("qkvpre", [S + 3, 1536], F32)
    C.zbuf = dscr("zbuf", [S, 512], F32)
    C.Vdf = dscr("Vdf", [S, 512], BF16)
    C.catT = dscr("catT", [D, S], BF16)
    C.QT = [dscr(f"QT{h}", [128, S], BF16) for h in range(4)]
    C.KT = [dscr(f"KT{h}", [128, S], BF16) for h in range(4)]
    C.dn_dump = {}
    for nm in C.debug:
        if nm.startswith("d_"):
            shp, dt = DUMP_SHAPES[nm]
            C.dn_dump[nm] = nc.dram_tensor(nm, list(shp), dt, kind="ExternalOutput").ap()
    gstack = ExitStack()
    C.ab_sb = gstack.enter_context(nc.sbuf_tensor("ab_sb", [128, NT, 16], F32))
    C.gate_sb = gstack.enter_context(nc.sbuf_tensor("gate_sb", [128, NT, NE], F32))
    stats = {}
    C.stats = stats
    P = Phase(nc, "z0")
    zt = P.sb("zt", [3, 1536], F32)
    P.op(DVE, lambda e: e.memset(zt[:], 0.0), [], ["zt"])
    P.dma(SP, C.qkvpre[0:3, :], zt[:], ["zt"], ["pad"], "zt")
    P.emit()
    if only is None or "x0" in only:
        stats["x0"] = phase_x0(C)
    done = False
    for layer in range(n_layers):
        for nm, fn in (("ip", phase_inproj), ("dn", phase_dn), ("at", phase_attn), ("op", phase_outproj)):
            if only is None or f"{nm}{layer}" in only:
                stats[f"{nm}{layer}"] = fn(C, layer)
            if stop_after == f"{nm}{layer}":
                done = True
                break
        if done:
            break
        if only is None or f"ff{layer}" in only:
            stats[f"ff{layer}"] = phase_ffn(C, layer, layer == n_layers - 1)
        if stop_after == f"ff{layer}":
            break
    gstack.close()
    return nc, C


def t5_bucket_np(dist):
    dist = np.asarray(dist, dtype=np.int64)
    d = np.maximum(dist, 1).astype(np.float32)
    large = 16 + (np.log(d / np.float32(16.0)) / np.float32(math.log(128 / 16)) * np.float32(16.0)).astype(np.int32)
    large = np.minimum(large, 31)
    return np.where(dist < 16, dist, large)


def host_inputs(inputs):
    f32 = np.float32
    ii = np.arange(128)
    m = {}
    m["ident_bf"] = np.eye(128, dtype=f32).astype(ml_dtypes.bfloat16)
    m["ident_f32"] = np.eye(128, dtype=f32)
    m["ones_f32"] = np.ones((128, 128), f32)
    m["tri_f32"] = (ii[:, None] <= ii[None, :]).astype(f32)
    m["mcausT"] = (ii[None, :] >= ii[:, None]).astype(f32)
    m["mnegT"] = -(ii[None, :] > ii[:, None]).astype(f32)
    m["maskneg"] = np.where(ii[None, :] >= ii[:, None], 0.0, -1e5).astype(f32)

    def bc128(a):
        a = np.asarray(a, dtype=f32)
        return np.ascontiguousarray(np.broadcast_to(a[:, None, :], (a.shape[0], 128, a.shape[1])))

    m["conv_w_bc"] = bc128(np.asarray(inputs["conv_w"]).reshape(DEPTH, 4 * 1536))
    m["a_log_bc"] = bc128(inputs["dn_a_log"])
    m["dt_bias_bc"] = bc128(inputs["dn_dt_bias"])
    m["dn_norm_bc"] = bc128(inputs["dn_norm_w"])
    m["df_lambda_bc"] = bc128(np.asarray(inputs["df_lambda"]).reshape(DEPTH, 256))
    m["df_subln_col"] = np.ascontiguousarray(np.asarray(inputs["df_subln_w"], dtype=f32).reshape(DEPTH, 128, 1))
    for k in ("ln1_g", "ln1_b", "ln2_g", "ln2_b"):
        m[k + "_bc"] = bc128(inputs[k])
    r = np.asarray(inputs["moe_router"], dtype=f32).transpose(0, 2, 1)
    m["router_bc"] = np.ascontiguousarray(np.broadcast_to(r[:, None, :, :], (2, 128, NE, D)))
    rb = np.asarray(inputs["rel_bias"], dtype=f32)
    bt = np.zeros((4, 2, 128, 128), f32)
    for rel in range(2):
        dist = rel * 128 + ii[None, :] - ii[:, None]
        bk = t5_bucket_np(np.maximum(dist, 0))
        g = rb[bk]
        g = np.where((dist >= 0)[:, :, None], g, 0.0)
        bt[:, rel] = g.transpose(2, 0, 1)
    m["bias_tiles"] = bt
    m["cfar"] = np.ascontiguousarray(np.broadcast_to(rb[31][None, :], (128, 4)))
    for k in ("w_in", "w_out", "ffn_w_gate", "ffn_w_up", "ffn_w_down", "moe_w_gate", "moe_w_up", "moe_w_down"):
        m[k] = np.ascontiguousarray(np.asarray(inputs[k], dtype=f32))
    return m


def kernel(**inputs):
    nc, C = build()
    shared = host_inputs(inputs)
    in_maps = []
    for b in range(8):
        m = dict(shared)
        m["x"] = np.ascontiguousarray(np.asarray(inputs["x"][b], dtype=np.float32))
        in_maps.append(m)
    res = run_bass_kernel_spmd(nc, in_maps, core_ids=list(range(8)))
    return np.stack([np.asarray(r["out"], dtype=np.float32) for r in res.results], axis=0)
```

```python
import math
import numpy as np
import ml_dtypes
from contextlib import ExitStack
import concourse.bass as bass
import concourse.mybir as mybir
from concourse.bass_utils import run_bass_kernel_spmd

F32 = mybir.dt.float32
BF16 = mybir.dt.bfloat16
AF = mybir.ActivationFunctionType
ALU = mybir.AluOpType
AX = mybir.AxisListType

PE, ACT, DVE, POOL, SP = "pe", "act", "dve", "pool", "sp"
ENGMAP = {PE: "tensor", ACT: "scalar", DVE: "vector", POOL: "gpsimd", SP: "sync"}
SEM_EPOCH = 30000

S = 8192
D = 1024
NT = S // 128
DEPTH = 4
IN_W = 3600
DFF = 2816
DFE = 3584
NE = 8
ALPHA = (2 * DEPTH) ** 0.25
LN_EPS = 1e-5


class Op:
    __slots__ = ("eng", "fn", "deps", "signal", "sem", "val", "is_dma", "grp", "ndep", "seq")

    def __init__(self, eng, fn, is_dma, grp):
        self.eng = eng
        self.fn = fn
        self.deps = []
        self.signal = False
        self.sem = None
        self.val = 0
        self.is_dma = is_dma
        self.grp = grp
        self.ndep = 0


class Phase:
    def __init__(self, nc, name):
        self.nc = nc
        self.name = name
        self.ops = []
        self.last_w = {}
        self.readers = {}
        self.stack = ExitStack()
        self.excl = set()
        self.alias = {}
        self.eng_seq = {}

    def sb(self, name, shape, dt):
        return self.stack.enter_context(self.nc.sbuf_tensor(f"{self.name}_{name}", list(shape), dt))

    def ps(self, name, shape, dt=F32):
        return self.stack.enter_context(self.nc.psum_tensor(f"{self.name}_{name}", list(shape), dt))

    def op(self, eng, fn, r=(), w=(), dma=False, grp=None):
        if self.alias:
            r = [self.alias.get(k, k) for k in r]
            w = [self.alias.get(k, k) for k in w]
        o = Op(eng, fn, dma, grp)
        o.seq = self.eng_seq.get(eng, 0)
        self.eng_seq[eng] = o.seq + 1
        deps = []
        seen = set()
        raw = set()
        for k in r:
            lw = self.last_w.get(k)
            if lw is not None:
                raw.add(id(lw))
                if id(lw) not in seen:
                    seen.add(id(lw))
                    deps.append(lw)
            if k in self.excl:
                for rd in self.readers.get(k, ()):
                    if rd.eng != eng and id(rd) not in seen:
                        seen.add(id(rd))
                        raw.add(id(rd))
                        deps.append(rd)
        for k in w:
            lw = self.last_w.get(k)
            if lw is not None and id(lw) not in seen:
                seen.add(id(lw))
                deps.append(lw)
            for rd in self.readers.get(k, ()):
                if id(rd) not in seen:
                    seen.add(id(rd))
                    deps.append(rd)
        for d in deps:
            if d.eng == eng and not d.is_dma and not dma:
                if eng == PE:
                    continue
                if id(d) not in raw and o.seq - d.seq > 1:
                    continue
            o.deps.append(d)
            d.ndep += 1
        for k in r:
            self.readers.setdefault(k, []).append(o)
        for k in w:
            self.last_w[k] = o
            self.readers[k] = []
        self.ops.append(o)
        return o

    def mm(self, out, lhsT, rhs, start, stop, r, w, **kw):
        return self.op(PE, lambda e: e.matmul(out, lhsT, rhs, start=start, stop=stop, **kw), r, w)

    def tr(self, out, in_, ident, r, w):
        return self.op(PE, lambda e: e.transpose(out, in_, ident), r, w)

    def act(self, out, in_, func, r, w, **kw):
        return self.op(ACT, lambda e: e.activation(out, in_, func, **kw), r, w)

    def copy(self, eng, out, in_, r, w):
        if eng == ACT:
            return self.op(ACT, lambda e: e.copy(out, in_), r, w)
        return self.op(eng, lambda e: e.tensor_copy(out, in_), r, w)

    def tt(self, eng, out, in0, in1, op, r, w):
        return self.op(eng, lambda e: e.tensor_tensor(out, in0, in1, op), r, w)

    def ts(self, eng, out, in0, s1, s2, op0, op1, r, w):
        return self.op(eng, lambda e: e.tensor_scalar(out, in0, s1, s2, op0, op1), r, w)

    def stt(self, out, in0, scalar, in1, op0, op1, r, w):
        return self.op(DVE, lambda e: e.scalar_tensor_tensor(out, in0, scalar, in1, op0, op1), r, w)

    def dma(self, q, out, in_, r, w, grp, **kw):
        return self.op(q, lambda e: e.dma_start(out, in_, **kw), r, w, dma=True, grp=grp)

    def emit(self):
        nc = self.nc
        leaf = [o for o in self.ops if o.is_dma and o.ndep == 0]
        last = {}
        for o in self.ops:
            if not o.is_dma:
                last[o.eng] = o
        leaf += list(last.values())
        if leaf:
            fin = Op(SP, lambda e: e.nop(), False, None)
            fin.deps = leaf
            self.ops.append(fin)
        for o in self.ops:
            for d in o.deps:
                d.signal = True
        sem_state = {}
        nsem = [0]

        sem_handles = []

        def new_sem():
            nsem[0] += 1
            h = nc.alloc_semaphore(name=f"{self.name}_s{nsem[0]}")
            sem_handles.append(h)
            return h

        for o in self.ops:
            if not o.signal:
                continue
            key = ("dma", o.grp) if o.is_dma else o.eng
            st = sem_state.get(key)
            inc = 16 if o.is_dma else 1
            if st is None or st[1] + inc > SEM_EPOCH:
                st = [new_sem(), 0]
                sem_state[key] = st
            st[1] += inc
            o.sem = st[0]
            o.val = st[1]
        self.n_sems = nsem[0]
        per_eng = {}
        for o in self.ops:
            per_eng.setdefault(o.eng, []).append(o)
        with nc.Block() as block:
            for ename, lst in per_eng.items():
                def body(e, lst=lst):
                    waited = {}
                    for o in lst:
                        need = {}
                        for d in o.deps:
                            k = id(d.sem)
                            if k not in need or need[k][1] < d.val:
                                need[k] = (d.sem, d.val)
                        for k, (s, v) in need.items():
                            if waited.get(k, 0) >= v:
                                continue
                            e.wait_ge(s, v)
                            waited[k] = v
                        ins = o.fn(e)
                        if o.signal:
                            ins.then_inc(o.sem, 16 if o.is_dma else 1)
                getattr(block, ENGMAP[ename])(body)
        if sem_handles:
            nc.clear_and_free_semaphores(sem_handles)
            nc.all_engine_barrier()
        nops = len(self.ops)
        self.ops = None
        self.last_w = None
        self.readers = None
        self.stack.close()
        return nops


class Ctx:
    pass


def rr(lst, state=[0]):
    state[0] += 1
    return lst[state[0] % len(lst)]


def phase_x0(C):
    nc = C.nc
    P = Phase(nc, "x0")
    ident = P.sb("ident", [128, 128], BF16)
    P.dma(SP, ident[:], C.ident_bf, [], ["ident"], "ident")
    xin = [P.sb(f"xin{i}", [128, D], F32) for i in range(2)]
    xb = [P.sb(f"xb{i}", [128, D], BF16) for i in range(2)]
    xt = [P.sb(f"xt{i}", [128, 8, 128], BF16) for i in range(2)]
    pt = [P.ps(f"pt{i}", [128, 8, 128], BF16) for i in range(2)]
    P.excl.update(["pt0", "pt1"])
    xT3 = C.xT.rearrange("(c p) t -> p c t", p=128)
    for t in range(NT):
        b = t % 2
        P.dma(SP, xin[b][:], C.x[t * 128:(t + 1) * 128, :], [], [f"xin{b}"], f"xin{b}")
        P.copy(DVE if t % 2 else POOL, xb[b][:], xin[b][:], [f"xin{b}"], [f"xb{b}"])
        for c in range(8):
            P.tr(pt[b][:, c, :], xb[b][:, c * 128:(c + 1) * 128], ident[:], [f"xb{b}", "ident"], [f"pt{b}"])
        P.copy(ACT if t % 2 else DVE, xt[b][:], pt[b][:], [f"pt{b}"], [f"xt{b}"])
        P.dma(POOL, xT3[:, :, t * 128:(t + 1) * 128], xt[b][:], [f"xt{b}"], [("xT", t)], f"xt{b}")
    return P.emit()


def load_cast_w(P, dst3, src2, nchunk, ncols, r_keys, wkey, stg, colstep, cnt):
    for c in range(nchunk):
        for c0 in range(0, ncols, colstep):
            n = min(colstep, ncols - c0)
            i = cnt[0] % len(stg)
            cnt[0] += 1
            P.dma(SP, stg[i][:, 0:n], src2[c * 128:(c + 1) * 128, c0:c0 + n], list(r_keys), [f"stg{i}"], f"stg{i}")
            eng = (DVE, POOL, ACT)[cnt[0] % 3]
            P.copy(eng, dst3[:, c, c0:c0 + n], stg[i][:, 0:n], [f"stg{i}"], [wkey])


def phase_inproj(C, layer):
    nc = C.nc
    P = Phase(nc, f"ip{layer}")
    W = P.sb("W", [128, 8, IN_W], BF16)
    stg = [P.sb(f"stg{i}", [128, 1800], F32) for i in range(3)]
    cnt = [0]
    load_cast_w(P, W, C.w_in[layer], 8, IN_W, [], "W", stg, 1800, cnt)
    xTb = [P.sb(f"xTb{i}", [128, 8, 512], BF16) for i in range(2)]
    qk_sb = [P.sb(f"qk{i}", [128, 512], BF16) for i in range(3)]
    qkv_sb = [P.sb(f"qkv{i}", [128, 1536], F32) for i in range(2)]
    z_sb = [P.sb(f"z{i}", [128, 512], F32) for i in range(2)]
    v_sb = [P.sb(f"v{i}", [128, 512], BF16) for i in range(2)]
    ps = [P.ps(f"ps{i}", [128, 512], F32) for i in range(8)]
    P.excl.update([f"ps{i}" for i in range(8)])
    xT3 = C.xT.rearrange("(c p) t -> p c t", p=128)
    pi = 0
    ev = 0
    nqk = 0
    for tb in range(S // 512):
        b = tb % 2
        P.dma(SP, xTb[b][:], xT3[:, :, tb * 512:(tb + 1) * 512], [("xT", tb * 4 + i) for i in range(4)],
              [f"xTb{b}"], f"xTb{b}")
        for g in range(8):
            col0 = 2064 + g * 128
            p = pi % 8
            pi += 1
            for c in range(8):
                P.mm(ps[p][:, :], W[:, c, col0:col0 + 128], xTb[b][:, c, :], c == 0, c == 7,
                     ["W", f"xTb{b}"], [f"ps{p}"])
            i = nqk % 3
            nqk += 1
            P.copy(ACT if ev % 2 else DVE, qk_sb[i][:], ps[p][:, :], [f"ps{p}"], [f"qk{i}"])
            ev += 1
            dst = C.QT[g] if g < 4 else C.KT[g - 4]
            P.dma(POOL, dst[:, tb * 512:(tb + 1) * 512], qk_sb[i][:], [f"qk{i}"], [("qkT", g, tb)], f"qk{i}")
        for tt in range(4):
            t = tb * 4 + tt
            tb2 = t % 2
            groups = [(0, 512, "qkv"), (512, 512, "qkv"), (1024, 512, "qkv"), (1536, 512, "z"),
                      (2048, 16, "ab"), (3088, 512, "v")]
            for (col0, n, kind) in groups:
                p = pi % 8
                pi += 1
                for c in range(8):
                    P.mm(ps[p][:, 0:n], xTb[b][:, c, tt * 128:(tt + 1) * 128], W[:, c, col0:col0 + n],
                         c == 0, c == 7, ["W", f"xTb{b}"], [f"ps{p}"])
                eng = ACT if ev % 2 else DVE
                ev += 1
                if kind == "qkv":
                    P.copy(eng, qkv_sb[tb2][:, col0:col0 + 512], ps[p][:, 0:512], [f"ps{p}"], [f"qkv{tb2}_{col0}"])
                elif kind == "z":
                    P.copy(eng, z_sb[tb2][:], ps[p][:, 0:512], [f"ps{p}"], [f"z{tb2}"])
                elif kind == "ab":
                    P.copy(eng, C.ab_sb[:, t, :], ps[p][:, 0:16], [f"ps{p}"], [("ab", t)])
                else:
                    P.copy(eng, v_sb[tb2][:], ps[p][:, 0:512], [f"ps{p}"], [f"v{tb2}"])
            P.dma(POOL, C.qkvpre[3 + t * 128:3 + (t + 1) * 128, :], qkv_sb[tb2][:],
                  [f"qkv{tb2}_0", f"qkv{tb2}_512", f"qkv{tb2}_1024"], [("qkvpre", t)], f"qkv{tb2}")
            P.dma(POOL, C.zbuf[t * 128:(t + 1) * 128, :], z_sb[tb2][:], [f"z{tb2}"], [("zbuf", t)], f"z{tb2}")
            P.dma(POOL, C.Vdf[t * 128:(t + 1) * 128, :], v_sb[tb2][:], [f"v{tb2}"], [("Vdf", t)], f"v{tb2}")
    return P.emit()


def phase_attn(C, layer):
    nc = C.nc
    P = Phase(nc, f"at{layer}")
    lambda_init = 0.8 - 0.6 * math.exp(-0.3 * layer)
    identf = P.sb("identf", [128, 128], F32)
    onesf = P.sb("onesf", [128, 128], F32)
    maskneg = P.sb("maskneg", [128, 128], F32)
    cfar = P.sb("cfar", [128, 4], F32)
    bt = P.sb("bt", [128, 8, 128], F32)
    badd = P.sb("badd", [128, 8, 128], F32)
    dl = P.sb("dl", [128, 256], F32)
    dlp = P.sb("dlp", [128, 128], F32)
    ls = P.sb("ls", [128, 2], F32)
    le = P.sb("le", [128, 2], F32)
    neglam = P.sb("neglam", [128, 1], F32)
    wcol = P.sb("wcol", [128, 1], F32)
    P.dma(SP, identf[:], C.ident_f32, [], ["identf"], "c0")
    P.dma(SP, onesf[:], C.ones_f32, [], ["onesf"], "c1")
    P.dma(SP, maskneg[:], C.maskneg, [], ["maskneg"], "c2")
    P.dma(SP, cfar[:], C.cfar, [], ["cfar"], "c3")
    P.dma(SP, bt[:], C.bias_tiles.rearrange("h r k q -> k (h r) q"), [], ["bt"], "c4")
    P.dma(SP, dl[:], C.df_lambda_bc[layer], [], ["dl"], "c5")
    P.dma(SP, wcol[:], C.df_subln_col[layer], [], ["wcol"], "c6")
    for h in range(4):
        for rel in range(2):
            i = h * 2 + rel
            P.ts(DVE, badd[:, i, :], bt[:, i, :], cfar[:, h:h + 1], 8.0, ALU.subtract, ALU.mult,
                 ["bt", "cfar"], [("badd", i)])
            if rel == 0:
                P.tt(DVE, badd[:, i, :], badd[:, i, :], maskneg[:], ALU.add, [("badd", i), "maskneg"], [("badd", i)])
    P.tt(DVE, dlp[:, 0:64], dl[:, 0:64], dl[:, 64:128], ALU.mult, ["dl"], ["dlp0"])
    P.tt(DVE, dlp[:, 64:128], dl[:, 128:192], dl[:, 192:256], ALU.mult, ["dl"], ["dlp1"])
    P.op(DVE, lambda e: e.tensor_reduce(ls[:], dlp[:].rearrange("p (a b) -> p a b", a=2), AX.X, ALU.add),
         ["dlp0", "dlp1"], ["ls"])
    P.act(le[:], ls[:], AF.Exp, ["ls"], ["le"])
    P.tt(DVE, neglam[:], le[:, 1:2], le[:, 0:1], ALU.subtract, ["le"], ["neglam"])
    P.ts(DVE, neglam[:], neglam[:], -lambda_init, None, ALU.add, ALU.bypass, ["neglam"], ["neglam"])
    P.ts(DVE, wcol[:], wcol[:], 1.0 - lambda_init, None, ALU.mult, ALU.bypass, ["wcol"], ["wcol"])

    KTh = [P.sb(f"KTh{i}", [128, S], BF16) for i in range(2)]
    QTh = [P.sb(f"QTh{i}", [128, S], BF16) for i in range(2)]
    Vh = [P.sb(f"Vh{i}", [128, NT, 128], BF16) for i in range(2)]
    pT = [P.sb(f"pT{i}", [128, 512], BF16) for i in range(4)]
    PSa = [P.sb(f"PSa{i}", [128, 512], F32) for i in range(2)]
    rden = [P.sb(f"rden{i}", [128, 512], F32) for i in range(2)]
    on = [P.sb(f"on{i}", [128, 512], F32) for i in range(2)]
    o_sb = P.sb("o_sb", [128, 512], F32)
    sq = P.sb("sq", [128, 512], F32)
    rstd = P.sb("rstd", [128, 512], F32)
    of = [P.sb(f"of{i}", [128, 512], BF16) for i in range(2)]
    accO = [P.ps(f"accO{i}", [128, 512], F32) for i in range(2)]
    sc = [P.ps(f"sc{i}", [128, 512], F32) for i in range(4)]
    aux = [P.ps(f"aux{i}", [128, 512], F32) for i in range(2)]
    P.excl.update(["accO0", "accO1", "sc0", "sc1", "sc2", "sc3", "aux0", "aux1"])
    V3 = C.Vdf.rearrange("(t p) c -> p t c", p=128)

    def load_head(h):
        b = h % 2
        P.dma(SP, KTh[b][:], C.KT[h], [], [f"KTh{b}"], f"KTh{b}")
        P.dma(SP, QTh[b][:], C.QT[h], [], [f"QTh{b}"], f"QTh{b}")
        for i in range(8):
            P.dma(SP, Vh[b][:, i * 8:(i + 1) * 8, :], V3[:, i * 8:(i + 1) * 8, h * 128:(h + 1) * 128], [],
                  [(f"Vh{b}", i)], f"Vh{b}_{i}")

    load_head(0)
    LA = 3
    nq = 0
    gstep = 0
    for h in range(4):
        b = h % 2
        if h + 1 < 4:
            load_head(h + 1)
        steps = []
        for Qb in range(S // 512):
            nk = 4 * Qb + 4
            for kt in range(nk):
                for m in range(2):
                    steps.append((Qb, kt, m, nk))
        ns = len(steps)

        def front(si, gs):
            Qb, kt, m, nk = steps[si]
            j = kt - 4 * Qb
            c0 = max(0, j) * 128
            sI = gs % 4
            pi = gs % 4
            adds = []
            if j >= 0:
                adds.append((j * 128, h * 2 + 0))
            if j >= -1 and j + 1 <= 3:
                adds.append(((j + 1) * 128, h * 2 + 1))
            P.mm(sc[sI][:, c0:512], KTh[b][m * 64:(m + 1) * 64, kt * 128:(kt + 1) * 128],
                 QTh[b][m * 64:(m + 1) * 64, Qb * 512 + c0:Qb * 512 + 512], True, len(adds) == 0,
                 [f"KTh{b}", f"QTh{b}"], [f"sc{sI}"])
            for ai, (cc, bi) in enumerate(adds):
                P.mm(sc[sI][:, cc:cc + 128], identf[:], badd[:, bi, :], False, ai == len(adds) - 1,
                     ["identf", ("badd", bi)], [f"sc{sI}"])
            P.act(pT[pi][:, c0:512], sc[sI][:, c0:512], AF.Exp, [f"sc{sI}"], [f"pT{pi}"], scale=0.125)

        def back(si, gs):
            nonlocal nq
            Qb, kt, m, nk = steps[si]
            j = kt - 4 * Qb
            c0 = max(0, j) * 128
            pi = gs % 4
            P.mm(accO[m][:, c0:512], Vh[b][:, kt, :], pT[pi][:, c0:512], kt == 0, kt == nk - 1,
                 [(f"Vh{b}", kt // 8), f"pT{pi}"], [f"accO{m}"])
            eng = DVE if m == 0 else POOL
            if kt == 0:
                P.copy(eng, PSa[m][:], pT[pi][:], [f"pT{pi}"], [f"PSa{m}"])
            else:
                P.tt(eng, PSa[m][:, c0:512], PSa[m][:, c0:512], pT[pi][:, c0:512], ALU.add,
                     [f"PSa{m}", f"pT{pi}"], [f"PSa{m}"])
            if not (kt == nk - 1 and m == 1):
                return
            for mm_ in range(2):
                P.mm(aux[mm_][:], onesf[:], PSa[mm_][:], True, True, ["onesf", f"PSa{mm_}"], [f"aux{mm_}"])
                P.op(DVE, lambda e, mm_=mm_: e.reciprocal(rden[mm_][:], aux[mm_][:]), [f"aux{mm_}"], [f"rden{mm_}"])
                P.tt(DVE, on[mm_][:], accO[mm_][:], rden[mm_][:], ALU.mult, [f"accO{mm_}", f"rden{mm_}"], [f"on{mm_}"])
            P.stt(o_sb[:], on[1][:], neglam[:, 0:1], on[0][:], ALU.mult, ALU.add, ["on0", "on1", "neglam"], ["o_sb"])
            P.tt(POOL, sq[:], o_sb[:], o_sb[:], ALU.mult, ["o_sb"], ["sq"])
            P.mm(aux[0][:], onesf[:], sq[:], True, True, ["onesf", "sq"], ["aux0"])
            P.ts(DVE, rstd[:], aux[0][:], 1.0 / 128.0, 1e-6, ALU.mult, ALU.add, ["aux0"], ["rstd"])
            P.act(rstd[:], rstd[:], AF.Ln, ["rstd"], ["rstd"])
            P.act(rstd[:], rstd[:], AF.Exp, ["rstd"], ["rstd"], scale=-0.5)
            P.tt(DVE, o_sb[:], o_sb[:], rstd[:], ALU.mult, ["o_sb", "rstd"], ["o_sb"])
            ob = nq % 2
            nq += 1
            P.ts(DVE, of[ob][:], o_sb[:], wcol[:, 0:1], None, ALU.mult, ALU.bypass, ["o_sb", "wcol"], [f"of{ob}"])
            P.dma(POOL, C.catT[512 + h * 128:512 + (h + 1) * 128, Qb * 512:(Qb + 1) * 512], of[ob][:],
                  [f"of{ob}"], [("catT_df", h, Qb)], f"of{ob}")

        for idx in range(ns + LA):
            if idx < ns:
                front(idx, gstep + idx)
            if idx - LA >= 0:
                back(idx - LA, gstep + idx - LA)
        gstep += ns
    return P.emit()


def ln_epilogue(P, C, t, src_halves, src_keys, res_src, gb, ident, bufs, pt, pt_key, out_dst=None, router=None):
    i = t % 2
    xr, y, xo, xb, xt = bufs["xr"][i], bufs["y"][i], bufs["xo"][i], bufs["xb"][i], bufs["xt"][i]
    kxr, ky, kxo, kxb, kxt = bufs["kxr"][i], bufs["ky"][i], bufs["kxo"][i], bufs["kxb"][i], bufs["kxt"][i]
    st, mv, rs, nmr = bufs["st"][i], bufs["mv"][i], bufs["rs"][i], bufs["nmr"][i]
    ks = f"lnsmall{i}"
    P.dma(SP, xr[:, 0:D], res_src[t * 128:(t + 1) * 128, :], [("xres", t)], [kxr], f"ln_{kxr}")
    for hf in range(2):
        P.stt(y[:, hf * 512:(hf + 1) * 512], xr[:, hf * 512:(hf + 1) * 512], ALPHA, src_halves[hf], ALU.mult, ALU.add,
              [kxr, src_keys[hf]], [ky])
        P.op(DVE, lambda e, hf=hf: e.bn_stats(st[:, hf, :], y[:, hf * 512:(hf + 1) * 512]), [ky], [ks + f"st{hf}"])
    P.op(DVE, lambda e: e.bn_aggr(mv[:], st[:].rearrange("p a b -> p (a b)")), [ks + "st0", ks + "st1"], [ks + "mv"])
    P.ts(DVE, rs[:], mv[:, 1:2], LN_EPS, None, ALU.add, ALU.bypass, [ks + "mv"], [ks + "rs"])
    P.act(rs[:], rs[:], AF.Ln, [ks + "rs"], [ks + "rs"])
    P.act(rs[:], rs[:], AF.Exp, [ks + "rs"], [ks + "rs"], scale=-0.5)
    P.ts(DVE, nmr[:], mv[:, 0:1], rs[:, 0:1], -1.0, ALU.mult, ALU.mult, [ks + "mv", ks + "rs"], [ks + "nmr"])
    P.ts(POOL, y[:, 0:D], y[:, 0:D], rs[:, 0:1], nmr[:, 0:1], ALU.mult, ALU.add, [ky, ks + "rs", ks + "nmr"],
         [ky])
    P.tt(POOL, y[:, 0:D], y[:, 0:D], gb[0][:], ALU.mult, [ky, "ln_g"], [ky])
    P.tt(DVE, xo[:, 0:D], y[:, 0:D], gb[1][:], ALU.add, [ky, "ln_b"], [kxo])
    if out_dst is not None:
        P.dma(POOL, out_dst[t * 128:(t + 1) * 128, :], xo[:, 0:D], [kxo], [("out", t)], f"ln_{kxo}")
        return
    P.dma(POOL, C.xres[t * 128:(t + 1) * 128, :], xo[:, 0:D], [kxo], [("xres", t)], f"ln_{kxo}")
    P.copy(ACT, xb[:, 0:D], xo[:, 0:D], [kxo], [kxb])
    for c in range(8):
        P.tr(pt[:, c, :], xb[:, c * 128:(c + 1) * 128], ident[:], [kxb, "ident"], [pt_key])
    P.copy(ACT, xt[:], pt, [pt_key], [kxt])
    xT3 = C.xT.rearrange("(c p) t -> p c t", p=128)
    P.dma(POOL, xT3[:, :, t * 128:(t + 1) * 128], xt[:], [kxt], [("xT", t)], f"ln_{kxt}")
    if router is not None:
        rbc, lg, junk = router
        for e in range(NE):
            P.op(DVE, lambda en, e=e: en.scalar_tensor_tensor(junk[:], xo[:, 0:D], 1.0, rbc[:, e, :], ALU.mult, ALU.mult,
                                                               accum_out=lg[i][:, e:e + 1]),
                 [kxo, "rbc"], [f"lg{i}_{e}", "junk"])
        lgk = [f"lg{i}_{e}" for e in range(NE)]
        sm = bufs["sm"][i]
        kk = f"sm{i}"
        P.op(DVE, lambda en: en.tensor_reduce(sm[:, 0:1], lg[i][:], AX.X, ALU.max), lgk, [kk + "m1"])
        P.ts(DVE, sm[:, 8:16], lg[i][:], sm[:, 0:1], None, ALU.is_equal, ALU.bypass, lgk + [kk + "m1"], [kk + "k1"])
        P.stt(sm[:, 24:32], sm[:, 8:16], -1e30, lg[i][:], ALU.mult, ALU.add, [kk + "k1"] + lgk, [kk + "l2"])
        P.op(DVE, lambda en: en.tensor_reduce(sm[:, 1:2], sm[:, 24:32], AX.X, ALU.max), [kk + "l2"], [kk + "m2"])
        P.ts(DVE, sm[:, 16:24], sm[:, 24:32], sm[:, 1:2], None, ALU.is_equal, ALU.bypass, [kk + "l2", kk + "m2"], [kk + "k2"])
        P.tt(DVE, sm[:, 2:3], sm[:, 1:2], sm[:, 0:1], ALU.subtract, [kk + "m1", kk + "m2"], [kk + "d"])
        P.act(sm[:, 2:3], sm[:, 2:3], AF.Exp, [kk + "d"], [kk + "d"])
        P.ts(DVE, sm[:, 3:4], sm[:, 2:3], 1.0, None, ALU.add, ALU.bypass, [kk + "d"], [kk + "g1"])
        P.op(DVE, lambda en: en.reciprocal(sm[:, 3:4], sm[:, 3:4]), [kk + "g1"], [kk + "g1"])
        P.tt(DVE, sm[:, 4:5], sm[:, 2:3], sm[:, 3:4], ALU.mult, [kk + "d", kk + "g1"], [kk + "g2"])
        P.ts(DVE, sm[:, 8:16], sm[:, 8:16], sm[:, 3:4], None, ALU.mult, ALU.bypass, [kk + "k1", kk + "g1"], [kk + "k1"])
        P.stt(C.gate_sb[:, t, :], sm[:, 16:24], sm[:, 4:5], sm[:, 8:16], ALU.mult, ALU.add,
              [kk + "k2", kk + "g2", kk + "k1"], [("gate", t)])


def ln_bufs(P):
    b = {}
    for nm, shape, dt in [("xr", [128, D], F32), ("y", [128, D], F32), ("xo", [128, D], F32), ("xb", [128, D], BF16),
                          ("xt", [128, 8, 128], BF16), ("st", [128, 2, 6], F32), ("mv", [128, 2], F32),
                          ("rs", [128, 1], F32), ("nmr", [128, 1], F32), ("sm", [128, 40], F32)]:
        b[nm] = [P.sb(f"ln_{nm}{i}", shape, dt) for i in range(2)]
        b["k" + nm] = [f"ln_{nm}{i}" for i in range(2)]
    return b


def phase_outproj(C, layer):
    nc = C.nc
    P = Phase(nc, f"op{layer}")
    moe = (layer % 2 == 1)
    ident = P.sb("ident", [128, 128], BF16)
    P.dma(SP, ident[:], C.ident_bf, [], ["ident"], "c0")
    gb = [P.sb("ln_g", [128, D], F32), P.sb("ln_b", [128, D], F32)]
    P.dma(SP, gb[0][:], C.ln1_g_bc[layer], [], ["ln_g"], "c1")
    P.dma(SP, gb[1][:], C.ln1_b_bc[layer], [], ["ln_b"], "c2")
    router = None
    if moe:
        rbc = P.sb("rbc", [128, NE, D], F32)
        P.dma(SP, rbc[:], C.router_bc[layer // 2], [], ["rbc"], "c3")
        lg = [P.sb(f"lg{i}", [128, NE], F32) for i in range(2)]
        junk = P.sb("junk", [128, D], F32)
        router = (rbc, lg, junk)
    Wo = P.sb("Wo", [128, 8, D], BF16)
    stg = [P.sb(f"stg{i}", [128, 1024], F32) for i in range(3)]
    load_cast_w(P, Wo, C.w_out[layer], 8, D, [], "Wo", stg, 1024, [0])
    bufs = ln_bufs(P)
    ct = [P.sb(f"ct{i}", [128, 8, 512], BF16) for i in range(2)]
    ps = [P.ps(f"ps{i}", [128, 512], F32) for i in range(4)]
    pt = [P.ps(f"pt{i}", [128, 8, 128], BF16) for i in range(2)]
    P.excl.update(["ps0", "ps1", "ps2", "ps3", "pt0", "pt1"])
    catT3 = C.catT.rearrange("(c p) t -> p c t", p=128)
    res_src = C.x if layer == 0 else C.xres
    for tb in range(S // 512):
        b = tb % 2
        P.dma(SP, ct[b][:], catT3[:, :, tb * 512:(tb + 1) * 512], [], [f"ct{b}"], f"ct{b}")
        for tt in range(4):
            t = tb * 4 + tt
            pp = (t % 2) * 2
            for hf in range(2):
                for c in range(8):
                    P.mm(ps[pp + hf][:], ct[b][:, c, tt * 128:(tt + 1) * 128], Wo[:, c, hf * 512:(hf + 1) * 512],
                         c == 0, c == 7, [f"ct{b}", "Wo"], [f"ps{pp + hf}"])
            ln_epilogue(P, C, t, [ps[pp][:], ps[pp + 1][:]], [f"ps{pp}", f"ps{pp + 1}"], res_src, gb, ident, bufs,
                        pt[t % 2][:], f"pt{t % 2}", router=router)
    return P.emit()


def phase_ffn(C, layer, last):
    nc = C.nc
    P = Phase(nc, f"ff{layer}")
    moe = (layer % 2 == 1)
    li = layer // 2
    if moe:
        E, F = NE, DFE
        wg_of = lambda e: C.moe_w_gate[li, e]
        wu_of = lambda e: C.moe_w_up[li, e]
        wd_of = lambda e: C.moe_w_down[li, e]
    else:
        E, F = 1, DFF
        wg_of = lambda e: C.ffn_w_gate[li]
        wu_of = lambda e: C.ffn_w_up[li]
        wd_of = lambda e: C.ffn_w_down[li]
    nfc = F // 128
    groups = [(f0, min(4, nfc - f0)) for f0 in range(0, nfc, 4)]
    SBT = 16
    ident = P.sb("ident", [128, 128], BF16)
    P.dma(SP, ident[:], C.ident_bf, [], ["ident"], "c0")
    gb = [P.sb("ln_g", [128, D], F32), P.sb("ln_b", [128, D], F32)]
    P.dma(SP, gb[0][:], C.ln2_g_bc[layer], [], ["ln_g"], "c1")
    P.dma(SP, gb[1][:], C.ln2_b_bc[layer], [], ["ln_b"], "c2")
    acc = P.sb("acc", [128, SBT, D], F32)
    xTs = P.sb("xTs", [128, 8, SBT * 128], BF16)
    Wg = [P.sb(f"Wg{i}", [128, 8, 512], BF16) for i in range(2)]
    Wu = [P.sb(f"Wu{i}", [128, 8, 512], BF16) for i in range(2)]
    Wd = [P.sb(f"Wd{i}", [128, 4, D], BF16) for i in range(2)]
    stg = [P.sb(f"stg{i}", [128, 2048], F32) for i in range(3)]
    hT = [P.sb(f"hT{i}", [128, 4, 512], BF16) for i in range(2)]
    sg = [P.sb(f"sg{i}", [128, 512], F32) for i in range(2)]
    bufs = {}
    for nm, shape, dt in [("xb", [128, D], BF16), ("xt", [128, 8, 128], BF16), ("st", [128, 2, 6], F32),
                          ("mv", [128, 2], F32), ("rs", [128, 1], F32), ("nmr", [128, 1], F32), ("sm", [128, 40], F32)]:
        bufs[nm] = [P.sb(f"ln_{nm}{i}", shape, dt) for i in range(2)]
        bufs["k" + nm] = [f"ln_{nm}{i}" for i in range(2)]
    bufs["xr"] = [stg[0], stg[0]]
    bufs["kxr"] = ["stg0", "stg0"]
    bufs["y"] = [stg[1], stg[1]]
    bufs["ky"] = ["stg1", "stg1"]
    bufs["xo"] = [stg[2], stg[2]]
    bufs["kxo"] = ["stg2", "stg2"]
    pg = [P.ps(f"pg{i}", [128, 512], F32) for i in range(2)]
    pu = [P.ps(f"pu{i}", [128, 512], F32) for i in range(2)]
    py = [P.ps(f"py{i}", [128, 512], F32) for i in range(4)]
    P.excl.update(["pg0", "pg1", "pu0", "pu1", "py0", "py1", "py2", "py3"])
    xT3 = C.xT.rearrange("(c p) t -> p c t", p=128)
    cnt = [0]
    ngu = 0
    nh = 0
    ny = 0
    wi = 0
    import os as _os
    dbg_sb = int(_os.environ.get("FF_SB", NT // SBT))
    dbg_ng = int(_os.environ.get("FF_NG", 10 ** 6))
    dbg_noln = bool(int(_os.environ.get("FF_NOLN", "0")))
    for sbk in range(min(NT // SBT, dbg_sb)):
        t0 = sbk * SBT
        for q4 in range(4):
            P.dma(SP, xTs[:, :, q4 * 512:(q4 + 1) * 512], xT3[:, :, t0 * 128 + q4 * 512:t0 * 128 + (q4 + 1) * 512],
                  [("xT", t0 + q4 * 4 + i) for i in range(4)], [("xTs", q4)], f"xTs{q4}")
        first = [True] * SBT
        for e in range(E):
            for (f0, nf) in groups[:dbg_ng]:
                wb = wi % 2
                wi += 1
                for c in range(8):
                    for (dst, src, key) in ((Wg[wb], wg_of(e), f"Wg{wb}"), (Wu[wb], wu_of(e), f"Wu{wb}")):
                        i = cnt[0] % 3
                        cnt[0] += 1
                        P.dma(SP, stg[i][:, 0:nf * 128], src[c * 128:(c + 1) * 128, f0 * 128:(f0 + nf) * 128], [],
                              [f"stg{i}"], f"stg{i}")
                        P.copy(POOL, dst[:, c, 0:nf * 128], stg[i][:, 0:nf * 128], [f"stg{i}"], [key])
                for fc in range(0, nf, 2):
                    n2 = min(2, nf - fc)
                    i = cnt[0] % 3
                    cnt[0] += 1
                    src = wd_of(e)[(f0 + fc) * 128:(f0 + fc + n2) * 128, :].rearrange("(a p) d -> p a d", p=128)
                    P.dma(SP, stg[i][:, 0:n2 * D].rearrange("p (a d) -> p a d", a=n2), src, [], [f"stg{i}"], f"stg{i}")
                    P.copy(POOL, Wd[wb][:, fc:fc + n2, :], stg[i][:, 0:n2 * D].rearrange("p (a d) -> p a d", a=n2),
                           [f"stg{i}"], [f"Wd{wb}"])
                for q4 in range(SBT // 4):
                    hb = nh % 2
                    nh += 1
                    for fc in range(nf):
                        gi = ngu % 2
                        ngu += 1
                        for c in range(8):
                            P.mm(pg[gi][:], Wg[wb][:, c, fc * 128:(fc + 1) * 128], xTs[:, c, q4 * 512:(q4 + 1) * 512],
                                 c == 0, c == 7, [f"Wg{wb}", ("xTs", q4)], [f"pg{gi}"])
                        for c in range(8):
                            P.mm(pu[gi][:], Wu[wb][:, c, fc * 128:(fc + 1) * 128], xTs[:, c, q4 * 512:(q4 + 1) * 512],
                                 c == 0, c == 7, [f"Wu{wb}", ("xTs", q4)], [f"pu{gi}"])
                        P.act(sg[gi][:], pg[gi][:], AF.Silu, [f"pg{gi}"], [f"sg{gi}"])
                        P.tt(DVE, hT[hb][:, fc, :], sg[gi][:], pu[gi][:], ALU.mult, [f"sg{gi}", f"pu{gi}"], [(f"hT{hb}", fc)])
                    for tt in range(4):
                        tl = q4 * 4 + tt
                        yb = (ny % 2) * 2
                        ny += 1
                        for hf in range(2):
                            for fc in range(nf):
                                P.mm(py[yb + hf][:], hT[hb][:, fc, tt * 128:(tt + 1) * 128], Wd[wb][:, fc, hf * 512:(hf + 1) * 512],
                                     fc == 0, fc == nf - 1, [(f"hT{hb}", fc), f"Wd{wb}"], [f"py{yb + hf}"])
                            dst = acc[:, tl, hf * 512:(hf + 1) * 512]
                            akey = ("acc", tl, hf)
                            if moe:
                                gsc = C.gate_sb[:, t0 + tl, e:e + 1]
                                if first[tl]:
                                    P.ts(DVE, dst, py[yb + hf][:], gsc, None, ALU.mult, ALU.bypass, [f"py{yb + hf}", ("gate", t0 + tl)], [akey])
                                else:
                                    P.stt(dst, py[yb + hf][:], gsc, dst, ALU.mult, ALU.add, [f"py{yb + hf}", akey, ("gate", t0 + tl)], [akey])
                            else:
                                if first[tl]:
                                    P.copy(DVE, dst, py[yb + hf][:], [f"py{yb + hf}"], [akey])
                                else:
                                    P.tt(DVE, dst, py[yb + hf][:], dst, ALU.add, [f"py{yb + hf}", akey], [akey])
                        first[tl] = False
        for tl in range(0 if dbg_noln else SBT):
            t = t0 + tl
            ptv = py[tl % 4][:].bitcast(BF16).rearrange("p (c t) -> p c t", c=8)
            ln_epilogue(P, C, t, [acc[:, tl, 0:512], acc[:, tl, 512:1024]], [("acc", tl, 0), ("acc", tl, 1)], C.xres, gb, ident,
                        bufs, ptv, f"py{tl % 4}", out_dst=(C.out if last else None))
    return P.emit()


def bc(ap, shape):
    return ap.to_broadcast(list(shape))


def phase_dn(C, layer):
    nc = C.nc
    P = Phase(nc, f"dn{layer}")
    identb = P.sb("identb", [128, 128], BF16)
    identf = P.sb("identf", [128, 128], F32)
    onesf = P.sb("onesf", [128, 128], F32)
    trif = P.sb("trif", [128, 128], F32)
    mcaus = P.sb("mcaus", [128, 128], F32)
    mneg = P.sb("mneg", [128, 128], F32)
    cw = P.sb("cw", [128, 4, 1536], F32)
    alog = P.sb("alog", [128, 8], F32)
    dtb = P.sb("dtb", [128, 8], F32)
    wn = P.sb("wn", [128, 64], F32)
    for i, (dst, src, key) in enumerate([(identb, C.ident_bf, "identb"), (identf, C.ident_f32, "identf"), (onesf, C.ones_f32, "onesf"),
                                         (trif, C.tri_f32, "trif"), (mcaus, C.mcausT, "mcaus"), (mneg, C.mnegT, "mneg"),
                                         (alog, C.a_log_bc[layer], "alog"), (dtb, C.dt_bias_bc[layer], "dtb"),
                                         (wn, C.dn_norm_bc[layer], "wn")]):
        P.dma(SP, dst[:], src, [], [key], f"c{i}")
    P.dma(SP, cw[:], C.conv_w_bc[layer].rearrange("p (j c) -> p j c", j=4), [], ["cw"], "c_cw")

    def g8(name):
        return P.sb(name, [128, NT, 8], F32)
    x8, gg8, beta8, gc8, glb8, eg8 = [g8(n) for n in ("x8", "gg8", "beta8", "gc8", "glb8", "eg8")]
    kd8, negeg8, egl8 = x8, gg8, glb8
    P.alias = {"kd8": "x8", "negeg8": "gg8", "egl8": "glb8"}
    eglS = P.sb("eglS", [128, NT, 4], F32)
    negA = P.sb("negA", [128, 8], F32)
    pp = [P.ps(f"pp{i}", [128, 512], F32) for i in range(4)]
    rA, rB, rC, rD = [P.ps(f"r{n}", [128, 512], F32) for n in "ABCD"]
    P.excl.update(["pp0", "pp1", "pp2", "pp3", "rK0", "rK1", "rC", "rD"])
    P.act(negA[:], alog[:], AF.Exp, ["alog"], ["negA"])
    P.ts(DVE, negA[:], negA[:], -1.0, None, ALU.mult, ALU.bypass, ["negA"], ["negA"])
    for h in range(8):
        P.ts(DVE, x8[:, :, h], C.ab_sb[:, :, h], dtb[:, h:h + 1], None, ALU.add, ALU.bypass, ["dtb"], ["x8"])
    P.act(x8[:], x8[:], AF.Exp, ["x8"], ["x8"])
    P.act(x8[:], x8[:], AF.Ln, ["x8"], ["x8"], bias=1.0)
    for h in range(8):
        P.ts(DVE, gg8[:, :, h], x8[:, :, h], negA[:, h:h + 1], None, ALU.mult, ALU.bypass, ["x8", "negA"], ["gg8"])
    P.act(beta8[:], C.ab_sb[:, :, 8:16], AF.Exp, [], ["beta8"], scale=-1.0)
    P.ts(DVE, beta8[:], beta8[:], 1.0, None, ALU.add, ALU.bypass, ["beta8"], ["beta8"])
    P.op(DVE, lambda e: e.reciprocal(beta8[:], beta8[:]), ["beta8"], ["beta8"])
    ggf = gg8[:].rearrange("p t h -> p (t h)")
    P.mm(pp[0][:], trif[:], ggf, True, True, ["trif", "gg8"], ["pp0"])
    P.mm(pp[1][:], onesf[:], ggf, True, True, ["onesf", "gg8"], ["pp1"])
    P.copy(DVE, gc8[:].rearrange("p t h -> p (t h)"), pp[0][:], ["pp0"], ["gc8"])
    P.copy(DVE, glb8[:].rearrange("p t h -> p (t h)"), pp[1][:], ["pp1"], ["glb8"])
    P.act(eg8[:], gc8[:], AF.Exp, ["gc8"], ["eg8"])
    P.ts(DVE, negeg8[:], eg8[:], -1.0, None, ALU.mult, ALU.bypass, ["eg8"], ["negeg8"])
    P.tt(DVE, kd8[:], glb8[:], gc8[:], ALU.subtract, ["glb8", "gc8"], ["kd8"])
    P.act(kd8[:], kd8[:], AF.Exp, ["kd8"], ["kd8"])
    P.act(egl8[:], glb8[:], AF.Exp, ["glb8"], ["egl8"])
    for par in range(2):
        P.copy(DVE, eglS[par * 64:(par + 1) * 64, :, :], egl8[par * 64:(par + 1) * 64, :, par::2], ["egl8"], ["eglS"])

    cv = [P.sb(f"cv{i}", [128, 4, 1536], F32) for i in range(2)]
    act = [P.sb(f"act{i}", [128, 1536], F32) for i in range(2)]
    ss = [P.sb(f"ss{i}", [128, 16], F32) for i in range(2)]
    qkb = [P.sb(f"qkb{i}", [128, 1024], BF16) for i in range(2)]
    dg = [P.sb(f"dg{i}", [128, 4, 128], F32) for i in range(2)]
    dsb = [P.sb(f"dsb{i}", [128, 4, 128], F32) for i in range(2)]
    DTs = [P.sb(f"DTs{i}", [128, 4, 128], F32) for i in range(2)]
    DTc = [P.sb(f"DTc{i}", [128, 4, 128], F32) for i in range(2)]
    Mb = [[P.sb(f"M{i}_{k}", [128, 8, 128], BF16) for k in range(2)] for i in range(2)]
    MTb = [[P.sb(f"MT{i}_{k}", [128, 8, 128], BF16) for k in range(2)] for i in range(2)]
    PTf = [P.sb(f"PTf{i}", [128, 8, 128], F32) for i in range(2)]
    PTw = [P.sb(f"PTw{i}", [128, 8, 128], BF16) for i in range(2)]
    qkT = [P.sb(f"qkT{i}", [128, 8, 128], BF16) for i in range(3)]
    PTb = [P.sb(f"PTb{i}", [128, 8, 128], BF16) for i in range(3)]
    inT = [P.sb(f"inT{i}", [128, 8, 128], BF16) for i in range(3)]
    v_f = [P.sb(f"v_f{i}", [128, 512], F32) for i in range(3)]
    kdec = [P.sb(f"kdec{i}", [128, 512], BF16) for i in range(3)]
    zt = [P.sb(f"zt{i}", [128, 512], F32) for i in range(3)]
    o_f = [P.sb(f"o_f{i}", [128, 512], F32) for i in range(3)]
    Sf = P.sb("Sf", [128, 4, 64], F32)
    Sb = P.sb("Sb", [128, 4, 64], BF16)
    tmp = P.sb("tmp", [128, 8, 64], F32)
    rp = P.sb("rp", [128, 8, 64], BF16)
    vn = P.sb("vn", [128, 8, 64], BF16)
    o2 = [P.sb(f"o2{i}", [128, 512], F32) for i in range(2)]
    ssn = [P.sb(f"ssn{i}", [128, 8], F32) for i in range(2)]
    o_bf = [P.sb(f"o_bf{i}", [128, 512], BF16) for i in range(2)]
    oT = [P.sb(f"oT{i}", [128, 4, 128], BF16) for i in range(2)]
    P.op(DVE, lambda e: e.memset(Sf[:], 0.0), [], ["Sf"])
    P.op(DVE, lambda e: e.memset(Sb[:], 0.0), [], ["Sb"])
    pp_free = [0, 1, 2, 3]

    def acquire(n):
        while len(pp_free) < n:
            yield
        return [pp_free.pop(0) for _ in range(n)]

    def release(*idx):
        pp_free.extend(idx)

    def hp(h):
        return (h % 2) * 64

    def prep(t):
        pb = t % 2
        hb = t % 3
        K = lambda s: f"{s}{pb}"
        H = lambda s: f"{s}{hb}"
        src = bass.AP(tensor=C.qkvpre.tensor, offset=t * 128 * 1536, ap=[[1536, 128], [1536, 4], [1, 1536]])
        P.dma(SP, cv[pb][:], src, [("qkvpre", t)], [K("cv")], K("cv"))
        yield
        P.tt(POOL, cv[pb][:], cv[pb][:], cw[:], ALU.mult, [K("cv"), "cw"], [K("cv")])
        yield
        P.tt(DVE, cv[pb][:, 0:2, :], cv[pb][:, 0:2, :], cv[pb][:, 2:4, :], ALU.add, [K("cv")], [K("cv")])
        P.tt(DVE, act[pb][:], cv[pb][:, 0, :], cv[pb][:, 1, :], ALU.add, [K("cv")], [K("act")])
        yield
        P.act(act[pb][:], act[pb][:], AF.Silu, [K("act")], [K("act")])
        yield
        sqv = cv[pb][:, 2, 0:1024]
        P.tt(POOL, sqv, act[pb][:, 0:1024], act[pb][:, 0:1024], ALU.mult, [K("act")], [K("cv")])
        P.copy(POOL, v_f[hb][:], act[pb][:, 1024:1536], [K("act")], [H("v_f")])
        yield
        P.op(DVE, lambda e: e.tensor_reduce(ss[pb][:], sqv.rearrange("p (h d) -> p h d", h=16), AX.X, ALU.add),
             [K("cv")], [K("ss")])
        P.ts(DVE, ss[pb][:], ss[pb][:], 1e-6, None, ALU.add, ALU.bypass, [K("ss")], [K("ss")])
        yield
        P.act(ss[pb][:], ss[pb][:], AF.Ln, [K("ss")], [K("ss")])
        P.act(ss[pb][:], ss[pb][:], AF.Exp, [K("ss")], [K("ss")], scale=-0.5)
        yield
        P.ts(DVE, ss[pb][:, 0:8], ss[pb][:, 0:8], 0.125, None, ALU.mult, ALU.bypass, [K("ss")], [K("ss")])
        P.tt(DVE, qkb[pb][:].rearrange("p (h d) -> p h d", h=16), act[pb][:, 0:1024].rearrange("p (h d) -> p h d", h=16),
             bc(ss[pb][:].unsqueeze(2), [128, 16, 64]), ALU.mult, [K("act"), K("ss")], [K("qkb")])
        yield
        P.tt(POOL, kdec[hb][:].rearrange("p (h d) -> p h d", h=8), qkb[pb][:, 512:1024].rearrange("p (h d) -> p h d", h=8),
             bc(kd8[:, t, :].unsqueeze(2), [128, 8, 64]), ALU.mult, [K("qkb"), "kd8"], [H("kdec")])
        (pi,) = yield from acquire(1)
        ptv = pp[pi][:].bitcast(BF16).rearrange("p (a t) -> p a t", a=8)
        for a in range(8):
            P.tr(ptv[:, a, :], qkb[pb][:, a * 128:(a + 1) * 128], identb[:], [K("qkb"), "identb"], [f"pp{pi}"])
        yield
        P.copy(ACT, qkT[hb][:], ptv, [f"pp{pi}"], [H("qkT")])
        release(pi)
        yield
        for hg in range(2):
            iG, iQ, iR = yield from acquire(3)
            P.tt(POOL, dg[pb][:], bc(identf[:].unsqueeze(1), [128, 4, 128]),
                 bc(gc8[:, t, hg::2].unsqueeze(2), [128, 4, 128]), ALU.mult, ["identf", "gc8"], [K("dg")])
            for h4 in range(4):
                h = 2 * h4 + hg
                kT_h = qkT[hb][hp(h):hp(h) + 64, 4 + h // 2, :]
                qT_h = qkT[hb][hp(h):hp(h) + 64, h // 2, :]
                P.mm(pp[iG][:, h4 * 128:(h4 + 1) * 128], kT_h, kT_h, True, True, [H("qkT")], [f"pp{iG}"])
                P.mm(pp[iQ][:, h4 * 128:(h4 + 1) * 128], kT_h, qT_h, True, True, [H("qkT")], [f"pp{iQ}"])
            P.mm(pp[iR][:], onesf[:], dg[pb][:].rearrange("p h i -> p (h i)"), True, True,
                 ["onesf", K("dg")], [f"pp{iR}"])
            yield
            for h4 in range(4):
                h = 2 * h4 + hg
                P.ts(DVE, dsb[pb][:, h4, :], pp[iR][:, h4 * 128:(h4 + 1) * 128], gc8[:, t, h:h + 1], 0.0,
                     ALU.subtract, ALU.min, [f"pp{iR}", "gc8"], [K("dsb")])
            release(iR)
            yield
            hs = slice(hg * 4, (hg + 1) * 4)
            P.act(dsb[pb][:], dsb[pb][:], AF.Exp, [K("dsb")], [K("dsb")])
            yield
            P.tt(POOL, DTs[pb][:], dsb[pb][:], bc(mneg[:].unsqueeze(1), [128, 4, 128]), ALU.mult,
                 [K("dsb"), "mneg"], [K("DTs")])
            P.tt(POOL, DTc[pb][:], dsb[pb][:], bc(mcaus[:].unsqueeze(1), [128, 4, 128]), ALU.mult,
                 [K("dsb"), "mcaus"], [K("DTc")])
            yield
            for h4 in range(4):
                h = 2 * h4 + hg
                P.stt(MTb[pb][0][:, hg * 4 + h4, :], pp[iG][:, h4 * 128:(h4 + 1) * 128], beta8[:, t, h:h + 1],
                      DTs[pb][:, h4, :], ALU.mult, ALU.mult, [f"pp{iG}", "beta8", K("DTs")],
                      [K("MT") + f"0_{hg}"])
            P.tt(DVE, inT[hb][:, hs, :], pp[iQ][:].rearrange("p (h i) -> p h i", h=4), DTc[pb][:], ALU.mult,
                 [f"pp{iQ}", K("DTc")], [H("inT") + f"_{hg}"])
            release(iG, iQ)
            yield
        (pi,) = yield from acquire(1)
        ptv = pp[pi][:].bitcast(BF16).rearrange("p (a t) -> p a t", a=8)
        for h in range(8):
            P.tr(ptv[:, h, :], MTb[pb][0][:, h, :], identb[:], [K("MT") + f"0_{h // 4}", "identb"], [f"pp{pi}"])
        P.tt(POOL, PTf[pb][:], MTb[pb][0][:], bc(identf[:].unsqueeze(1), [128, 8, 128]), ALU.add,
             [K("MT") + "0_0", K("MT") + "0_1", "identf"], [K("PTf") + "_0", K("PTf") + "_1"])
        P.tt(POOL, PTw[pb][:], MTb[pb][0][:], bc(identf[:].unsqueeze(1), [128, 8, 128]), ALU.add,
             [K("MT") + "0_0", K("MT") + "0_1", "identf"], [K("PTw") + "_0", K("PTw") + "_1"])
        yield
        P.copy(ACT, Mb[pb][0][:, 0:4, :], ptv[:, 0:4, :], [f"pp{pi}"], [K("M") + "0_0"])
        P.copy(DVE, Mb[pb][0][:, 4:8, :], ptv[:, 4:8, :], [f"pp{pi}"], [K("M") + "0_1"])
        release(pi)
        yield
        for s in range(1, 7):
            cur, prv = s % 2, (s - 1) % 2
            for hg in range(2):
                hs = slice(hg * 4, (hg + 1) * 4)
                kM, kMT = K("M") + f"{cur}_{hg}", K("MT") + f"{cur}_{hg}"
                kMp, kMTp = K("M") + f"{prv}_{hg}", K("MT") + f"{prv}_{hg}"
                if s <= 5:
                    iM, iT = yield from acquire(2)
                else:
                    (iM,) = yield from acquire(1)
                for h4 in range(4):
                    h = hg * 4 + h4
                    P.mm(pp[iM][:, h4 * 128:(h4 + 1) * 128], MTb[pb][prv][:, h, :], Mb[pb][prv][:, h, :], True, True,
                         [kMp, kMTp], [f"pp{iM}"])
                if s <= 5:
                    for h4 in range(4):
                        h = hg * 4 + h4
                        P.mm(pp[iT][:, h4 * 128:(h4 + 1) * 128], Mb[pb][prv][:, h, :], MTb[pb][prv][:, h, :], True, True,
                             [kMp, kMTp], [f"pp{iT}"])
                yield
                P.copy(ACT, Mb[pb][cur][:, hs, :], pp[iM][:].rearrange("p (h i) -> p h i", h=4), [f"pp{iM}"], [kM])
                if s <= 5:
                    P.copy(DVE, MTb[pb][cur][:, hs, :], pp[iT][:].rearrange("p (h i) -> p h i", h=4), [f"pp{iT}"], [kMT])
                    release(iT)
                release(iM)
                yield
                (iA,) = yield from acquire(1)
                for h4 in range(4):
                    h = hg * 4 + h4
                    P.mm(pp[iA][:, h4 * 128:(h4 + 1) * 128], Mb[pb][cur][:, h, :], PTw[pb][:, h, :], True, True,
                         [kM, K("PTw") + f"_{hg}"], [f"pp{iA}"])
                yield
                P.tt(DVE, PTf[pb][:, hs, :], PTf[pb][:, hs, :], pp[iA][:].rearrange("p (h i) -> p h i", h=4), ALU.add,
                     [K("PTf") + f"_{hg}", f"pp{iA}"], [K("PTf") + f"_{hg}"])
                release(iA)
                yield
                if s < 6:
                    P.copy(ACT, PTw[pb][:, hs, :], PTf[pb][:, hs, :], [K("PTf") + f"_{hg}"], [K("PTw") + f"_{hg}"])
                else:
                    P.copy(ACT, PTb[hb][:, hs, :], PTf[pb][:, hs, :], [K("PTf") + f"_{hg}"], [H("PTb") + f"_{hg}"])
                yield

    def hi(h):
        return (h % 2) * 4 + h // 2

    def recur(t):
        hb = t % 3
        H = lambda s: f"{s}{hb}"
        rK = [rA, rB]
        rCv = rC[:].rearrange("p (h d) -> p h d", h=8)
        rDv = rD[:, 0:256].rearrange("p (a d) -> p a d", a=4)
        tmp4 = tmp[:].rearrange("p (a q) d -> p a q d", q=2)
        for h in range(8):
            par, a = h % 2, h // 2
            kT_h = qkT[hb][hp(h):hp(h) + 64, 4 + a, :]
            qT_h = qkT[hb][hp(h):hp(h) + 64, a, :]
            S_h = Sb[hp(h):hp(h) + 64, a, :]
            P.mm(rK[par][:, a * 64:(a + 1) * 64], kT_h, S_h, True, True, [H("qkT"), "Sb"], [f"rK{par}"])
            P.mm(rK[par][:, 256 + a * 64:256 + (a + 1) * 64], qT_h, S_h, True, True, [H("qkT"), "Sb"], [f"rK{par}"])
        yield
        for par in range(2):
            P.tt(DVE, tmp4[:, :, par, :], rK[par][:, 0:256].rearrange("p (a d) -> p a d", a=4),
                 bc(negeg8[:, t, par::2].unsqueeze(2), [128, 4, 64]), ALU.mult, [f"rK{par}", "negeg8"], ["tmp"])
        P.tt(DVE, rp[:], tmp[:], v_f[hb][:].rearrange("p (h d) -> p h d", h=8), ALU.add, ["tmp", H("v_f")], ["rp"])
        yield
        for h in range(8):
            P.mm(rCv[:, h, :], PTb[hb][:, hi(h), :], rp[:, h, :], True, True, [H("PTb") + f"_{h % 2}", "rp"], ["rC"])
        yield
        P.tt(DVE, vn[:], rCv, bc(beta8[:, t, :].unsqueeze(2), [128, 8, 64]), ALU.mult, ["rC", "beta8"], ["vn"])
        yield
        for h in range(8):
            P.mm(rDv[hp(h):hp(h) + 64, h // 2, :], kdec[hb][:, h * 64:(h + 1) * 64], vn[:, h, :], True, True,
                 [H("kdec"), "vn"], ["rD"])
        for h in range(8):
            P.mm(rCv[:, h, :], inT[hb][:, hi(h), :], vn[:, h, :], True, True, [H("inT") + f"_{h % 2}", "vn"], ["rC"])
        yield
        P.tt(DVE, Sf[:], Sf[:], bc(eglS[:, t, :].unsqueeze(2), [128, 4, 64]), ALU.mult, ["Sf", "eglS"], ["Sf"])
        P.tt(DVE, Sb[:], Sf[:], rDv, ALU.add, ["Sf", "rD"], ["Sb"])
        P.tt(DVE, Sf[:], Sf[:], rDv, ALU.add, ["Sf", "rD"], ["Sf"])
        for par in range(2):
            P.tt(DVE, tmp4[:, :, par, :], rK[par][:, 256:512].rearrange("p (a d) -> p a d", a=4),
                 bc(eg8[:, t, par::2].unsqueeze(2), [128, 4, 64]), ALU.mult, [f"rK{par}", "eg8"], ["tmp"])
        P.tt(DVE, o_f[hb][:].rearrange("p (h d) -> p h d", h=8), tmp[:], rCv, ALU.add, ["tmp", "rC"], [H("o_f")])
        yield

    def epi(t):
        hb = t % 3
        H = lambda s: f"{s}{hb}"
        ob = t % 2
        E = lambda s: f"{s}{ob}"
        P.dma(SP, zt[hb][:], C.zbuf[t * 128:(t + 1) * 128, :], [], [H("zt")], H("zt"))
        P.tt(POOL, o2[ob][:], o_f[hb][:], o_f[hb][:], ALU.mult, [H("o_f")], [E("o2")])
        yield
        P.act(zt[hb][:], zt[hb][:], AF.Silu, [H("zt")], [H("zt")])
        P.op(DVE, lambda e: e.tensor_reduce(ssn[ob][:], o2[ob][:].rearrange("p (h d) -> p h d", h=8), AX.X, ALU.add),
             [E("o2")], [E("ssn")])
        P.ts(DVE, ssn[ob][:], ssn[ob][:], 1.0 / 64.0, 1e-6, ALU.mult, ALU.add, [E("ssn")], [E("ssn")])
        yield
        P.act(ssn[ob][:], ssn[ob][:], AF.Ln, [E("ssn")], [E("ssn")])
        P.act(ssn[ob][:], ssn[ob][:], AF.Exp, [E("ssn")], [E("ssn")], scale=-0.5)
        yield
        P.tt(POOL, o2[ob][:].rearrange("p (h d) -> p h d", h=8), o_f[hb][:].rearrange("p (h d) -> p h d", h=8),
             bc(ssn[ob][:].unsqueeze(2), [128, 8, 64]), ALU.mult, [H("o_f"), E("ssn")], [E("o2")])
        yield
        P.tt(POOL, o2[ob][:].rearrange("p (h d) -> p h d", h=8), o2[ob][:].rearrange("p (h d) -> p h d", h=8),
             bc(wn[:].unsqueeze(1), [128, 8, 64]), ALU.mult, [E("o2"), "wn"], [E("o2")])
        yield
        P.tt(POOL, o_bf[ob][:], o2[ob][:], zt[hb][:], ALU.mult, [E("o2"), H("zt")], [E("o_bf")])
        yield
        (pi,) = yield from acquire(1)
        ptv = pp[pi][:].bitcast(BF16).rearrange("p (a t) -> p a t", a=8)
        for a in range(4):
            P.tr(ptv[:, a, :], o_bf[ob][:, a * 128:(a + 1) * 128], identb[:], [E("o_bf"), "identb"], [f"pp{pi}"])
        yield
        P.copy(ACT, oT[ob][:], ptv[:, 0:4, :], [f"pp{pi}"], [f"oT{ob}"])
        release(pi)
        dst = C.catT[0:512, t * 128:(t + 1) * 128].rearrange("(a p) t -> p a t", p=128)
        P.dma(POOL, dst, oT[ob][:], [f"oT{ob}"], [("catT_dn", t)], f"oT{ob}")
        yield

    preps = {}
    epis = []
    prep_done = set()
    next_prep = 0
    rec_t = 0
    rec_gen = None
    NTD = C.dn_tiles
    while rec_t < NTD or epis or preps:
        while next_prep < NTD and next_prep <= rec_t + 2 and len(preps) < 2:
            preps[next_prep] = prep(next_prep)
            next_prep += 1
        for tt_ in list(preps):
            try:
                C.dbg_stage = getattr(C, "dbg_stage", 0) + 1
                if C.dbg_stage > getattr(C, "dbg_maxstage", 10 ** 9):
                    raise StopIteration
                next(preps[tt_])
            except StopIteration:
                prep_done.add(tt_)
                del preps[tt_]
        if getattr(C, "dbg_norec", False) and not preps:
            break
        if rec_gen is None and rec_t < NTD and rec_t in prep_done:
            rec_gen = recur(rec_t)
        if rec_gen is not None:
            try:
                next(rec_gen)
            except StopIteration:
                rec_gen = None
                epis.append(epi(rec_t))
                rec_t += 1
        for g in list(epis):
            try:
                next(g)
            except StopIteration:
                epis.remove(g)
    if getattr(C, "dn_dump", None):
        dumps = {"d_qkT": (qkT[0], ["qkT0"]), "d_kdec": (kdec[0], ["kdec0"]), "d_vf": (v_f[0], ["v_f0"]),
                 "d_inT": (inT[0], ["inT0_0", "inT0_1"]), "d_PTb": (PTb[0], ["PTb0_0", "PTb0_1"]),
                 "d_MT0": (MTb[0][0], ["MT00_0", "MT00_1"]), "d_M0": (Mb[0][0], ["M00_0", "M00_1"]),
                 "d_gc8": (gc8, ["gc8"]), "d_beta8": (beta8, ["beta8"]), "d_eg8": (eg8, ["eg8"]), "d_Sf": (Sf, ["Sf"]),
                 "d_of": (o_f[0], ["o_f0"]), "d_gg8": (gg8, ["gg8"]), "d_kd8": (kd8, ["kd8"]), "d_glb8": (glb8, ["glb8"]),
                 "d_act": (act[0], ["act0"]), "d_qkb": (qkb[0], ["qkb0"]), "d_PTf": (PTf[0], ["PTf0_0", "PTf0_1"]),
                 "d_vn": (vn, ["vn"]), "d_rp": (rp, ["rp"])}
        for nm, (tile, keys) in dumps.items():
            if nm in C.dn_dump:
                P.dma(SP, C.dn_dump[nm], tile[:], keys, [nm], nm)
    return P.emit()


INPUT_SHAPES = {
    "x": ([S, D], F32), "w_in": ([DEPTH, D, IN_W], F32), "w_out": ([DEPTH, D, D], F32),
    "ffn_w_gate": ([2, D, DFF], F32), "ffn_w_up": ([2, D, DFF], F32), "ffn_w_down": ([2, DFF, D], F32),
    "moe_w_gate": ([2, NE, D, DFE], F32), "moe_w_up": ([2, NE, D, DFE], F32), "moe_w_down": ([2, NE, DFE, D], F32),
    "ident_bf": ([128, 128], BF16), "ident_f32": ([128, 128], F32), "ones_f32": ([128, 128], F32),
    "tri_f32": ([128, 128], F32), "mcausT": ([128, 128], F32), "mnegT": ([128, 128], F32), "maskneg": ([128, 128], F32),
    "conv_w_bc": ([DEPTH, 128, 6144], F32), "a_log_bc": ([DEPTH, 128, 8], F32), "dt_bias_bc": ([DEPTH, 128, 8], F32),
    "dn_norm_bc": ([DEPTH, 128, 64], F32), "df_lambda_bc": ([DEPTH, 128, 256], F32), "df_subln_col": ([DEPTH, 128, 1], F32),
    "ln1_g_bc": ([DEPTH, 128, D], F32), "ln1_b_bc": ([DEPTH, 128, D], F32), "ln2_g_bc": ([DEPTH, 128, D], F32),
    "ln2_b_bc": ([DEPTH, 128, D], F32), "router_bc": ([2, 128, NE, D], F32),
    "bias_tiles": ([4, 2, 128, 128], F32), "cfar": ([128, 4], F32),
}


DUMP_SHAPES = {"d_qkT": ([128, 8, 128], BF16), "d_kdec": ([128, 512], BF16), "d_vf": ([128, 512], F32),
               "d_inT": ([128, 8, 128], BF16), "d_PTb": ([128, 8, 128], BF16), "d_MT0": ([128, 8, 128], BF16),
               "d_M0": ([128, 8, 128], BF16), "d_gc8": ([128, NT, 8], F32), "d_beta8": ([128, NT, 8], F32),
               "d_eg8": ([128, NT, 8], F32), "d_Sf": ([128, 4, 64], F32), "d_of": ([128, 512], F32),
               "d_gg8": ([128, NT, 8], F32), "d_kd8": ([128, NT, 8], F32), "d_glb8": ([128, NT, 8], F32),
               "d_act": ([128, 1536], F32), "d_qkb": ([128, 1024], BF16), "d_PTf": ([128, 8, 128], F32),
               "d_vn": ([128, 8, 64], BF16), "d_rp": ([128, 8, 64], BF16)}


def build(n_layers=DEPTH, debug=(), stop_after=None, skip_inputs=(), dn_tiles=NT, only=None):
    nc = bass.Bass("TRN2", target_bir_lowering=False)
    C = Ctx()
    C.nc = nc
    C.debug = set(debug)
    C.dn_tiles = dn_tiles
    import os as _os
    C.dbg_maxstage = int(_os.environ.get('DN_MAXSTAGE', 10 ** 9))
    C.dbg_norec = bool(int(_os.environ.get('DN_NOREC', '0')))
    for name, (shape, dt) in INPUT_SHAPES.items():
        if name in skip_inputs:
            continue
        setattr(C, name, nc.dram_tensor(name, list(shape), dt, kind="ExternalInput").ap())

    def dscr(name, shape, dt):
        kind = "ExternalOutput" if name in C.debug else "Internal"
        return nc.dram_tensor(name, list(shape), dt, kind=kind).ap()

    C.out = nc.dram_tensor("out", [S, D], F32, kind="ExternalOutput").ap()
    C.xT = dscr("xT", [D, S], BF16)
    C.xres = dscr("xres", [S, D], F32)
    C.qkvpre = dscr("qkvpre", [S + 3, 1536], F32)
    C.zbuf = dscr("zbuf", [S, 512], F32)
    C.Vdf = dscr("Vdf", [S, 512], BF16)
    C.catT = dscr("catT", [D, S], BF16)
    C.QT = [dscr(f"QT{h}", [128, S], BF16) for h in range(4)]
    C.KT = [dscr(f"KT{h}", [128, S], BF16) for h in range(4)]
    C.dn_dump = {}
    for nm in C.debug:
        if nm.startswith("d_"):
            shp, dt = DUMP_SHAPES[nm]
            C.dn_dump[nm] = nc.dram_tensor(nm, list(shp), dt, kind="ExternalOutput").ap()
    gstack = ExitStack()
    C.ab_sb = gstack.enter_context(nc.sbuf_tensor("ab_sb", [128, NT, 16], F32))
    C.gate_sb = gstack.enter_context(nc.sbuf_tensor("gate_sb", [128, NT, NE], F32))
    stats = {}
    C.stats = stats
    P = Phase(nc, "z0")
    zt = P.sb("zt", [3, 1536], F32)
    P.op(DVE, lambda e: e.memset(zt[:], 0.0), [], ["zt"])
    P.dma(SP, C.qkvpre[0:3, :], zt[:], ["zt"], ["pad"], "zt")
    P.emit()
    if only is None or "x0" in only:
        stats["x0"] = phase_x0(C)
    done = False
    for layer in range(n_layers):
        for nm, fn in (("ip", phase_inproj), ("dn", phase_dn), ("at", phase_attn), ("op", phase_outproj)):
            if only is None or f"{nm}{layer}" in only:
                stats[f"{nm}{layer}"] = fn(C, layer)
            if stop_after == f"{nm}{layer}":
                done = True
                break
        if done:
            break
        if only is None or f"ff{layer}" in only:
            stats[f"ff{layer}"] = phase_ffn(C, layer, layer == n_layers - 1)
        if stop_after == f"ff{layer}":
            break
    gstack.close()
    return nc, C


def t5_bucket_np(dist):
    dist = np.asarray(dist, dtype=np.int64)
    d = np.maximum(dist, 1).astype(np.float32)
    large = 16 + (np.log(d / np.float32(16.0)) / np.float32(math.log(128 / 16)) * np.float32(16.0)).astype(np.int32)
    large = np.minimum(large, 31)
    return np.where(dist < 16, dist, large)


def host_inputs(inputs):
    f32 = np.float32
    ii = np.arange(128)
    m = {}
    m["ident_bf"] = np.eye(128, dtype=f32).astype(ml_dtypes.bfloat16)
    m["ident_f32"] = np.eye(128, dtype=f32)
    m["ones_f32"] = np.ones((128, 128), f32)
    m["tri_f32"] = (ii[:, None] <= ii[None, :]).astype(f32)
    m["mcausT"] = (ii[None, :] >= ii[:, None]).astype(f32)
    m["mnegT"] = -(ii[None, :] > ii[:, None]).astype(f32)
    m["maskneg"] = np.where(ii[None, :] >= ii[:, None], 0.0, -1e5).astype(f32)

    def bc128(a):
        a = np.asarray(a, dtype=f32)
        return np.ascontiguousarray(np.broadcast_to(a[:, None, :], (a.shape[0], 128, a.shape[1])))

    m["conv_w_bc"] = bc128(np.asarray(inputs["conv_w"]).reshape(DEPTH, 4 * 1536))
    m["a_log_bc"] = bc128(inputs["dn_a_log"])
    m["dt_bias_bc"] = bc128(inputs["dn_dt_bias"])
    m["dn_norm_bc"] = bc128(inputs["dn_norm_w"])
    m["df_lambda_bc"] = bc128(np.asarray(inputs["df_lambda"]).reshape(DEPTH, 256))
    m["df_subln_col"] = np.ascontiguousarray(np.asarray(inputs["df_subln_w"], dtype=f32).reshape(DEPTH, 128, 1))
    for k in ("ln1_g", "ln1_b", "ln2_g", "ln2_b"):
        m[k + "_bc"] = bc128(inputs[k])
    r = np.asarray(inputs["moe_router"], dtype=f32).transpose(0, 2, 1)
    m["router_bc"] = np.ascontiguousarray(np.broadcast_to(r[:, None, :, :], (2, 128, NE, D)))
    rb = np.asarray(inputs["rel_bias"], dtype=f32)
    bt = np.zeros((4, 2, 128, 128), f32)
    for rel in range(2):
        dist = rel * 128 + ii[None, :] - ii[:, None]
        bk = t5_bucket_np(np.maximum(dist, 0))
        g = rb[bk]
        g = np.where((dist >= 0)[:, :, None], g, 0.0)
        bt[:, rel] = g.transpose(2, 0, 1)
    m["bias_tiles"] = bt
    m["cfar"] = np.ascontiguousarray(np.broadcast_to(rb[31][None, :], (128, 4)))
    for k in ("w_in", "w_out", "ffn_w_gate", "ffn_w_up", "ffn_w_down", "moe_w_gate", "moe_w_up", "moe_w_down"):
        m[k] = np.ascontiguousarray(np.asarray(inputs[k], dtype=f32))
    return m


def kernel(**inputs):
    nc, C = build()
    shared = host_inputs(inputs)
    in_maps = []
    for b in range(8):
        m = dict(shared)
        m["x"] = np.ascontiguousarray(np.asarray(inputs["x"][b], dtype=np.float32))
        in_maps.append(m)
    res = run_bass_kernel_spmd(nc, in_maps, core_ids=list(range(8)))
    return np.stack([np.asarray(r["out"], dtype=np.float32) for r in res.results], axis=0)
```

```python
import math
import numpy as np
import ml_dtypes
from contextlib import ExitStack
import concourse.bass as bass
import concourse.mybir as mybir
from concourse.bass_utils import run_bass_kernel_spmd

F32 = mybir.dt.float32
BF16 = mybir.dt.bfloat16
AF = mybir.ActivationFunctionType
ALU = mybir.AluOpType
AX = mybir.AxisListType

PE, ACT, DVE, POOL, SP = "pe", "act", "dve", "pool", "sp"
ENGMAP = {PE: "tensor", ACT: "scalar", DVE: "vector", POOL: "gpsimd", SP: "sync"}
SEM_EPOCH = 30000

S = 8192
D = 1024
NT = S // 128
DEPTH = 4
IN_W = 3600
DFF = 2816
DFE = 3584
NE = 8
ALPHA = (2 * DEPTH) ** 0.25
LN_EPS = 1e-5


class Op:
    __slots__ = ("eng", "fn", "deps", "signal", "sem", "val", "is_dma", "grp", "ndep", "seq")

    def __init__(self, eng, fn, is_dma, grp):
        self.eng = eng
        self.fn = fn
        self.deps = []
        self.signal = False
        self.sem = None
        self.val = 0
        self.is_dma = is_dma
        self.grp = grp
        self.ndep = 0


class Phase:
    def __init__(self, nc, name):
        self.nc = nc
        self.name = name
        self.ops = []
        self.last_w = {}
        self.readers = {}
        self.stack = ExitStack()
        self.excl = set()
        self.alias = {}
        self.eng_seq = {}

    def sb(self, name, shape, dt):
        return self.stack.enter_context(self.nc.sbuf_tensor(f"{self.name}_{name}", list(shape), dt))

    def ps(self, name, shape, dt=F32):
        return self.stack.enter_context(self.nc.psum_tensor(f"{self.name}_{name}", list(shape), dt))

    def op(self, eng, fn, r=(), w=(), dma=False, grp=None):
        if self.alias:
            r = [self.alias.get(k, k) for k in r]
            w = [self.alias.get(k, k) for k in w]
        o = Op(eng, fn, dma, grp)
        o.seq = self.eng_seq.get(eng, 0)
        self.eng_seq[eng] = o.seq + 1
        deps = []
        seen = set()
        raw = set()
        for k in r:
            lw = self.last_w.get(k)
            if lw is not None:
                raw.add(id(lw))
                if id(lw) not in seen:
                    seen.add(id(lw))
                    deps.append(lw)
            if k in self.excl:
                for rd in self.readers.get(k, ()):
                    if rd.eng != eng and id(rd) not in seen:
                        seen.add(id(rd))
                        raw.add(id(rd))
                        deps.append(rd)
        for k in w:
            lw = self.last_w.get(k)
            if lw is not None and id(lw) not in seen:
                seen.add(id(lw))
                deps.append(lw)
            for rd in self.readers.get(k, ()):
                if id(rd) not in seen:
                    seen.add(id(rd))
                    deps.append(rd)
        for d in deps:
            if d.eng == eng and not d.is_dma and not dma:
                if eng == PE:
                    continue
                if id(d) not in raw and o.seq - d.seq > 1:
                    continue
            o.deps.append(d)
            d.ndep += 1
        for k in r:
            self.readers.setdefault(k, []).append(o)
        for k in w:
            self.last_w[k] = o
            self.readers[k] = []
        self.ops.append(o)
        return o

    def mm(self, out, lhsT, rhs, start, stop, r, w, **kw):
        return self.op(PE, lambda e: e.matmul(out, lhsT, rhs, start=start, stop=stop, **kw), r, w)

    def tr(self, out, in_, ident, r, w):
        return self.op(PE, lambda e: e.transpose(out, in_, ident), r, w)

    def act(self, out, in_, func, r, w, **kw):
        return self.op(ACT, lambda e: e.activation(out, in_, func, **kw), r, w)

    def copy(self, eng, out, in_, r, w):
        if eng == ACT:
            return self.op(ACT, lambda e: e.copy(out, in_), r, w)
        return self.op(eng, lambda e: e.tensor_copy(out, in_), r, w)

    def tt(self, eng, out, in0, in1, op, r, w):
        return self.op(eng, lambda e: e.tensor_tensor(out, in0, in1, op), r, w)

    def ts(self, eng, out, in0, s1, s2, op0, op1, r, w):
        return self.op(eng, lambda e: e.tensor_scalar(out, in0, s1, s2, op0, op1), r, w)

    def stt(self, out, in0, scalar, in1, op0, op1, r, w):
        return self.op(DVE, lambda e: e.scalar_tensor_tensor(out, in0, scalar, in1, op0, op1), r, w)

    def dma(self, q, out, in_, r, w, grp, **kw):
        return self.op(q, lambda e: e.dma_start(out, in_, **kw), r, w, dma=True, grp=grp)

    def emit(self):
        nc = self.nc
        leaf = [o for o in self.ops if o.is_dma and o.ndep == 0]
        last = {}
        for o in self.ops:
            if not o.is_dma:
                last[o.eng] = o
        leaf += list(last.values())
        if leaf:
            fin = Op(SP, lambda e: e.nop(), False, None)
            fin.deps = leaf
            self.ops.append(fin)
        for o in self.ops:
            for d in o.deps:
                d.signal = True
        sem_state = {}
        nsem = [0]

        sem_handles = []

        def new_sem():
            nsem[0] += 1
            h = nc.alloc_semaphore(name=f"{self.name}_s{nsem[0]}")
            sem_handles.append(h)
            return h

        for o in self.ops:
            if not o.signal:
                continue
            key = ("dma", o.grp) if o.is_dma else o.eng
            st = sem_state.get(key)
            inc = 16 if o.is_dma else 1
            if st is None or st[1] + inc > SEM_EPOCH:
                st = [new_sem(), 0]
                sem_state[key] = st
            st[1] += inc
            o.sem = st[0]
            o.val = st[1]
        self.n_sems = nsem[0]
        per_eng = {}
        for o in self.ops:
            per_eng.setdefault(o.eng, []).append(o)
        with nc.Block() as block:
            for ename, lst in per_eng.items():
                def body(e, lst=lst):
                    waited = {}
                    for o in lst:
                        need = {}
                        for d in o.deps:
                            k = id(d.sem)
                            if k not in need or need[k][1] < d.val:
                                need[k] = (d.sem, d.val)
                        for k, (s, v) in need.items():
                            if waited.get(k, 0) >= v:
                                continue
                            e.wait_ge(s, v)
                            waited[k] = v
                        ins = o.fn(e)
                        if o.signal:
                            ins.then_inc(o.sem, 16 if o.is_dma else 1)
                getattr(block, ENGMAP[ename])(body)
        if sem_handles:
            nc.clear_and_free_semaphores(sem_handles)
            nc.all_engine_barrier()
        nops = len(self.ops)
        self.ops = None
        self.last_w = None
        self.readers = None
        self.stack.close()
        return nops


class Ctx:
    pass


def rr(lst, state=[0]):
    state[0] += 1
    return lst[state[0] % len(lst)]


def phase_x0(C):
    nc = C.nc
    P = Phase(nc, "x0")
    ident = P.sb("ident", [128, 128], BF16)
    P.dma(SP, ident[:], C.ident_bf, [], ["ident"], "ident")
    xin = [P.sb(f"xin{i}", [128, D], F32) for i in range(2)]
    xb = [P.sb(f"xb{i}", [128, D], BF16) for i in range(2)]
    xt = [P.sb(f"xt{i}", [128, 8, 128], BF16) for i in range(2)]
    pt = [P.ps(f"pt{i}", [128, 8, 128], BF16) for i in range(2)]
    P.excl.update(["pt0", "pt1"])
    xT3 = C.xT.rearrange("(c p) t -> p c t", p=128)
    for t in range(NT):
        b = t % 2
        P.dma(SP, xin[b][:], C.x[t * 128:(t + 1) * 128, :], [], [f"xin{b}"], f"xin{b}")
        P.copy(DVE if t % 2 else POOL, xb[b][:], xin[b][:], [f"xin{b}"], [f"xb{b}"])
        for c in range(8):
            P.tr(pt[b][:, c, :], xb[b][:, c * 128:(c + 1) * 128], ident[:], [f"xb{b}", "ident"], [f"pt{b}"])
        P.copy(ACT if t % 2 else DVE, xt[b][:], pt[b][:], [f"pt{b}"], [f"xt{b}"])
        P.dma(POOL, xT3[:, :, t * 128:(t + 1) * 128], xt[b][:], [f"xt{b}"], [("xT", t)], f"xt{b}")
    return P.emit()


def load_cast_w(P, dst3, src2, nchunk, ncols, r_keys, wkey, stg, colstep, cnt):
    for c in range(nchunk):
        for c0 in range(0, ncols, colstep):
            n = min(colstep, ncols - c0)
            i = cnt[0] % len(stg)
            cnt[0] += 1
            P.dma(SP, stg[i][:, 0:n], src2[c * 128:(c + 1) * 128, c0:c0 + n], list(r_keys), [f"stg{i}"], f"stg{i}")
            eng = (DVE, POOL, ACT)[cnt[0] % 3]
            P.copy(eng, dst3[:, c, c0:c0 + n], stg[i][:, 0:n], [f"stg{i}"], [wkey])


def phase_inproj(C, layer):
    nc = C.nc
    P = Phase(nc, f"ip{layer}")
    W = P.sb("W", [128, 8, IN_W], BF16)
    stg = [P.sb(f"stg{i}", [128, 1800], F32) for i in range(3)]
    cnt = [0]
    load_cast_w(P, W, C.w_in[layer], 8, IN_W, [], "W", stg, 1800, cnt)
    xTb = [P.sb(f"xTb{i}", [128, 8, 512], BF16) for i in range(2)]
    qk_sb = [P.sb(f"qk{i}", [128, 512], BF16) for i in range(3)]
    qkv_sb = [P.sb(f"qkv{i}", [128, 1536], F32) for i in range(2)]
    z_sb = [P.sb(f"z{i}", [128, 512], F32) for i in range(2)]
    v_sb = [P.sb(f"v{i}", [128, 512], BF16) for i in range(2)]
    ps = [P.ps(f"ps{i}", [128, 512], F32) for i in range(8)]
    P.excl.update([f"ps{i}" for i in range(8)])
    xT3 = C.xT.rearrange("(c p) t -> p c t", p=128)
    pi = 0
    ev = 0
    nqk = 0
    for tb in range(S // 512):
        b = tb % 2
        P.dma(SP, xTb[b][:], xT3[:, :, tb * 512:(tb + 1) * 512], [("xT", tb * 4 + i) for i in range(4)],
              [f"xTb{b}"], f"xTb{b}")
        for g in range(8):
            col0 = 2064 + g * 128
            p = pi % 8
            pi += 1
            for c in range(8):
                P.mm(ps[p][:, :], W[:, c, col0:col0 + 128], xTb[b][:, c, :], c == 0, c == 7,
                     ["W", f"xTb{b}"], [f"ps{p}"])
            i = nqk % 3
            nqk += 1
            P.copy(ACT if ev % 2 else DVE, qk_sb[i][:], ps[p][:, :], [f"ps{p}"], [f"qk{i}"])
            ev += 1
            dst = C.QT[g] if g < 4 else C.KT[g - 4]
            P.dma(POOL, dst[:, tb * 512:(tb + 1) * 512], qk_sb[i][:], [f"qk{i}"], [("qkT", g, tb)], f"qk{i}")
        for tt in range(4):
            t = tb * 4 + tt
            tb2 = t % 2
            groups = [(0, 512, "qkv"), (512, 512, "qkv"), (1024, 512, "qkv"), (1536, 512, "z"),
                      (2048, 16, "ab"), (3088, 512, "v")]
            for (col0, n, kind) in groups:
                p = pi % 8
                pi += 1
                for c in range(8):
                    P.mm(ps[p][:, 0:n], xTb[b][:, c, tt * 128:(tt + 1) * 128], W[:, c, col0:col0 + n],
                         c == 0, c == 7, ["W", f"xTb{b}"], [f"ps{p}"])
                eng = ACT if ev % 2 else DVE
                ev += 1
                if kind == "qkv":
                    P.copy(eng, qkv_sb[tb2][:, col0:col0 + 512], ps[p][:, 0:512], [f"ps{p}"], [f"qkv{tb2}_{col0}"])
                elif kind == "z":
                    P.copy(eng, z_sb[tb2][:], ps[p][:, 0:512], [f"ps{p}"], [f"z{tb2}"])
                elif kind == "ab":
                    P.copy(eng, C.ab_sb[:, t, :], ps[p][:, 0:16], [f"ps{p}"], [("ab", t)])
                else:
                    P.copy(eng, v_sb[tb2][:], ps[p][:, 0:512], [f"ps{p}"], [f"v{tb2}"])
            P.dma(POOL, C.qkvpre[3 + t * 128:3 + (t + 1) * 128, :], qkv_sb[tb2][:],
                  [f"qkv{tb2}_0", f"qkv{tb2}_512", f"qkv{tb2}_1024"], [("qkvpre", t)], f"qkv{tb2}")
            P.dma(POOL, C.zbuf[t * 128:(t + 1) * 128, :], z_sb[tb2][:], [f"z{tb2}"], [("zbuf", t)], f"z{tb2}")
            P.dma(POOL, C.Vdf[t * 128:(t + 1) * 128, :], v_sb[tb2][:], [f"v{tb2}"], [("Vdf", t)], f"v{tb2}")
    return P.emit()


def phase_attn(C, layer):
    nc = C.nc
    P = Phase(nc, f"at{layer}")
    lambda_init = 0.8 - 0.6 * math.exp(-0.3 * layer)
    identf = P.sb("identf", [128, 128], F32)
    onesf = P.sb("onesf", [128, 128], F32)
    maskneg = P.sb("maskneg", [128, 128], F32)
    cfar = P.sb("cfar", [128, 4], F32)
    bt = P.sb("bt", [128, 8, 128], F32)
    badd = P.sb("badd", [128, 8, 128], F32)
    dl = P.sb("dl", [128, 256], F32)
    dlp = P.sb("dlp", [128, 128], F32)
    ls = P.sb("ls", [128, 2], F32)
    le = P.sb("le", [128, 2], F32)
    neglam = P.sb("neglam", [128, 1], F32)
    wcol = P.sb("wcol", [128, 1], F32)
    P.dma(SP, identf[:], C.ident_f32, [], ["identf"], "c0")
    P.dma(SP, onesf[:], C.ones_f32, [], ["onesf"], "c1")
    P.dma(SP, maskneg[:], C.maskneg, [], ["maskneg"], "c2")
    P.dma(SP, cfar[:], C.cfar, [], ["cfar"], "c3")
    P.dma(SP, bt[:], C.bias_tiles.rearrange("h r k q -> k (h r) q"), [], ["bt"], "c4")
    P.dma(SP, dl[:], C.df_lambda_bc[layer], [], ["dl"], "c5")
    P.dma(SP, wcol[:], C.df_subln_col[layer], [], ["wcol"], "c6")
    for h in range(4):
        for rel in range(2):
            i = h * 2 + rel
            P.ts(DVE, badd[:, i, :], bt[:, i, :], cfar[:, h:h + 1], 8.0, ALU.subtract, ALU.mult,
                 ["bt", "cfar"], [("badd", i)])
            if rel == 0:
                P.tt(DVE, badd[:, i, :], badd[:, i, :], maskneg[:], ALU.add, [("badd", i), "maskneg"], [("badd", i)])
    P.tt(DVE, dlp[:, 0:64], dl[:, 0:64], dl[:, 64:128], ALU.mult, ["dl"], ["dlp0"])
    P.tt(DVE, dlp[:, 64:128], dl[:, 128:192], dl[:, 192:256], ALU.mult, ["dl"], ["dlp1"])
    P.op(DVE, lambda e: e.tensor_reduce(ls[:], dlp[:].rearrange("p (a b) -> p a b", a=2), AX.X, ALU.add),
         ["dlp0", "dlp1"], ["ls"])
    P.act(le[:], ls[:], AF.Exp, ["ls"], ["le"])
    P.tt(DVE, neglam[:], le[:, 1:2], le[:, 0:1], ALU.subtract, ["le"], ["neglam"])
    P.ts(DVE, neglam[:], neglam[:], -lambda_init, None, ALU.add, ALU.bypass, ["neglam"], ["neglam"])
    P.ts(DVE, wcol[:], wcol[:], 1.0 - lambda_init, None, ALU.mult, ALU.bypass, ["wcol"], ["wcol"])

    KTh = [P.sb(f"KTh{i}", [128, S], BF16) for i in range(2)]
    QTz = [[P.sb(f"QTz{i}_{m}", [128, S], BF16) for m in range(2)] for i in range(2)]
    for i in range(2):
        for m in range(2):
            P.op(POOL if (i + m) % 2 else DVE, lambda e, i=i, m=m: e.memset(QTz[i][m][:], 0.0), [], [f"QTh{i}"])
    Vh = [P.sb(f"Vh{i}", [128, NT, 128], BF16) for i in range(2)]
    pT = [P.sb(f"pT{i}", [128, 512], BF16) for i in range(4)]
    PSa = [P.sb(f"PSa{i}", [128, 512], F32) for i in range(2)]
    rden = [P.sb(f"rden{i}", [128, 512], F32) for i in range(2)]
    on = [P.sb(f"on{i}", [128, 512], F32) for i in range(2)]
    o_sb = P.sb("o_sb", [128, 512], F32)
    sq = P.sb("sq", [128, 512], F32)
    rstd = P.sb("rstd", [128, 512], F32)
    of = [P.sb(f"of{i}", [128, 512], BF16) for i in range(2)]
    accO = [P.ps(f"accO{i}", [128, 512], F32) for i in range(2)]
    sc = [P.ps(f"sc{i}", [128, 512], F32) for i in range(4)]
    aux = [P.ps(f"aux{i}", [128, 512], F32) for i in range(2)]
    P.excl.update(["accO0", "accO1", "sc0", "sc1", "sc2", "sc3", "aux0", "aux1"])
    V3 = C.Vdf.rearrange("(t p) c -> p t c", p=128)

    def load_head(h):
        b = h % 2
        P.dma(SP, KTh[b][:], C.KT[h], [], [f"KTh{b}"], f"KTh{b}")
        for m in range(2):
            P.dma(SP, QTz[b][m][m * 64:(m + 1) * 64, :], C.QT[h][m * 64:(m + 1) * 64, :], [], [f"QTh{b}"], f"QTh{b}_{m}")
        for i in range(8):
            P.dma(SP, Vh[b][:, i * 8:(i + 1) * 8, :], V3[:, i * 8:(i + 1) * 8, h * 128:(h + 1) * 128], [],
                  [(f"Vh{b}", i)], f"Vh{b}_{i}")

    load_head(0)
    LA = 3
    nq = 0
    gstep = 0
    for h in range(4):
        b = h % 2
        if h + 1 < 4:
            load_head(h + 1)
        steps = []
        for Qb in range(S // 512):
            nk = 4 * Qb + 4
            for kt in range(nk):
                for m in range(2):
                    steps.append((Qb, kt, m, nk))
        ns = len(steps)

        def front(si, gs):
            Qb, kt, m, nk = steps[si]
            j = kt - 4 * Qb
            c0 = max(0, j) * 128
            sI = gs % 4
            pi = gs % 4
            adds = []
            if j >= 0:
                adds.append((j * 128, h * 2 + 0))
            if j >= -1 and j + 1 <= 3:
                adds.append(((j + 1) * 128, h * 2 + 1))
            P.mm(sc[sI][:, c0:512], KTh[b][:, kt * 128:(kt + 1) * 128],
                 QTz[b][m][:, Qb * 512 + c0:Qb * 512 + 512], True, len(adds) == 0,
                 [f"KTh{b}", f"QTh{b}"], [f"sc{sI}"])
            for ai, (cc, bi) in enumerate(adds):
                P.mm(sc[sI][:, cc:cc + 128], identf[:], badd[:, bi, :], False, ai == len(adds) - 1,
                     ["identf", ("badd", bi)], [f"sc{sI}"])
            P.act(pT[pi][:, c0:512], sc[sI][:, c0:512], AF.Exp, [f"sc{sI}"], [f"pT{pi}"], scale=0.125)

        def back(si, gs):
            nonlocal nq
            Qb, kt, m, nk = steps[si]
            j = kt - 4 * Qb
            c0 = max(0, j) * 128
            pi = gs % 4
            P.mm(accO[m][:, c0:512], Vh[b][:, kt, :], pT[pi][:, c0:512], kt == 0, kt == nk - 1,
                 [(f"Vh{b}", kt // 8), f"pT{pi}"], [f"accO{m}"])
            eng = DVE if m == 0 else POOL
            if kt == 0:
                P.copy(eng, PSa[m][:], pT[pi][:], [f"pT{pi}"], [f"PSa{m}"])
            else:
                P.tt(eng, PSa[m][:, c0:512], PSa[m][:, c0:512], pT[pi][:, c0:512], ALU.add,
                     [f"PSa{m}", f"pT{pi}"], [f"PSa{m}"])
            if not (kt == nk - 1 and m == 1):
                return
            for mm_ in range(2):
                P.mm(aux[mm_][:], onesf[:], PSa[mm_][:], True, True, ["onesf", f"PSa{mm_}"], [f"aux{mm_}"])
                P.op(DVE, lambda e, mm_=mm_: e.reciprocal(rden[mm_][:], aux[mm_][:]), [f"aux{mm_}"], [f"rden{mm_}"])
                P.tt(DVE, on[mm_][:], accO[mm_][:], rden[mm_][:], ALU.mult, [f"accO{mm_}", f"rden{mm_}"], [f"on{mm_}"])
            P.stt(o_sb[:], on[1][:], neglam[:, 0:1], on[0][:], ALU.mult, ALU.add, ["on0", "on1", "neglam"], ["o_sb"])
            P.tt(POOL, sq[:], o_sb[:], o_sb[:], ALU.mult, ["o_sb"], ["sq"])
            P.mm(aux[0][:], onesf[:], sq[:], True, True, ["onesf", "sq"], ["aux0"])
            P.ts(DVE, rstd[:], aux[0][:], 1.0 / 128.0, 1e-6, ALU.mult, ALU.add, ["aux0"], ["rstd"])
            P.act(rstd[:], rstd[:], AF.Ln, ["rstd"], ["rstd"])
            P.act(rstd[:], rstd[:], AF.Exp, ["rstd"], ["rstd"], scale=-0.5)
            P.tt(DVE, o_sb[:], o_sb[:], rstd[:], ALU.mult, ["o_sb", "rstd"], ["o_sb"])
            ob = nq % 2
            nq += 1
            P.ts(DVE, of[ob][:], o_sb[:], wcol[:, 0:1], None, ALU.mult, ALU.bypass, ["o_sb", "wcol"], [f"of{ob}"])
            P.dma(POOL, C.catT[512 + h * 128:512 + (h + 1) * 128, Qb * 512:(Qb + 1) * 512], of[ob][:],
                  [f"of{ob}"], [("catT_df", h, Qb)], f"of{ob}")

        for idx in range(ns + LA):
            if idx < ns:
                front(idx, gstep + idx)
            if idx - LA >= 0:
                back(idx - LA, gstep + idx - LA)
        gstep += ns
    return P.emit()


def ln_epilogue(P, C, t, src_halves, src_keys, res_src, gb, ident, bufs, pt, pt_key, out_dst=None, router=None):
    i = t % 2
    xr, y, xo, xb, xt = bufs["xr"][i], bufs["y"][i], bufs["xo"][i], bufs["xb"][i], bufs["xt"][i]
    kxr, ky, kxo, kxb, kxt = bufs["kxr"][i], bufs["ky"][i], bufs["kxo"][i], bufs["kxb"][i], bufs["kxt"][i]
    st, mv, rs, nmr = bufs["st"][i], bufs["mv"][i], bufs["rs"][i], bufs["nmr"][i]
    ks = f"lnsmall{i}"
    P.dma(SP, xr[:, 0:D], res_src[t * 128:(t + 1) * 128, :], [("xres", t)], [kxr], f"ln_{kxr}")
    for hf in range(2):
        P.stt(y[:, hf * 512:(hf + 1) * 512], xr[:, hf * 512:(hf + 1) * 512], ALPHA, src_halves[hf], ALU.mult, ALU.add,
              [kxr, src_keys[hf]], [ky])
        P.op(DVE, lambda e, hf=hf: e.bn_stats(st[:, hf, :], y[:, hf * 512:(hf + 1) * 512]), [ky], [ks + f"st{hf}"])
    P.op(DVE, lambda e: e.bn_aggr(mv[:], st[:].rearrange("p a b -> p (a b)")), [ks + "st0", ks + "st1"], [ks + "mv"])
    P.ts(DVE, rs[:], mv[:, 1:2], LN_EPS, None, ALU.add, ALU.bypass, [ks + "mv"], [ks + "rs"])
    P.act(rs[:], rs[:], AF.Ln, [ks + "rs"], [ks + "rs"])
    P.act(rs[:], rs[:], AF.Exp, [ks + "rs"], [ks + "rs"], scale=-0.5)
    P.ts(DVE, nmr[:], mv[:, 0:1], rs[:, 0:1], -1.0, ALU.mult, ALU.mult, [ks + "mv", ks + "rs"], [ks + "nmr"])
    P.ts(POOL, y[:, 0:D], y[:, 0:D], rs[:, 0:1], nmr[:, 0:1], ALU.mult, ALU.add, [ky, ks + "rs", ks + "nmr"],
         [ky])
    P.tt(POOL, y[:, 0:D], y[:, 0:D], gb[0][:], ALU.mult, [ky, "ln_g"], [ky])
    P.tt(DVE, xo[:, 0:D], y[:, 0:D], gb[1][:], ALU.add, [ky, "ln_b"], [kxo])
    if out_dst is not None:
        P.dma(POOL, out_dst[t * 128:(t + 1) * 128, :], xo[:, 0:D], [kxo], [("out", t)], f"ln_{kxo}")
        return
    P.dma(POOL, C.xres[t * 128:(t + 1) * 128, :], xo[:, 0:D], [kxo], [("xres", t)], f"ln_{kxo}")
    P.copy(ACT, xb[:, 0:D], xo[:, 0:D], [kxo], [kxb])
    for c in range(8):
        P.tr(pt[:, c, :], xb[:, c * 128:(c + 1) * 128], ident[:], [kxb, "ident"], [pt_key])
    P.copy(ACT, xt[:], pt, [pt_key], [kxt])
    xT3 = C.xT.rearrange("(c p) t -> p c t", p=128)
    P.dma(POOL, xT3[:, :, t * 128:(t + 1) * 128], xt[:], [kxt], [("xT", t)], f"ln_{kxt}")
    if router is not None:
        rbc, lg, junk = router
        for e in range(NE):
            P.op(DVE, lambda en, e=e: en.scalar_tensor_tensor(junk[:], xo[:, 0:D], 1.0, rbc[:, e, :], ALU.mult, ALU.mult,
                                                               accum_out=lg[i][:, e:e + 1]),
                 [kxo, "rbc"], [f"lg{i}_{e}", "junk"])
        lgk = [f"lg{i}_{e}" for e in range(NE)]
        sm = bufs["sm"][i]
        kk = f"sm{i}"
        P.op(DVE, lambda en: en.tensor_reduce(sm[:, 0:1], lg[i][:], AX.X, ALU.max), lgk, [kk + "m1"])
        P.ts(DVE, sm[:, 8:16], lg[i][:], sm[:, 0:1], None, ALU.is_equal, ALU.bypass, lgk + [kk + "m1"], [kk + "k1"])
        P.stt(sm[:, 24:32], sm[:, 8:16], -1e30, lg[i][:], ALU.mult, ALU.add, [kk + "k1"] + lgk, [kk + "l2"])
        P.op(DVE, lambda en: en.tensor_reduce(sm[:, 1:2], sm[:, 24:32], AX.X, ALU.max), [kk + "l2"], [kk + "m2"])
        P.ts(DVE, sm[:, 16:24], sm[:, 24:32], sm[:, 1:2], None, ALU.is_equal, ALU.bypass, [kk + "l2", kk + "m2"], [kk + "k2"])
        P.tt(DVE, sm[:, 2:3], sm[:, 1:2], sm[:, 0:1], ALU.subtract, [kk + "m1", kk + "m2"], [kk + "d"])
        P.act(sm[:, 2:3], sm[:, 2:3], AF.Exp, [kk + "d"], [kk + "d"])
        P.ts(DVE, sm[:, 3:4], sm[:, 2:3], 1.0, None, ALU.add, ALU.bypass, [kk + "d"], [kk + "g1"])
        P.op(DVE, lambda en: en.reciprocal(sm[:, 3:4], sm[:, 3:4]), [kk + "g1"], [kk + "g1"])
        P.tt(DVE, sm[:, 4:5], sm[:, 2:3], sm[:, 3:4], ALU.mult, [kk + "d", kk + "g1"], [kk + "g2"])
        P.ts(DVE, sm[:, 8:16], sm[:, 8:16], sm[:, 3:4], None, ALU.mult, ALU.bypass, [kk + "k1", kk + "g1"], [kk + "k1"])
        P.stt(C.gate_sb[:, t, :], sm[:, 16:24], sm[:, 4:5], sm[:, 8:16], ALU.mult, ALU.add,
              [kk + "k2", kk + "g2", kk + "k1"], [("gate", t)])


def ln_bufs(P):
    b = {}
    for nm, shape, dt in [("xr", [128, D], F32), ("y", [128, D], F32), ("xo", [128, D], F32), ("xb", [128, D], BF16),
                          ("xt", [128, 8, 128], BF16), ("st", [128, 2, 6], F32), ("mv", [128, 2], F32),
                          ("rs", [128, 1], F32), ("nmr", [128, 1], F32), ("sm", [128, 40], F32)]:
        b[nm] = [P.sb(f"ln_{nm}{i}", shape, dt) for i in range(2)]
        b["k" + nm] = [f"ln_{nm}{i}" for i in range(2)]
    return b


def phase_outproj(C, layer):
    nc = C.nc
    P = Phase(nc, f"op{layer}")
    moe = (layer % 2 == 1)
    ident = P.sb("ident", [128, 128], BF16)
    P.dma(SP, ident[:], C.ident_bf, [], ["ident"], "c0")
    gb = [P.sb("ln_g", [128, D], F32), P.sb("ln_b", [128, D], F32)]
    P.dma(SP, gb[0][:], C.ln1_g_bc[layer], [], ["ln_g"], "c1")
    P.dma(SP, gb[1][:], C.ln1_b_bc[layer], [], ["ln_b"], "c2")
    router = None
    if moe:
        rbc = P.sb("rbc", [128, NE, D], F32)
        P.dma(SP, rbc[:], C.router_bc[layer // 2], [], ["rbc"], "c3")
        lg = [P.sb(f"lg{i}", [128, NE], F32) for i in range(2)]
        junk = P.sb("junk", [128, D], F32)
        router = (rbc, lg, junk)
    Wo = P.sb("Wo", [128, 8, D], BF16)
    stg = [P.sb(f"stg{i}", [128, 1024], F32) for i in range(3)]
    load_cast_w(P, Wo, C.w_out[layer], 8, D, [], "Wo", stg, 1024, [0])
    bufs = ln_bufs(P)
    ct = [P.sb(f"ct{i}", [128, 8, 512], BF16) for i in range(2)]
    ps = [P.ps(f"ps{i}", [128, 512], F32) for i in range(4)]
    pt = [P.ps(f"pt{i}", [128, 8, 128], BF16) for i in range(2)]
    P.excl.update(["ps0", "ps1", "ps2", "ps3", "pt0", "pt1"])
    catT3 = C.catT.rearrange("(c p) t -> p c t", p=128)
    res_src = C.x if layer == 0 else C.xres
    for tb in range(S // 512):
        b = tb % 2
        P.dma(SP, ct[b][:], catT3[:, :, tb * 512:(tb + 1) * 512], [], [f"ct{b}"], f"ct{b}")
        for tt in range(4):
            t = tb * 4 + tt
            pp = (t % 2) * 2
            for hf in range(2):
                for c in range(8):
                    P.mm(ps[pp + hf][:], ct[b][:, c, tt * 128:(tt + 1) * 128], Wo[:, c, hf * 512:(hf + 1) * 512],
                         c == 0, c == 7, [f"ct{b}", "Wo"], [f"ps{pp + hf}"])
            ln_epilogue(P, C, t, [ps[pp][:], ps[pp + 1][:]], [f"ps{pp}", f"ps{pp + 1}"], res_src, gb, ident, bufs,
                        pt[t % 2][:], f"pt{t % 2}", router=router)
    return P.emit()


def phase_ffn(C, layer, last):
    nc = C.nc
    P = Phase(nc, f"ff{layer}")
    moe = (layer % 2 == 1)
    li = layer // 2
    if moe:
        E, F = NE, DFE
        wg_of = lambda e: C.moe_w_gate[li, e]
        wu_of = lambda e: C.moe_w_up[li, e]
        wd_of = lambda e: C.moe_w_down[li, e]
    else:
        E, F = 1, DFF
        wg_of = lambda e: C.ffn_w_gate[li]
        wu_of = lambda e: C.ffn_w_up[li]
        wd_of = lambda e: C.ffn_w_down[li]
    nfc = F // 128
    groups = [(f0, min(4, nfc - f0)) for f0 in range(0, nfc, 4)]
    SBT = 16
    ident = P.sb("ident", [128, 128], BF16)
    P.dma(SP, ident[:], C.ident_bf, [], ["ident"], "c0")
    gb = [P.sb("ln_g", [128, D], F32), P.sb("ln_b", [128, D], F32)]
    P.dma(SP, gb[0][:], C.ln2_g_bc[layer], [], ["ln_g"], "c1")
    P.dma(SP, gb[1][:], C.ln2_b_bc[layer], [], ["ln_b"], "c2")
    acc = P.sb("acc", [128, SBT, D], F32)
    xTs = P.sb("xTs", [128, 8, SBT * 128], BF16)
    Wg = [P.sb(f"Wg{i}", [128, 8, 512], BF16) for i in range(2)]
    Wu = [P.sb(f"Wu{i}", [128, 8, 512], BF16) for i in range(2)]
    Wd = [P.sb(f"Wd{i}", [128, 4, D], BF16) for i in range(2)]
    stg = [P.sb(f"stg{i}", [128, 2048], F32) for i in range(3)]
    hT = [P.sb(f"hT{i}", [128, 4, 512], BF16) for i in range(2)]
    sg = [P.sb(f"sg{i}", [128, 512], F32) for i in range(2)]
    bufs = {}
    for nm, shape, dt in [("xb", [128, D], BF16), ("xt", [128, 8, 128], BF16), ("st", [128, 2, 6], F32),
                          ("mv", [128, 2], F32), ("rs", [128, 1], F32), ("nmr", [128, 1], F32), ("sm", [128, 40], F32)]:
        bufs[nm] = [P.sb(f"ln_{nm}{i}", shape, dt) for i in range(2)]
        bufs["k" + nm] = [f"ln_{nm}{i}" for i in range(2)]
    bufs["xr"] = [stg[0], stg[0]]
    bufs["kxr"] = ["stg0", "stg0"]
    bufs["y"] = [stg[1], stg[1]]
    bufs["ky"] = ["stg1", "stg1"]
    bufs["xo"] = [stg[2], stg[2]]
    bufs["kxo"] = ["stg2", "stg2"]
    pg = [P.ps(f"pg{i}", [128, 512], F32) for i in range(2)]
    pu = [P.ps(f"pu{i}", [128, 512], F32) for i in range(2)]
    py = [P.ps(f"py{i}", [128, 512], F32) for i in range(4)]
    P.excl.update(["pg0", "pg1", "pu0", "pu1", "py0", "py1", "py2", "py3"])
    xT3 = C.xT.rearrange("(c p) t -> p c t", p=128)
    cnt = [0]
    ngu = 0
    nh = 0
    ny = 0
    wi = 0
    import os as _os
    dbg_sb = int(_os.environ.get("FF_SB", NT // SBT))
    dbg_ng = int(_os.environ.get("FF_NG", 10 ** 6))
    dbg_noln = bool(int(_os.environ.get("FF_NOLN", "0")))
    for sbk in range(min(NT // SBT, dbg_sb)):
        t0 = sbk * SBT
        for q4 in range(4):
            P.dma(SP, xTs[:, :, q4 * 512:(q4 + 1) * 512], xT3[:, :, t0 * 128 + q4 * 512:t0 * 128 + (q4 + 1) * 512],
                  [("xT", t0 + q4 * 4 + i) for i in range(4)], [("xTs", q4)], f"xTs{q4}")
        first = [True] * SBT
        for e in range(E):
            for (f0, nf) in groups[:dbg_ng]:
                wb = wi % 2
                wi += 1
                for c in range(8):
                    for (dst, src, key) in ((Wg[wb], wg_of(e), f"Wg{wb}"), (Wu[wb], wu_of(e), f"Wu{wb}")):
                        i = cnt[0] % 3
                        cnt[0] += 1
                        P.dma(SP, stg[i][:, 0:nf * 128], src[c * 128:(c + 1) * 128, f0 * 128:(f0 + nf) * 128], [],
                              [f"stg{i}"], f"stg{i}")
                        P.copy(POOL, dst[:, c, 0:nf * 128], stg[i][:, 0:nf * 128], [f"stg{i}"], [key])
                for fc in range(0, nf, 2):
                    n2 = min(2, nf - fc)
                    i = cnt[0] % 3
                    cnt[0] += 1
                    src = wd_of(e)[(f0 + fc) * 128:(f0 + fc + n2) * 128, :].rearrange("(a p) d -> p a d", p=128)
                    P.dma(SP, stg[i][:, 0:n2 * D].rearrange("p (a d) -> p a d", a=n2), src, [], [f"stg{i}"], f"stg{i}")
                    P.copy(POOL, Wd[wb][:, fc:fc + n2, :], stg[i][:, 0:n2 * D].rearrange("p (a d) -> p a d", a=n2),
                           [f"stg{i}"], [f"Wd{wb}"])
                for q4 in range(SBT // 4):
                    hb = nh % 2
                    nh += 1
                    for fc in range(nf):
                        gi = ngu % 2
                        ngu += 1
                        for c in range(8):
                            P.mm(pg[gi][:], Wg[wb][:, c, fc * 128:(fc + 1) * 128], xTs[:, c, q4 * 512:(q4 + 1) * 512],
                                 c == 0, c == 7, [f"Wg{wb}", ("xTs", q4)], [f"pg{gi}"])
                        for c in range(8):
                            P.mm(pu[gi][:], Wu[wb][:, c, fc * 128:(fc + 1) * 128], xTs[:, c, q4 * 512:(q4 + 1) * 512],
                                 c == 0, c == 7, [f"Wu{wb}", ("xTs", q4)], [f"pu{gi}"])
                        P.act(sg[gi][:], pg[gi][:], AF.Silu, [f"pg{gi}"], [f"sg{gi}"])
                        P.tt(DVE, hT[hb][:, fc, :], sg[gi][:], pu[gi][:], ALU.mult, [f"sg{gi}", f"pu{gi}"], [(f"hT{hb}", fc)])
                    for tt in range(4):
                        tl = q4 * 4 + tt
                        yb = (ny % 2) * 2
                        ny += 1
                        for hf in range(2):
                            for fc in range(nf):
                                P.mm(py[yb + hf][:], hT[hb][:, fc, tt * 128:(tt + 1) * 128], Wd[wb][:, fc, hf * 512:(hf + 1) * 512],
                                     fc == 0, fc == nf - 1, [(f"hT{hb}", fc), f"Wd{wb}"], [f"py{yb + hf}"])
                            dst = acc[:, tl, hf * 512:(hf + 1) * 512]
                            akey = ("acc", tl, hf)
                            if moe:
                                gsc = C.gate_sb[:, t0 + tl, e:e + 1]
                                if first[tl]:
                                    P.ts(DVE, dst, py[yb + hf][:], gsc, None, ALU.mult, ALU.bypass, [f"py{yb + hf}", ("gate", t0 + tl)], [akey])
                                else:
                                    P.stt(dst, py[yb + hf][:], gsc, dst, ALU.mult, ALU.add, [f"py{yb + hf}", akey, ("gate", t0 + tl)], [akey])
                            else:
                                if first[tl]:
                                    P.copy(DVE, dst, py[yb + hf][:], [f"py{yb + hf}"], [akey])
                                else:
                                    P.tt(DVE, dst, py[yb + hf][:], dst, ALU.add, [f"py{yb + hf}", akey], [akey])
                        first[tl] = False
        for tl in range(0 if dbg_noln else SBT):
            t = t0 + tl
            ptv = py[tl % 4][:].bitcast(BF16).rearrange("p (c t) -> p c t", c=8)
            ln_epilogue(P, C, t, [acc[:, tl, 0:512], acc[:, tl, 512:1024]], [("acc", tl, 0), ("acc", tl, 1)], C.xres, gb, ident,
                        bufs, ptv, f"py{tl % 4}", out_dst=(C.out if last else None))
    return P.emit()


def bc(ap, shape):
    return ap.to_broadcast(list(shape))


def phase_dn(C, layer):
    nc = C.nc
    P = Phase(nc, f"dn{layer}")
    identb = P.sb("identb", [128, 128], BF16)
    identf = P.sb("identf", [128, 128], F32)
    onesf = P.sb("onesf", [128, 128], F32)
    trif = P.sb("trif", [128, 128], F32)
    mcaus = P.sb("mcaus", [128, 128], F32)
    mneg = P.sb("mneg", [128, 128], F32)
    cw = P.sb("cw", [128, 4, 1536], F32)
    alog = P.sb("alog", [128, 8], F32)
    dtb = P.sb("dtb", [128, 8], F32)
    wn = P.sb("wn", [128, 64], F32)
    for i, (dst, src, key) in enumerate([(identb, C.ident_bf, "identb"), (identf, C.ident_f32, "identf"), (onesf, C.ones_f32, "onesf"),
                                         (trif, C.tri_f32, "trif"), (mcaus, C.mcausT, "mcaus"), (mneg, C.mnegT, "mneg"),
                                         (alog, C.a_log_bc[layer], "alog"), (dtb, C.dt_bias_bc[layer], "dtb"),
                                         (wn, C.dn_norm_bc[layer], "wn")]):
        P.dma(SP, dst[:], src, [], [key], f"c{i}")
    P.dma(SP, cw[:], C.conv_w_bc[layer].rearrange("p (j c) -> p j c", j=4), [], ["cw"], "c_cw")

    def g8(name):
        return P.sb(name, [128, NT, 8], F32)
    x8, gg8, beta8, gc8, glb8, eg8 = [g8(n) for n in ("x8", "gg8", "beta8", "gc8", "glb8", "eg8")]
    kd8, negeg8, egl8 = x8, gg8, glb8
    P.alias = {"kd8": "x8", "negeg8": "gg8", "egl8": "glb8"}
    eglS = P.sb("eglS", [128, NT, 4], F32)
    negA = P.sb("negA", [128, 8], F32)
    pp = [P.ps(f"pp{i}", [128, 512], F32) for i in range(4)]
    rA, rB, rC, rD = [P.ps(f"r{n}", [128, 512], F32) for n in "ABCD"]
    P.excl.update(["pp0", "pp1", "pp2", "pp3", "rK0", "rK1", "rC", "rD"])
    P.act(negA[:], alog[:], AF.Exp, ["alog"], ["negA"])
    P.ts(DVE, negA[:], negA[:], -1.0, None, ALU.mult, ALU.bypass, ["negA"], ["negA"])
    for h in range(8):
        P.ts(DVE, x8[:, :, h], C.ab_sb[:, :, h], dtb[:, h:h + 1], None, ALU.add, ALU.bypass, ["dtb"], ["x8"])
    P.act(x8[:], x8[:], AF.Exp, ["x8"], ["x8"])
    P.act(x8[:], x8[:], AF.Ln, ["x8"], ["x8"], bias=1.0)
    for h in range(8):
        P.ts(DVE, gg8[:, :, h], x8[:, :, h], negA[:, h:h + 1], None, ALU.mult, ALU.bypass, ["x8", "negA"], ["gg8"])
    P.act(beta8[:], C.ab_sb[:, :, 8:16], AF.Exp, [], ["beta8"], scale=-1.0)
    P.ts(DVE, beta8[:], beta8[:], 1.0, None, ALU.add, ALU.bypass, ["beta8"], ["beta8"])
    P.op(DVE, lambda e: e.reciprocal(beta8[:], beta8[:]), ["beta8"], ["beta8"])
    ggf = gg8[:].rearrange("p t h -> p (t h)")
    P.mm(pp[0][:], trif[:], ggf, True, True, ["trif", "gg8"], ["pp0"])
    P.mm(pp[1][:], onesf[:], ggf, True, True, ["onesf", "gg8"], ["pp1"])
    P.copy(DVE, gc8[:].rearrange("p t h -> p (t h)"), pp[0][:], ["pp0"], ["gc8"])
    P.copy(DVE, glb8[:].rearrange("p t h -> p (t h)"), pp[1][:], ["pp1"], ["glb8"])
    P.act(eg8[:], gc8[:], AF.Exp, ["gc8"], ["eg8"])
    P.ts(DVE, negeg8[:], eg8[:], -1.0, None, ALU.mult, ALU.bypass, ["eg8"], ["negeg8"])
    P.tt(DVE, kd8[:], glb8[:], gc8[:], ALU.subtract, ["glb8", "gc8"], ["kd8"])
    P.act(kd8[:], kd8[:], AF.Exp, ["kd8"], ["kd8"])
    P.act(egl8[:], glb8[:], AF.Exp, ["glb8"], ["egl8"])
    for par in range(2):
        P.copy(DVE, eglS[par * 64:(par + 1) * 64, :, :], egl8[par * 64:(par + 1) * 64, :, par::2], ["egl8"], ["eglS"])

    cv = [P.sb(f"cv{i}", [128, 4, 1536], F32) for i in range(2)]
    act = [P.sb(f"act{i}", [128, 1536], F32) for i in range(2)]
    ss = [P.sb(f"ss{i}", [128, 16], F32) for i in range(2)]
    qkb = [P.sb(f"qkb{i}", [128, 1024], BF16) for i in range(2)]
    dg = [P.sb(f"dg{i}", [128, 4, 128], F32) for i in range(2)]
    dsb = [P.sb(f"dsb{i}", [128, 4, 128], F32) for i in range(2)]
    DTs = [P.sb(f"DTs{i}", [128, 4, 128], F32) for i in range(2)]
    DTc = [P.sb(f"DTc{i}", [128, 4, 128], F32) for i in range(2)]
    Mb = [[P.sb(f"M{i}_{k}", [128, 8, 128], BF16) for k in range(2)] for i in range(2)]
    MTb = [[P.sb(f"MT{i}_{k}", [128, 8, 128], BF16) for k in range(2)] for i in range(2)]
    PTf = [P.sb(f"PTf{i}", [128, 8, 128], F32) for i in range(2)]
    PTw = [P.sb(f"PTw{i}", [128, 8, 128], BF16) for i in range(2)]
    qkT = [P.sb(f"qkT{i}", [128, 8, 128], BF16) for i in range(3)]
    PTb = [P.sb(f"PTb{i}", [128, 8, 128], BF16) for i in range(3)]
    inT = [P.sb(f"inT{i}", [128, 8, 128], BF16) for i in range(3)]
    v_f = [P.sb(f"v_f{i}", [128, 512], F32) for i in range(3)]
    kdec = [P.sb(f"kdec{i}", [128, 512], BF16) for i in range(3)]
    zt = [P.sb(f"zt{i}", [128, 512], F32) for i in range(3)]
    o_f = [P.sb(f"o_f{i}", [128, 512], F32) for i in range(3)]
    Sf = P.sb("Sf", [128, 4, 64], F32)
    Sb = P.sb("Sb", [128, 4, 64], BF16)
    tmp = P.sb("tmp", [128, 8, 64], F32)
    rp = P.sb("rp", [128, 8, 64], BF16)
    vn = P.sb("vn", [128, 8, 64], BF16)
    o2 = [P.sb(f"o2{i}", [128, 512], F32) for i in range(2)]
    ssn = [P.sb(f"ssn{i}", [128, 8], F32) for i in range(2)]
    o_bf = [P.sb(f"o_bf{i}", [128, 512], BF16) for i in range(2)]
    oT = [P.sb(f"oT{i}", [128, 4, 128], BF16) for i in range(2)]
    P.op(DVE, lambda e: e.memset(Sf[:], 0.0), [], ["Sf"])
    P.op(DVE, lambda e: e.memset(Sb[:], 0.0), [], ["Sb"])
    pp_free = [0, 1, 2, 3]

    def acquire(n):
        while len(pp_free) < n:
            yield
        return [pp_free.pop(0) for _ in range(n)]

    def release(*idx):
        pp_free.extend(idx)

    def hp(h):
        return (h % 2) * 64

    def prep(t):
        pb = t % 2
        hb = t % 3
        K = lambda s: f"{s}{pb}"
        H = lambda s: f"{s}{hb}"
        src = bass.AP(tensor=C.qkvpre.tensor, offset=t * 128 * 1536, ap=[[1536, 128], [1536, 4], [1, 1536]])
        P.dma(SP, cv[pb][:], src, [("qkvpre", t)], [K("cv")], K("cv"))
        yield
        P.tt(POOL, cv[pb][:], cv[pb][:], cw[:], ALU.mult, [K("cv"), "cw"], [K("cv")])
        yield
        P.tt(DVE, cv[pb][:, 0:2, :], cv[pb][:, 0:2, :], cv[pb][:, 2:4, :], ALU.add, [K("cv")], [K("cv")])
        P.tt(DVE, act[pb][:], cv[pb][:, 0, :], cv[pb][:, 1, :], ALU.add, [K("cv")], [K("act")])
        yield
        P.act(act[pb][:], act[pb][:], AF.Silu, [K("act")], [K("act")])
        yield
        sqv = cv[pb][:, 2, 0:1024]
        P.tt(POOL, sqv, act[pb][:, 0:1024], act[pb][:, 0:1024], ALU.mult, [K("act")], [K("cv")])
        P.copy(POOL, v_f[hb][:], act[pb][:, 1024:1536], [K("act")], [H("v_f")])
        yield
        P.op(DVE, lambda e: e.tensor_reduce(ss[pb][:], sqv.rearrange("p (h d) -> p h d", h=16), AX.X, ALU.add),
             [K("cv")], [K("ss")])
        P.ts(DVE, ss[pb][:], ss[pb][:], 1e-6, None, ALU.add, ALU.bypass, [K("ss")], [K("ss")])
        yield
        P.act(ss[pb][:], ss[pb][:], AF.Ln, [K("ss")], [K("ss")])
        P.act(ss[pb][:], ss[pb][:], AF.Exp, [K("ss")], [K("ss")], scale=-0.5)
        yield
        P.ts(DVE, ss[pb][:, 0:8], ss[pb][:, 0:8], 0.125, None, ALU.mult, ALU.bypass, [K("ss")], [K("ss")])
        P.tt(DVE, qkb[pb][:].rearrange("p (h d) -> p h d", h=16), act[pb][:, 0:1024].rearrange("p (h d) -> p h d", h=16),
             bc(ss[pb][:].unsqueeze(2), [128, 16, 64]), ALU.mult, [K("act"), K("ss")], [K("qkb")])
        yield
        P.tt(POOL, kdec[hb][:].rearrange("p (h d) -> p h d", h=8), qkb[pb][:, 512:1024].rearrange("p (h d) -> p h d", h=8),
             bc(kd8[:, t, :].unsqueeze(2), [128, 8, 64]), ALU.mult, [K("qkb"), "kd8"], [H("kdec")])
        (pi,) = yield from acquire(1)
        ptv = pp[pi][:].bitcast(BF16).rearrange("p (a t) -> p a t", a=8)
        for a in range(8):
            P.tr(ptv[:, a, :], qkb[pb][:, a * 128:(a + 1) * 128], identb[:], [K("qkb"), "identb"], [f"pp{pi}"])
        yield
        P.copy(ACT, qkT[hb][:], ptv, [f"pp{pi}"], [H("qkT")])
        release(pi)
        yield
        for hg in range(2):
            iG, iQ, iR = yield from acquire(3)
            P.tt(POOL, dg[pb][:], bc(identf[:].unsqueeze(1), [128, 4, 128]),
                 bc(gc8[:, t, hg::2].unsqueeze(2), [128, 4, 128]), ALU.mult, ["identf", "gc8"], [K("dg")])
            for h4 in range(4):
                h = 2 * h4 + hg
                kT_h = qkT[hb][hp(h):hp(h) + 64, 4 + h // 2, :]
                qT_h = qkT[hb][hp(h):hp(h) + 64, h // 2, :]
                P.mm(pp[iG][:, h4 * 128:(h4 + 1) * 128], kT_h, kT_h, True, True, [H("qkT")], [f"pp{iG}"])
                P.mm(pp[iQ][:, h4 * 128:(h4 + 1) * 128], kT_h, qT_h, True, True, [H("qkT")], [f"pp{iQ}"])
            P.mm(pp[iR][:], onesf[:], dg[pb][:].rearrange("p h i -> p (h i)"), True, True,
                 ["onesf", K("dg")], [f"pp{iR}"])
            yield
            for h4 in range(4):
                h = 2 * h4 + hg
                P.ts(DVE, dsb[pb][:, h4, :], pp[iR][:, h4 * 128:(h4 + 1) * 128], gc8[:, t, h:h + 1], 0.0,
                     ALU.subtract, ALU.min, [f"pp{iR}", "gc8"], [K("dsb")])
            release(iR)
            yield
            hs = slice(hg * 4, (hg + 1) * 4)
            P.act(dsb[pb][:], dsb[pb][:], AF.Exp, [K("dsb")], [K("dsb")])
            yield
            P.tt(POOL, DTs[pb][:], dsb[pb][:], bc(mneg[:].unsqueeze(1), [128, 4, 128]), ALU.mult,
                 [K("dsb"), "mneg"], [K("DTs")])
            P.tt(POOL, DTc[pb][:], dsb[pb][:], bc(mcaus[:].unsqueeze(1), [128, 4, 128]), ALU.mult,
                 [K("dsb"), "mcaus"], [K("DTc")])
            yield
            for h4 in range(4):
                h = 2 * h4 + hg
                P.stt(MTb[pb][0][:, hg * 4 + h4, :], pp[iG][:, h4 * 128:(h4 + 1) * 128], beta8[:, t, h:h + 1],
                      DTs[pb][:, h4, :], ALU.mult, ALU.mult, [f"pp{iG}", "beta8", K("DTs")],
                      [K("MT") + f"0_{hg}"])
            P.tt(DVE, inT[hb][:, hs, :], pp[iQ][:].rearrange("p (h i) -> p h i", h=4), DTc[pb][:], ALU.mult,
                 [f"pp{iQ}", K("DTc")], [H("inT") + f"_{hg}"])
            release(iG, iQ)
            yield
        (pi,) = yield from acquire(1)
        ptv = pp[pi][:].bitcast(BF16).rearrange("p (a t) -> p a t", a=8)
        for h in range(8):
            P.tr(ptv[:, h, :], MTb[pb][0][:, h, :], identb[:], [K("MT") + f"0_{h // 4}", "identb"], [f"pp{pi}"])
        P.tt(POOL, PTf[pb][:], MTb[pb][0][:], bc(identf[:].unsqueeze(1), [128, 8, 128]), ALU.add,
             [K("MT") + "0_0", K("MT") + "0_1", "identf"], [K("PTf") + "_0", K("PTf") + "_1"])
        P.tt(POOL, PTw[pb][:], MTb[pb][0][:], bc(identf[:].unsqueeze(1), [128, 8, 128]), ALU.add,
             [K("MT") + "0_0", K("MT") + "0_1", "identf"], [K("PTw") + "_0", K("PTw") + "_1"])
        yield
        P.copy(ACT, Mb[pb][0][:, 0:4, :], ptv[:, 0:4, :], [f"pp{pi}"], [K("M") + "0_0"])
        P.copy(DVE, Mb[pb][0][:, 4:8, :], ptv[:, 4:8, :], [f"pp{pi}"], [K("M") + "0_1"])
        release(pi)
        yield
        for s in range(1, 7):
            cur, prv = s % 2, (s - 1) % 2
            for hg in range(2):
                hs = slice(hg * 4, (hg + 1) * 4)
                kM, kMT = K("M") + f"{cur}_{hg}", K("MT") + f"{cur}_{hg}"
                kMp, kMTp = K("M") + f"{prv}_{hg}", K("MT") + f"{prv}_{hg}"
                if s <= 5:
                    iM, iT = yield from acquire(2)
                else:
                    (iM,) = yield from acquire(1)
                for h4 in range(4):
                    h = hg * 4 + h4
                    P.mm(pp[iM][:, h4 * 128:(h4 + 1) * 128], MTb[pb][prv][:, h, :], Mb[pb][prv][:, h, :], True, True,
                         [kMp, kMTp], [f"pp{iM}"])
                if s <= 5:
                    for h4 in range(4):
                        h = hg * 4 + h4
                        P.mm(pp[iT][:, h4 * 128:(h4 + 1) * 128], Mb[pb][prv][:, h, :], MTb[pb][prv][:, h, :], True, True,
                             [kMp, kMTp], [f"pp{iT}"])
                yield
                P.copy(ACT, Mb[pb][cur][:, hs, :], pp[iM][:].rearrange("p (h i) -> p h i", h=4), [f"pp{iM}"], [kM])
                if s <= 5:
                    P.copy(DVE, MTb[pb][cur][:, hs, :], pp[iT][:].rearrange("p (h i) -> p h i", h=4), [f"pp{iT}"], [kMT])
                    release(iT)
                release(iM)
                yield
                (iA,) = yield from acquire(1)
                for h4 in range(4):
                    h = hg * 4 + h4
                    P.mm(pp[iA][:, h4 * 128:(h4 + 1) * 128], Mb[pb][cur][:, h, :], PTw[pb][:, h, :], True, True,
                         [kM, K("PTw") + f"_{hg}"], [f"pp{iA}"])
                yield
                P.tt(DVE, PTf[pb][:, hs, :], PTf[pb][:, hs, :], pp[iA][:].rearrange("p (h i) -> p h i", h=4), ALU.add,
                     [K("PTf") + f"_{hg}", f"pp{iA}"], [K("PTf") + f"_{hg}"])
                release(iA)
                yield
                if s < 6:
                    P.copy(ACT, PTw[pb][:, hs, :], PTf[pb][:, hs, :], [K("PTf") + f"_{hg}"], [K("PTw") + f"_{hg}"])
                else:
                    P.copy(ACT, PTb[hb][:, hs, :], PTf[pb][:, hs, :], [K("PTf") + f"_{hg}"], [H("PTb") + f"_{hg}"])
                yield

    def hi(h):
        return (h % 2) * 4 + h // 2

    def recur(t):
        hb = t % 3
        H = lambda s: f"{s}{hb}"
        rK = [rA, rB]
        rCv = rC[:].rearrange("p (h d) -> p h d", h=8)
        rDv = rD[:, 0:256].rearrange("p (a d) -> p a d", a=4)
        tmp4 = tmp[:].rearrange("p (a q) d -> p a q d", q=2)
        for h in range(8):
            par, a = h % 2, h // 2
            kT_h = qkT[hb][hp(h):hp(h) + 64, 4 + a, :]
            qT_h = qkT[hb][hp(h):hp(h) + 64, a, :]
            S_h = Sb[hp(h):hp(h) + 64, a, :]
            P.mm(rK[par][:, a * 64:(a + 1) * 64], kT_h, S_h, True, True, [H("qkT"), "Sb"], [f"rK{par}"])
            P.mm(rK[par][:, 256 + a * 64:256 + (a + 1) * 64], qT_h, S_h, True, True, [H("qkT"), "Sb"], [f"rK{par}"])
        yield
        for par in range(2):
            P.tt(DVE, tmp4[:, :, par, :], rK[par][:, 0:256].rearrange("p (a d) -> p a d", a=4),
                 bc(negeg8[:, t, par::2].unsqueeze(2), [128, 4, 64]), ALU.mult, [f"rK{par}", "negeg8"], ["tmp"])
        P.tt(DVE, rp[:], tmp[:], v_f[hb][:].rearrange("p (h d) -> p h d", h=8), ALU.add, ["tmp", H("v_f")], ["rp"])
        yield
        for h in range(8):
            P.mm(rCv[:, h, :], PTb[hb][:, hi(h), :], rp[:, h, :], True, True, [H("PTb") + f"_{h % 2}", "rp"], ["rC"])
        yield
        P.tt(DVE, vn[:], rCv, bc(beta8[:, t, :].unsqueeze(2), [128, 8, 64]), ALU.mult, ["rC", "beta8"], ["vn"])
        yield
        for h in range(8):
            P.mm(rDv[hp(h):hp(h) + 64, h // 2, :], kdec[hb][:, h * 64:(h + 1) * 64], vn[:, h, :], True, True,
                 [H("kdec"), "vn"], ["rD"])
        for h in range(8):
            P.mm(rCv[:, h, :], inT[hb][:, hi(h), :], vn[:, h, :], True, True, [H("inT") + f"_{h % 2}", "vn"], ["rC"])
        yield
        P.tt(DVE, Sf[:], Sf[:], bc(eglS[:, t, :].unsqueeze(2), [128, 4, 64]), ALU.mult, ["Sf", "eglS"], ["Sf"])
        P.tt(DVE, Sb[:], Sf[:], rDv, ALU.add, ["Sf", "rD"], ["Sb"])
        P.tt(DVE, Sf[:], Sf[:], rDv, ALU.add, ["Sf", "rD"], ["Sf"])
        for par in range(2):
            P.tt(DVE, tmp4[:, :, par, :], rK[par][:, 256:512].rearrange("p (a d) -> p a d", a=4),
                 bc(eg8[:, t, par::2].unsqueeze(2), [128, 4, 64]), ALU.mult, [f"rK{par}", "eg8"], ["tmp"])
        P.tt(DVE, o_f[hb][:].rearrange("p (h d) -> p h d", h=8), tmp[:], rCv, ALU.add, ["tmp", "rC"], [H("o_f")])
        yield

    def epi(t):
        hb = t % 3
        H = lambda s: f"{s}{hb}"
        ob = t % 2
        E = lambda s: f"{s}{ob}"
        P.dma(SP, zt[hb][:], C.zbuf[t * 128:(t + 1) * 128, :], [], [H("zt")], H("zt"))
        P.tt(POOL, o2[ob][:], o_f[hb][:], o_f[hb][:], ALU.mult, [H("o_f")], [E("o2")])
        yield
        P.act(zt[hb][:], zt[hb][:], AF.Silu, [H("zt")], [H("zt")])
        P.op(DVE, lambda e: e.tensor_reduce(ssn[ob][:], o2[ob][:].rearrange("p (h d) -> p h d", h=8), AX.X, ALU.add),
             [E("o2")], [E("ssn")])
        P.ts(DVE, ssn[ob][:], ssn[ob][:], 1.0 / 64.0, 1e-6, ALU.mult, ALU.add, [E("ssn")], [E("ssn")])
        yield
        P.act(ssn[ob][:], ssn[ob][:], AF.Ln, [E("ssn")], [E("ssn")])
        P.act(ssn[ob][:], ssn[ob][:], AF.Exp, [E("ssn")], [E("ssn")], scale=-0.5)
        yield
        P.tt(POOL, o2[ob][:].rearrange("p (h d) -> p h d", h=8), o_f[hb][:].rearrange("p (h d) -> p h d", h=8),
             bc(ssn[ob][:].unsqueeze(2), [128, 8, 64]), ALU.mult, [H("o_f"), E("ssn")], [E("o2")])
        yield
        P.tt(POOL, o2[ob][:].rearrange("p (h d) -> p h d", h=8), o2[ob][:].rearrange("p (h d) -> p h d", h=8),
             bc(wn[:].unsqueeze(1), [128, 8, 64]), ALU.mult, [E("o2"), "wn"], [E("o2")])
        yield
        P.tt(POOL, o_bf[ob][:], o2[ob][:], zt[hb][:], ALU.mult, [E("o2"), H("zt")], [E("o_bf")])
        yield
        (pi,) = yield from acquire(1)
        ptv = pp[pi][:].bitcast(BF16).rearrange("p (a t) -> p a t", a=8)
        for a in range(4):
            P.tr(ptv[:, a, :], o_bf[ob][:, a * 128:(a + 1) * 128], identb[:], [E("o_bf"), "identb"], [f"pp{pi}"])
        yield
        P.copy(ACT, oT[ob][:], ptv[:, 0:4, :], [f"pp{pi}"], [f"oT{ob}"])
        release(pi)
        dst = C.catT[0:512, t * 128:(t + 1) * 128].rearrange("(a p) t -> p a t", p=128)
        P.dma(POOL, dst, oT[ob][:], [f"oT{ob}"], [("catT_dn", t)], f"oT{ob}")
        yield

    preps = {}
    epis = []
    prep_done = set()
    next_prep = 0
    rec_t = 0
    rec_gen = None
    NTD = C.dn_tiles
    while rec_t < NTD or epis or preps:
        while next_prep < NTD and next_prep <= rec_t + 2 and len(preps) < 2:
            preps[next_prep] = prep(next_prep)
            next_prep += 1
        for tt_ in list(preps):
            try:
                C.dbg_stage = getattr(C, "dbg_stage", 0) + 1
                if C.dbg_stage > getattr(C, "dbg_maxstage", 10 ** 9):
                    raise StopIteration
                next(preps[tt_])
            except StopIteration:
                prep_done.add(tt_)
                del preps[tt_]
        if getattr(C, "dbg_norec", False) and not preps:
            break
        if rec_gen is None and rec_t < NTD and rec_t in prep_done:
            rec_gen = recur(rec_t)
        if rec_gen is not None:
            try:
                next(rec_gen)
            except StopIteration:
                rec_gen = None
                epis.append(epi(rec_t))
                rec_t += 1
        for g in list(epis):
            try:
                next(g)
            except StopIteration:
                epis.remove(g)
    if getattr(C, "dn_dump", None):
        dumps = {"d_qkT": (qkT[0], ["qkT0"]), "d_kdec": (kdec[0], ["kdec0"]), "d_vf": (v_f[0], ["v_f0"]),
                 "d_inT": (inT[0], ["inT0_0", "inT0_1"]), "d_PTb": (PTb[0], ["PTb0_0", "PTb0_1"]),
                 "d_MT0": (MTb[0][0], ["MT00_0", "MT00_1"]), "d_M0": (Mb[0][0], ["M00_0", "M00_1"]),
                 "d_gc8": (gc8, ["gc8"]), "d_beta8": (beta8, ["beta8"]), "d_eg8": (eg8, ["eg8"]), "d_Sf": (Sf, ["Sf"]),
                 "d_of": (o_f[0], ["o_f0"]), "d_gg8": (gg8, ["gg8"]), "d_kd8": (kd8, ["kd8"]), "d_glb8": (glb8, ["glb8"]),
                 "d_act": (act[0], ["act0"]), "d_qkb": (qkb[0], ["qkb0"]), "d_PTf": (PTf[0], ["PTf0_0", "PTf0_1"]),
                 "d_vn": (vn, ["vn"]), "d_rp": (rp, ["rp"])}
        for nm, (tile, keys) in dumps.items():
            if nm in C.dn_dump:
                P.dma(SP, C.dn_dump[nm], tile[:], keys, [nm], nm)
    return P.emit()


INPUT_SHAPES = {
    "x": ([S, D], F32), "w_in": ([DEPTH, D, IN_W], F32), "w_out": ([DEPTH, D, D], F32),
    "ffn_w_gate": ([2, D, DFF], F32), "ffn_w_up": ([2, D, DFF], F32), "ffn_w_down": ([2, DFF, D], F32),
    "moe_w_gate": ([2, NE, D, DFE], F32), "moe_w_up": ([2, NE, D, DFE], F32), "moe_w_down": ([2, NE, DFE, D], F32),
    "ident_bf": ([128, 128], BF16), "ident_f32": ([128, 128], F32), "ones_f32": ([128, 128], F32),
    "tri_f32": ([128, 128], F32), "mcausT": ([128, 128], F32), "mnegT": ([128, 128], F32), "maskneg": ([128, 128], F32),
    "conv_w_bc": ([DEPTH, 128, 6144], F32), "a_log_bc": ([DEPTH, 128, 8], F32), "dt_bias_bc": ([DEPTH, 128, 8], F32),
    "dn_norm_bc": ([DEPTH, 128, 64], F32), "df_lambda_bc": ([DEPTH, 128, 256], F32), "df_subln_col": ([DEPTH, 128, 1], F32),
    "ln1_g_bc": ([DEPTH, 128, D], F32), "ln1_b_bc": ([DEPTH, 128, D], F32), "ln2_g_bc": ([DEPTH, 128, D], F32),
    "ln2_b_bc": ([DEPTH, 128, D], F32), "router_bc": ([2, 128, NE, D], F32),
    "bias_tiles": ([4, 2, 128, 128], F32), "cfar": ([128, 4], F32),
}


DUMP_SHAPES = {"d_qkT": ([128, 8, 128], BF16), "d_kdec": ([128, 512], BF16), "d_vf": ([128, 512], F32),
               "d_inT": ([128, 8, 128], BF16), "d_PTb": ([128, 8, 128], BF16), "d_MT0": ([128, 8, 128], BF16),
               "d_M0": ([128, 8, 128], BF16), "d_gc8": ([128, NT, 8], F32), "d_beta8": ([128, NT, 8], F32),
               "d_eg8": ([128, NT, 8], F32), "d_Sf": ([128, 4, 64], F32), "d_of": ([128, 512], F32),
               "d_gg8": ([128, NT, 8], F32), "d_kd8": ([128, NT, 8], F32), "d_glb8": ([128, NT, 8], F32),
               "d_act": ([128, 1536], F32), "d_qkb": ([128, 1024], BF16), "d_PTf": ([128, 8, 128], F32),
               "d_vn": ([128, 8, 64], BF16), "d_rp": ([128, 8, 64], BF16)}


def build(n_layers=DEPTH, debug=(), stop_after=None, skip_inputs=(), dn_tiles=NT, only=None):
    nc = bass.Bass("TRN2", target_bir_lowering=False)
    C = Ctx()
    C.nc = nc
    C.debug = set(debug)
    C.dn_tiles = dn_tiles
    import os as _os
    C.dbg_maxstage = int(_os.environ.get('DN_MAXSTAGE', 10 ** 9))
    C.dbg_norec = bool(int(_os.environ.get('DN_NOREC', '0')))
    for name, (shape, dt) in INPUT_SHAPES.items():
        if name in skip_inputs:
            continue
        setattr(C, name, nc.dram_tensor(name, list(shape), dt, kind="ExternalInput").ap())

    def dscr(name, shape, dt):
        kind = "ExternalOutput" if name in C.debug else "Internal"
        return nc.dram_tensor(name, list(shape), dt, kind=kind).ap()

    C.out = nc.dram_tensor("out", [S, D], F32, kind="ExternalOutput").ap()
    C.xT = dscr("xT", [D, S], BF16)
    C.xres = dscr("xres", [S, D], F32)
    C.qkvpre = dscr("qkvpre", [S + 3, 1536], F32)
    C.zbuf = dscr("zbuf", [S, 512], F32)
    C.Vdf = dscr("Vdf", [S, 512], BF16)
    C.catT = dscr("catT", [D, S], BF16)
    C.QT = [dscr(f"QT{h}", [128, S], BF16) for h in range(4)]
    C.KT = [dscr(f"KT{h}", [128, S], BF16) for h in range(4)]
    C.dn_dump = {}
    for nm in C.debug:
        if nm.startswith("d_"):
            shp, dt = DUMP_SHAPES[nm]
            C.dn_dump[nm] = nc.dram_tensor(nm, list(shp), dt, kind="ExternalOutput").ap()
    gstack = ExitStack()
    C.ab_sb = gstack.enter_context(nc.sbuf_tensor("ab_sb", [128, NT, 16], F32))
    C.gate_sb = gstack.enter_context(nc.sbuf_tensor("gate_sb", [128, NT, NE], F32))
    stats = {}
    C.stats = stats
    P = Phase(nc, "z0")
    zt = P.sb("zt", [3, 1536], F32)
    P.op(DVE, lambda e: e.memset(zt[:], 0.0), [], ["zt"])
    P.dma(SP, C.qkvpre[0:3, :], zt[:], ["zt"], ["pad"], "zt")
    P.emit()
    if only is None or "x0" in only:
        stats["x0"] = phase_x0(C)
    done = False
    for layer in range(n_layers):
        for nm, fn in (("ip", phase_inproj), ("dn", phase_dn), ("at", phase_attn), ("op", phase_outproj)):
            if only is None or f"{nm}{layer}" in only:
                stats[f"{nm}{layer}"] = fn(C, layer)
            if stop_after == f"{nm}{layer}":
                done = True
                break
        if done:
            break
        if only is None or f"ff{layer}" in only:
            stats[f"ff{layer}"] = phase_ffn(C, layer, layer == n_layers - 1)
        if stop_after == f"ff{layer}":
            break
    gstack.close()
    return nc, C


def t5_bucket_np(dist):
    dist = np.asarray(dist, dtype=np.int64)
    d = np.maximum(dist, 1).astype(np.float32)
    large = 16 + (np.log(d / np.float32(16.0)) / np.float32(math.log(128 / 16)) * np.float32(16.0)).astype(np.int32)
    large = np.minimum(large, 31)
    return np.where(dist < 16, dist, large)


def host_inputs(inputs):
    f32 = np.float32
    ii = np.arange(128)
    m = {}
    m["ident_bf"] = np.eye(128, dtype=f32).astype(ml_dtypes.bfloat16)
    m["ident_f32"] = np.eye(128, dtype=f32)
    m["ones_f32"] = np.ones((128, 128), f32)
    m["tri_f32"] = (ii[:, None] <= ii[None, :]).astype(f32)
    m["mcausT"] = (ii[None, :] >= ii[:, None]).astype(f32)
    m["mnegT"] = -(ii[None, :] > ii[:, None]).astype(f32)
    m["maskneg"] = np.where(ii[None, :] >= ii[:, None], 0.0, -1e5).astype(f32)

    def bc128(a):
        a = np.asarray(a, dtype=f32)
        return np.ascontiguousarray(np.broadcast_to(a[:, None, :], (a.shape[0], 128, a.shape[1])))

    m["conv_w_bc"] = bc128(np.asarray(inputs["conv_w"]).reshape(DEPTH, 4 * 1536))
    m["a_log_bc"] = bc128(inputs["dn_a_log"])
    m["dt_bias_bc"] = bc128(inputs["dn_dt_bias"])
    m["dn_norm_bc"] = bc128(inputs["dn_norm_w"])
    m["df_lambda_bc"] = bc128(np.asarray(inputs["df_lambda"]).reshape(DEPTH, 256))
    m["df_subln_col"] = np.ascontiguousarray(np.asarray(inputs["df_subln_w"], dtype=f32).reshape(DEPTH, 128, 1))
    for k in ("ln1_g", "ln1_b", "ln2_g", "ln2_b"):
        m[k + "_bc"] = bc128(inputs[k])
    r = np.asarray(inputs["moe_router"], dtype=f32).transpose(0, 2, 1)
    m["router_bc"] = np.ascontiguousarray(np.broadcast_to(r[:, None, :, :], (2, 128, NE, D)))
    rb = np.asarray(inputs["rel_bias"], dtype=f32)
    bt = np.zeros((4, 2, 128, 128), f32)
    for rel in range(2):
        dist = rel * 128 + ii[None, :] - ii[:, None]
        bk = t5_bucket_np(np.maximum(dist, 0))
        g = rb[bk]
        g = np.where((dist >= 0)[:, :, None], g, 0.0)
        bt[:, rel] = g.transpose(2, 0, 1)
    m["bias_tiles"] = bt
    m["cfar"] = np.ascontiguousarray(np.broadcast_to(rb[31][None, :], (128, 4)))
    for k in ("w_in", "w_out", "ffn_w_gate", "ffn_w_up", "ffn_w_down", "moe_w_gate", "moe_w_up", "moe_w_down"):
        m[k] = np.ascontiguousarray(np.asarray(inputs[k], dtype=f32))
    return m


def kernel(**inputs):
    nc, C = build()
    shared = host_inputs(inputs)
    in_maps = []
    for b in range(8):
        m = dict(shared)
        m["x"] = np.ascontiguousarray(np.asarray(inputs["x"][b], dtype=np.float32))
        in_maps.append(m)
    res = run_bass_kernel_spmd(nc, in_maps, core_ids=list(range(8)))
    return np.stack([np.asarray(r["out"], dtype=np.float32) for r in res.results], axis=0)
```

```python
import math
import numpy as np
import ml_dtypes
from contextlib import ExitStack
import concourse.bass as bass
import concourse.mybir as mybir
from concourse.bass_utils import run_bass_kernel_spmd

F32 = mybir.dt.float32
BF16 = mybir.dt.bfloat16
AF = mybir.ActivationFunctionType
ALU = mybir.AluOpType
AX = mybir.AxisListType

PE, ACT, DVE, POOL, SP = "pe", "act", "dve", "pool", "sp"
ENGMAP = {PE: "tensor", ACT: "scalar", DVE: "vector", POOL: "gpsimd", SP: "sync"}
SEM_EPOCH = 30000

S = 8192
D = 1024
NT = S // 128
DEPTH = 4
IN_W = 3600
DFF = 2816
DFE = 3584
NE = 8
ALPHA = (2 * DEPTH) ** 0.25
LN_EPS = 1e-5


class Op:
    __slots__ = ("eng", "fn", "deps", "signal", "sem", "val", "is_dma", "grp", "ndep", "seq")

    def __init__(self, eng, fn, is_dma, grp):
        self.eng = eng
        self.fn = fn
        self.deps = []
        self.signal = False
        self.sem = None
        self.val = 0
        self.is_dma = is_dma
        self.grp = grp
        self.ndep = 0


class Phase:
    def __init__(self, nc, name):
        self.nc = nc
        self.name = name
        self.ops = []
        self.last_w = {}
        self.readers = {}
        self.stack = ExitStack()
        self.excl = set()
        self.alias = {}
        self.eng_seq = {}

    def sb(self, name, shape, dt):
        return self.stack.enter_context(self.nc.sbuf_tensor(f"{self.name}_{name}", list(shape), dt))

    def ps(self, name, shape, dt=F32):
        return self.stack.enter_context(self.nc.psum_tensor(f"{self.name}_{name}", list(shape), dt))

    def op(self, eng, fn, r=(), w=(), dma=False, grp=None):
        if self.alias:
            r = [self.alias.get(k, k) for k in r]
            w = [self.alias.get(k, k) for k in w]
        o = Op(eng, fn, dma, grp)
        o.seq = self.eng_seq.get(eng, 0)
        self.eng_seq[eng] = o.seq + 1
        deps = []
        seen = set()
        raw = set()
        for k in r:
            lw = self.last_w.get(k)
            if lw is not None:
                raw.add(id(lw))
                if id(lw) not in seen:
                    seen.add(id(lw))
                    deps.append(lw)
            if k in self.excl:
                for rd in self.readers.get(k, ()):
                    if rd.eng != eng and id(rd) not in seen:
                        seen.add(id(rd))
                        raw.add(id(rd))
                        deps.append(rd)
        for k in w:
            lw = self.last_w.get(k)
            if lw is not None and id(lw) not in seen:
                seen.add(id(lw))
                deps.append(lw)
            for rd in self.readers.get(k, ()):
                if id(rd) not in seen:
                    seen.add(id(rd))
                    deps.append(rd)
        for d in deps:
            if d.eng == eng and not d.is_dma and not dma:
                if eng == PE:
                    continue
                if id(d) not in raw and o.seq - d.seq > 1:
                    continue
            o.deps.append(d)
            d.ndep += 1
        for k in r:
            self.readers.setdefault(k, []).append(o)
        for k in w:
            self.last_w[k] = o
            self.readers[k] = []
        self.ops.append(o)
        return o

    def mm(self, out, lhsT, rhs, start, stop, r, w, **kw):
        return self.op(PE, lambda e: e.matmul(out, lhsT, rhs, start=start, stop=stop, **kw), r, w)

    def tr(self, out, in_, ident, r, w):
        return self.op(PE, lambda e: e.transpose(out, in_, ident), r, w)

    def act(self, out, in_, func, r, w, **kw):
        return self.op(ACT, lambda e: e.activation(out, in_, func, **kw), r, w)

    def copy(self, eng, out, in_, r, w):
        if eng == ACT:
            return self.op(ACT, lambda e: e.copy(out, in_), r, w)
        return self.op(eng, lambda e: e.tensor_copy(out, in_), r, w)

    def tt(self, eng, out, in0, in1, op, r, w):
        return self.op(eng, lambda e: e.tensor_tensor(out, in0, in1, op), r, w)

    def ts(self, eng, out, in0, s1, s2, op0, op1, r, w):
        return self.op(eng, lambda e: e.tensor_scalar(out, in0, s1, s2, op0, op1), r, w)

    def stt(self, out, in0, scalar, in1, op0, op1, r, w):
        return self.op(DVE, lambda e: e.scalar_tensor_tensor(out, in0, scalar, in1, op0, op1), r, w)

    def dma(self, q, out, in_, r, w, grp, **kw):
        return self.op(q, lambda e: e.dma_start(out, in_, **kw), r, w, dma=True, grp=grp)

    def emit(self):
        nc = self.nc
        leaf = [o for o in self.ops if o.is_dma and o.ndep == 0]
        last = {}
        for o in self.ops:
            if not o.is_dma:
                last[o.eng] = o
        leaf += list(last.values())
        if leaf:
            fin = Op(SP, lambda e: e.nop(), False, None)
            fin.deps = leaf
            self.ops.append(fin)
        for o in self.ops:
            for d in o.deps:
                d.signal = True
        sem_state = {}
        nsem = [0]

        sem_handles = []

        def new_sem():
            nsem[0] += 1
            h = nc.alloc_semaphore(name=f"{self.name}_s{nsem[0]}")
            sem_handles.append(h)
            return h

        for o in self.ops:
            if not o.signal:
                continue
            key = ("dma", o.grp) if o.is_dma else o.eng
            st = sem_state.get(key)
            inc = 16 if o.is_dma else 1
            if st is None or st[1] + inc > SEM_EPOCH:
                st = [new_sem(), 0]
                sem_state[key] = st
            st[1] += inc
            o.sem = st[0]
            o.val = st[1]
        self.n_sems = nsem[0]
        per_eng = {}
        for o in self.ops:
            per_eng.setdefault(o.eng, []).append(o)
        with nc.Block() as block:
            for ename, lst in per_eng.items():
                def body(e, lst=lst):
                    waited = {}
                    for o in lst:
                        need = {}
                        for d in o.deps:
                            k = id(d.sem)
                            if k not in need or need[k][1] < d.val:
                                need[k] = (d.sem, d.val)
                        for k, (s, v) in need.items():
                            if waited.get(k, 0) >= v:
                                continue
                            e.wait_ge(s, v)
                            waited[k] = v
                        ins = o.fn(e)
                        if o.signal:
                            ins.then_inc(o.sem, 16 if o.is_dma else 1)
                getattr(block, ENGMAP[ename])(body)
        if sem_handles:
            nc.clear_and_free_semaphores(sem_handles)
            nc.all_engine_barrier()
        nops = len(self.ops)
        self.ops = None
        self.last_w = None
        self.readers = None
        self.stack.close()
        return nops


class Ctx:
    pass


def rr(lst, state=[0]):
    state[0] += 1
    return lst[state[0] % len(lst)]


def phase_x0(C):
    nc = C.nc
    P = Phase(nc, "x0")
    ident = P.sb("ident", [128, 128], BF16)
    P.dma(SP, ident[:], C.ident_bf, [], ["ident"], "ident")
    xin = [P.sb(f"xin{i}", [128, D], F32) for i in range(2)]
    xb = [P.sb(f"xb{i}", [128, D], BF16) for i in range(2)]
    xt = [P.sb(f"xt{i}", [128, 8, 128], BF16) for i in range(2)]
    pt = [P.ps(f"pt{i}", [128, 8, 128], BF16) for i in range(2)]
    P.excl.update(["pt0", "pt1"])
    xT3 = C.xT.rearrange("(c p) t -> p c t", p=128)
    for t in range(NT):
        b = t % 2
        P.dma(SP, xin[b][:], C.x[t * 128:(t + 1) * 128, :], [], [f"xin{b}"], f"xin{b}")
        P.copy(DVE if t % 2 else POOL, xb[b][:], xin[b][:], [f"xin{b}"], [f"xb{b}"])
        for c in range(8):
            P.tr(pt[b][:, c, :], xb[b][:, c * 128:(c + 1) * 128], ident[:], [f"xb{b}", "ident"], [f"pt{b}"])
        P.copy(ACT if t % 2 else DVE, xt[b][:], pt[b][:], [f"pt{b}"], [f"xt{b}"])
        P.dma(POOL, xT3[:, :, t * 128:(t + 1) * 128], xt[b][:], [f"xt{b}"], [("xT", t)], f"xt{b}")
    return P.emit()


def load_cast_w(P, dst3, src2, nchunk, ncols, r_keys, wkey, stg, colstep, cnt):
    for c in range(nchunk):
        for c0 in range(0, ncols, colstep):
            n = min(colstep, ncols - c0)
            i = cnt[0] % len(stg)
            cnt[0] += 1
            P.dma(SP, stg[i][:, 0:n], src2[c * 128:(c + 1) * 128, c0:c0 + n], list(r_keys), [f"stg{i}"], f"stg{i}")
            eng = (DVE, POOL, ACT)[cnt[0] % 3]
            P.copy(eng, dst3[:, c, c0:c0 + n], stg[i][:, 0:n], [f"stg{i}"], [wkey])


def phase_inproj(C, layer):
    nc = C.nc
    P = Phase(nc, f"ip{layer}")
    W = P.sb("W", [128, 8, IN_W], BF16)
    stg = [P.sb(f"stg{i}", [128, 1800], F32) for i in range(3)]
    cnt = [0]
    load_cast_w(P, W, C.w_in[layer], 8, IN_W, [], "W", stg, 1800, cnt)
    xTb = [P.sb(f"xTb{i}", [128, 8, 512], BF16) for i in range(2)]
    qk_sb = [P.sb(f"qk{i}", [128, 512], BF16) for i in range(3)]
    qkv_sb = [P.sb(f"qkv{i}", [128, 1536], F32) for i in range(2)]
    z_sb = [P.sb(f"z{i}", [128, 512], F32) for i in range(2)]
    v_sb = [P.sb(f"v{i}", [128, 512], BF16) for i in range(2)]
    ps = [P.ps(f"ps{i}", [128, 512], F32) for i in range(8)]
    P.excl.update([f"ps{i}" for i in range(8)])
    xT3 = C.xT.rearrange("(c p) t -> p c t", p=128)
    pi = 0
    ev = 0
    nqk = 0
    for tb in range(S // 512):
        b = tb % 2
        P.dma(SP, xTb[b][:], xT3[:, :, tb * 512:(tb + 1) * 512], [("xT", tb * 4 + i) for i in range(4)],
              [f"xTb{b}"], f"xTb{b}")
        for g in range(8):
            col0 = 2064 + g * 128
            p = pi % 8
            pi += 1
            for c in range(8):
                P.mm(ps[p][:, :], W[:, c, col0:col0 + 128], xTb[b][:, c, :], c == 0, c == 7,
                     ["W", f"xTb{b}"], [f"ps{p}"])
            i = nqk % 3
            nqk += 1
            P.copy(ACT if ev % 2 else DVE, qk_sb[i][:], ps[p][:, :], [f"ps{p}"], [f"qk{i}"])
            ev += 1
            dst = C.QT[g] if g < 4 else C.KT[g - 4]
            P.dma(POOL, dst[:, tb * 512:(tb + 1) * 512], qk_sb[i][:], [f"qk{i}"], [("qkT", g, tb)], f"qk{i}")
        for tt in range(4):
            t = tb * 4 + tt
            tb2 = t % 2
            groups = [(0, 512, "qkv"), (512, 512, "qkv"), (1024, 512, "qkv"), (1536, 512, "z"),
                      (2048, 16, "ab"), (3088, 512, "v")]
            for (col0, n, kind) in groups:
                p = pi % 8
                pi += 1
                for c in range(8):
                    P.mm(ps[p][:, 0:n], xTb[b][:, c, tt * 128:(tt + 1) * 128], W[:, c, col0:col0 + n],
                         c == 0, c == 7, ["W", f"xTb{b}"], [f"ps{p}"])
                eng = ACT if ev % 2 else DVE
                ev += 1
                if kind == "qkv":
                    P.copy(eng, qkv_sb[tb2][:, col0:col0 + 512], ps[p][:, 0:512], [f"ps{p}"], [f"qkv{tb2}_{col0}"])
                elif kind == "z":
                    P.copy(eng, z_sb[tb2][:], ps[p][:, 0:512], [f"ps{p}"], [f"z{tb2}"])
                elif kind == "ab":
                    P.copy(eng, C.ab_sb[:, t, :], ps[p][:, 0:16], [f"ps{p}"], [("ab", t)])
                else:
                    P.copy(eng, v_sb[tb2][:], ps[p][:, 0:512], [f"ps{p}"], [f"v{tb2}"])
            P.dma(POOL, C.qkvpre[3 + t * 128:3 + (t + 1) * 128, :], qkv_sb[tb2][:],
                  [f"qkv{tb2}_0", f"qkv{tb2}_512", f"qkv{tb2}_1024"], [("qkvpre", t)], f"qkv{tb2}")
            P.dma(POOL, C.zbuf[t * 128:(t + 1) * 128, :], z_sb[tb2][:], [f"z{tb2}"], [("zbuf", t)], f"z{tb2}")
            P.dma(POOL, C.Vdf[t * 128:(t + 1) * 128, :], v_sb[tb2][:], [f"v{tb2}"], [("Vdf", t)], f"v{tb2}")
    return P.emit()


def phase_attn(C, layer):
    nc = C.nc
    P = Phase(nc, f"at{layer}")
    lambda_init = 0.8 - 0.6 * math.exp(-0.3 * layer)
    identf = P.sb("identf", [128, 128], F32)
    onesf = P.sb("onesf", [128, 128], F32)
    maskneg = P.sb("maskneg", [128, 128], F32)
    cfar = P.sb("cfar", [128, 4], F32)
    bt = P.sb("bt", [128, 8, 128], F32)
    badd = P.sb("badd", [128, 8, 128], F32)
    dl = P.sb("dl", [128, 256], F32)
    dlp = P.sb("dlp", [128, 128], F32)
    ls = P.sb("ls", [128, 2], F32)
    le = P.sb("le", [128, 2], F32)
    neglam = P.sb("neglam", [128, 1], F32)
    wcol = P.sb("wcol", [128, 1], F32)
    P.dma(SP, identf[:], C.ident_f32, [], ["identf"], "c0")
    P.dma(SP, onesf[:], C.ones_f32, [], ["onesf"], "c1")
    P.dma(SP, maskneg[:], C.maskneg, [], ["maskneg"], "c2")
    P.dma(SP, cfar[:], C.cfar, [], ["cfar"], "c3")
    P.dma(SP, bt[:], C.bias_tiles.rearrange("h r k q -> k (h r) q"), [], ["bt"], "c4")
    P.dma(SP, dl[:], C.df_lambda_bc[layer], [], ["dl"], "c5")
    P.dma(SP, wcol[:], C.df_subln_col[layer], [], ["wcol"], "c6")
    for h in range(4):
        for rel in range(2):
            i = h * 2 + rel
            P.ts(DVE, badd[:, i, :], bt[:, i, :], cfar[:, h:h + 1], 8.0, ALU.subtract, ALU.mult,
                 ["bt", "cfar"], [("badd", i)])
            if rel == 0:
                P.tt(DVE, badd[:, i, :], badd[:, i, :], maskneg[:], ALU.add, [("badd", i), "maskneg"], [("badd", i)])
    P.tt(DVE, dlp[:, 0:64], dl[:, 0:64], dl[:, 64:128], ALU.mult, ["dl"], ["dlp0"])
    P.tt(DVE, dlp[:, 64:128], dl[:, 128:192], dl[:, 192:256], ALU.mult, ["dl"], ["dlp1"])
    P.op(DVE, lambda e: e.tensor_reduce(ls[:], dlp[:].rearrange("p (a b) -> p a b", a=2), AX.X, ALU.add),
         ["dlp0", "dlp1"], ["ls"])
    P.act(le[:], ls[:], AF.Exp, ["ls"], ["le"])
    P.tt(DVE, neglam[:], le[:, 1:2], le[:, 0:1], ALU.subtract, ["le"], ["neglam"])
    P.ts(DVE, neglam[:], neglam[:], -lambda_init, None, ALU.add, ALU.bypass, ["neglam"], ["neglam"])
    P.ts(DVE, wcol[:], wcol[:], 1.0 - lambda_init, None, ALU.mult, ALU.bypass, ["wcol"], ["wcol"])

    KTh = [P.sb(f"KTh{i}", [128, S], BF16) for i in range(2)]
    QTz = [[P.sb(f"QTz{i}_{m}", [128, S], BF16) for m in range(2)] for i in range(2)]
    for i in range(2):
        for m in range(2):
            P.op(POOL if (i + m) % 2 else DVE, lambda e, i=i, m=m: e.memset(QTz[i][m][:], 0.0), [], [f"QTh{i}"])
    Vh = [P.sb(f"Vh{i}", [128, NT, 128], BF16) for i in range(2)]
    pT = [P.sb(f"pT{i}", [128, 512], BF16) for i in range(4)]
    PSa = [P.sb(f"PSa{i}", [128, 512], F32) for i in range(2)]
    rden = [P.sb(f"rden{i}", [128, 512], F32) for i in range(2)]
    on = [P.sb(f"on{i}", [128, 512], F32) for i in range(2)]
    o_sb = P.sb("o_sb", [128, 512], F32)
    sq = P.sb("sq", [128, 512], F32)
    rstd = P.sb("rstd", [128, 512], F32)
    of = [P.sb(f"of{i}", [128, 512], BF16) for i in range(2)]
    accO = [P.ps(f"accO{i}", [128, 512], F32) for i in range(2)]
    sc = [P.ps(f"sc{i}", [128, 512], F32) for i in range(4)]
    aux = [P.ps(f"aux{i}", [128, 512], F32) for i in range(2)]
    P.excl.update(["accO0", "accO1", "sc0", "sc1", "sc2", "sc3", "aux0", "aux1"])
    V3 = C.Vdf.rearrange("(t p) c -> p t c", p=128)

    def load_head(h):
        b = h % 2
        P.dma(SP, KTh[b][:], C.KT[h], [], [f"KTh{b}"], f"KTh{b}")
        for m in range(2):
            P.dma(SP, QTz[b][m][m * 64:(m + 1) * 64, :], C.QT[h][m * 64:(m + 1) * 64, :], [], [f"QTh{b}"], f"QTh{b}_{m}")
        for i in range(8):
            P.dma(SP, Vh[b][:, i * 8:(i + 1) * 8, :], V3[:, i * 8:(i + 1) * 8, h * 128:(h + 1) * 128], [],
                  [(f"Vh{b}", i)], f"Vh{b}_{i}")

    load_head(0)
    LA = 3
    nq = 0
    gstep = 0
    for h in range(4):
        b = h % 2
        if h + 1 < 4:
            load_head(h + 1)
        steps = []
        for Qb in range(S // 512):
            nk = 4 * Qb + 4
            for kt in range(nk):
                for m in range(2):
                    steps.append((Qb, kt, m, nk))
        ns = len(steps)

        def front(si, gs):
            Qb, kt, m, nk = steps[si]
            j = kt - 4 * Qb
            c0 = max(0, j) * 128
            sI = gs % 4
            pi = gs % 4
            adds = []
            if j >= 0:
                adds.append((j * 128, h * 2 + 0))
            if j >= -1 and j + 1 <= 3:
                adds.append(((j + 1) * 128, h * 2 + 1))
            P.mm(sc[sI][:, c0:512], KTh[b][:, kt * 128:(kt + 1) * 128],
                 QTz[b][m][:, Qb * 512 + c0:Qb * 512 + 512], True, len(adds) == 0,
                 [f"KTh{b}", f"QTh{b}"], [f"sc{sI}"])
            for ai, (cc, bi) in enumerate(adds):
                P.mm(sc[sI][:, cc:cc + 128], identf[:], badd[:, bi, :], False, ai == len(adds) - 1,
                     ["identf", ("badd", bi)], [f"sc{sI}"])
            P.act(pT[pi][:, c0:512], sc[sI][:, c0:512], AF.Exp, [f"sc{sI}"], [f"pT{pi}"], scale=0.125)

        def back(si, gs):
            nonlocal nq
            Qb, kt, m, nk = steps[si]
            j = kt - 4 * Qb
            c0 = max(0, j) * 128
            pi = gs % 4
            P.mm(accO[m][:, c0:512], Vh[b][:, kt, :], pT[pi][:, c0:512], kt == 0, kt == nk - 1,
                 [(f"Vh{b}", kt // 8), f"pT{pi}"], [f"accO{m}"])
            eng = DVE if m == 0 else POOL
            if kt == 0:
                P.copy(eng, PSa[m][:], pT[pi][:], [f"pT{pi}"], [f"PSa{m}"])
            else:
                P.tt(eng, PSa[m][:, c0:512], PSa[m][:, c0:512], pT[pi][:, c0:512], ALU.add,
                     [f"PSa{m}", f"pT{pi}"], [f"PSa{m}"])
            if not (kt == nk - 1 and m == 1):
                return
            for mm_ in range(2):
                P.mm(aux[mm_][:], onesf[:], PSa[mm_][:], True, True, ["onesf", f"PSa{mm_}"], [f"aux{mm_}"])
                P.op(DVE, lambda e, mm_=mm_: e.reciprocal(rden[mm_][:], aux[mm_][:]), [f"aux{mm_}"], [f"rden{mm_}"])
                P.tt(DVE, on[mm_][:], accO[mm_][:], rden[mm_][:], ALU.mult, [f"accO{mm_}", f"rden{mm_}"], [f"on{mm_}"])
            P.stt(o_sb[:], on[1][:], neglam[:, 0:1], on[0][:], ALU.mult, ALU.add, ["on0", "on1", "neglam"], ["o_sb"])
            P.tt(POOL, sq[:], o_sb[:], o_sb[:], ALU.mult, ["o_sb"], ["sq"])
            P.mm(aux[0][:], onesf[:], sq[:], True, True, ["onesf", "sq"], ["aux0"])
            P.ts(DVE, rstd[:], aux[0][:], 1.0 / 128.0, 1e-6, ALU.mult, ALU.add, ["aux0"], ["rstd"])
            P.act(rstd[:], rstd[:], AF.Ln, ["rstd"], ["rstd"])
            P.act(rstd[:], rstd[:], AF.Exp, ["rstd"], ["rstd"], scale=-0.5)
            P.tt(DVE, o_sb[:], o_sb[:], rstd[:], ALU.mult, ["o_sb", "rstd"], ["o_sb"])
            ob = nq % 2
            nq += 1
            P.ts(DVE, of[ob][:], o_sb[:], wcol[:, 0:1], None, ALU.mult, ALU.bypass, ["o_sb", "wcol"], [f"of{ob}"])
            P.dma(POOL, C.catT[512 + h * 128:512 + (h + 1) * 128, Qb * 512:(Qb + 1) * 512], of[ob][:],
                  [f"of{ob}"], [("catT_df", h, Qb)], f"of{ob}")

        for idx in range(ns + LA):
            if idx < ns:
                front(idx, gstep + idx)
            if idx - LA >= 0:
                back(idx - LA, gstep + idx - LA)
        gstep += ns
    return P.emit()


def ln_epilogue(P, C, t, src_halves, src_keys, res_src, gb, ident, bufs, pt, pt_key, out_dst=None, router=None):
    i = t % 2
    xr, y, xo, xb, xt = bufs["xr"][i], bufs["y"][i], bufs["xo"][i], bufs["xb"][i], bufs["xt"][i]
    kxr, ky, kxo, kxb, kxt = bufs["kxr"][i], bufs["ky"][i], bufs["kxo"][i], bufs["kxb"][i], bufs["kxt"][i]
    st, mv, rs, nmr = bufs["st"][i], bufs["mv"][i], bufs["rs"][i], bufs["nmr"][i]
    ks = f"lnsmall{i}"
    P.dma(SP, xr[:, 0:D], res_src[t * 128:(t + 1) * 128, :], [("xres", t)], [kxr], f"ln_{kxr}")
    for hf in range(2):
        P.stt(y[:, hf * 512:(hf + 1) * 512], xr[:, hf * 512:(hf + 1) * 512], ALPHA, src_halves[hf], ALU.mult, ALU.add,
              [kxr, src_keys[hf]], [ky])
        P.op(DVE, lambda e, hf=hf: e.bn_stats(st[:, hf, :], y[:, hf * 512:(hf + 1) * 512]), [ky], [ks + f"st{hf}"])
    P.op(DVE, lambda e: e.bn_aggr(mv[:], st[:].rearrange("p a b -> p (a b)")), [ks + "st0", ks + "st1"], [ks + "mv"])
    P.ts(DVE, rs[:], mv[:, 1:2], LN_EPS, None, ALU.add, ALU.bypass, [ks + "mv"], [ks + "rs"])
    P.act(rs[:], rs[:], AF.Ln, [ks + "rs"], [ks + "rs"])
    P.act(rs[:], rs[:], AF.Exp, [ks + "rs"], [ks + "rs"], scale=-0.5)
    P.ts(DVE, nmr[:], mv[:, 0:1], rs[:, 0:1], -1.0, ALU.mult, ALU.mult, [ks + "mv", ks + "rs"], [ks + "nmr"])
    P.ts(POOL, y[:, 0:D], y[:, 0:D], rs[:, 0:1], nmr[:, 0:1], ALU.mult, ALU.add, [ky, ks + "rs", ks + "nmr"],
         [ky])
    P.tt(POOL, y[:, 0:D], y[:, 0:D], gb[0][:], ALU.mult, [ky, "ln_g"], [ky])
    P.tt(DVE, xo[:, 0:D], y[:, 0:D], gb[1][:], ALU.add, [ky, "ln_b"], [kxo])
    if out_dst is not None:
        P.dma(POOL, out_dst[t * 128:(t + 1) * 128, :], xo[:, 0:D], [kxo], [("out", t)], f"ln_{kxo}")
        return
    P.dma(POOL, C.xres[t * 128:(t + 1) * 128, :], xo[:, 0:D], [kxo], [("xres", t)], f"ln_{kxo}")
    P.copy(ACT, xb[:, 0:D], xo[:, 0:D], [kxo], [kxb])
    for c in range(8):
        P.tr(pt[:, c, :], xb[:, c * 128:(c + 1) * 128], ident[:], [kxb, "ident"], [pt_key])
    P.copy(ACT, xt[:], pt, [pt_key], [kxt])
    xT3 = C.xT.rearrange("(c p) t -> p c t", p=128)
    P.dma(POOL, xT3[:, :, t * 128:(t + 1) * 128], xt[:], [kxt], [("xT", t)], f"ln_{kxt}")
    if router is not None:
        rbc, lg, junk = router
        for e in range(NE):
            P.op(DVE, lambda en, e=e: en.scalar_tensor_tensor(junk[:], xo[:, 0:D], 1.0, rbc[:, e, :], ALU.mult, ALU.mult,
                                                               accum_out=lg[i][:, e:e + 1]),
                 [kxo, "rbc"], [f"lg{i}_{e}", "junk"])
        lgk = [f"lg{i}_{e}" for e in range(NE)]
        sm = bufs["sm"][i]
        kk = f"sm{i}"
        P.op(DVE, lambda en: en.tensor_reduce(sm[:, 0:1], lg[i][:], AX.X, ALU.max), lgk, [kk + "m1"])
        P.ts(DVE, sm[:, 8:16], lg[i][:], sm[:, 0:1], None, ALU.is_equal, ALU.bypass, lgk + [kk + "m1"], [kk + "k1"])
        P.stt(sm[:, 24:32], sm[:, 8:16], -1e30, lg[i][:], ALU.mult, ALU.add, [kk + "k1"] + lgk, [kk + "l2"])
        P.op(DVE, lambda en: en.tensor_reduce(sm[:, 1:2], sm[:, 24:32], AX.X, ALU.max), [kk + "l2"], [kk + "m2"])
        P.ts(DVE, sm[:, 16:24], sm[:, 24:32], sm[:, 1:2], None, ALU.is_equal, ALU.bypass, [kk + "l2", kk + "m2"], [kk + "k2"])
        P.tt(DVE, sm[:, 2:3], sm[:, 1:2], sm[:, 0:1], ALU.subtract, [kk + "m1", kk + "m2"], [kk + "d"])
        P.act(sm[:, 2:3], sm[:, 2:3], AF.Exp, [kk + "d"], [kk + "d"])
        P.ts(DVE, sm[:, 3:4], sm[:, 2:3], 1.0, None, ALU.add, ALU.bypass, [kk + "d"], [kk + "g1"])
        P.op(DVE, lambda en: en.reciprocal(sm[:, 3:4], sm[:, 3:4]), [kk + "g1"], [kk + "g1"])
        P.tt(DVE, sm[:, 4:5], sm[:, 2:3], sm[:, 3:4], ALU.mult, [kk + "d", kk + "g1"], [kk + "g2"])
        P.ts(DVE, sm[:, 8:16], sm[:, 8:16], sm[:, 3:4], None, ALU.mult, ALU.bypass, [kk + "k1", kk + "g1"], [kk + "k1"])
        P.stt(C.gate_sb[:, t, :], sm[:, 16:24], sm[:, 4:5], sm[:, 8:16], ALU.mult, ALU.add,
              [kk + "k2", kk + "g2", kk + "k1"], [("gate", t)])


def ln_bufs(P):
    b = {}
    for nm, shape, dt in [("xr", [128, D], F32), ("y", [128, D], F32), ("xo", [128, D], F32), ("xb", [128, D], BF16),
                          ("xt", [128, 8, 128], BF16), ("st", [128, 2, 6], F32), ("mv", [128, 2], F32),
                          ("rs", [128, 1], F32), ("nmr", [128, 1], F32), ("sm", [128, 40], F32)]:
        b[nm] = [P.sb(f"ln_{nm}{i}", shape, dt) for i in range(2)]
        b["k" + nm] = [f"ln_{nm}{i}" for i in range(2)]
    return b


def phase_outproj(C, layer):
    nc = C.nc
    P = Phase(nc, f"op{layer}")
    moe = (layer % 2 == 1)
    ident = P.sb("ident", [128, 128], BF16)
    P.dma(SP, ident[:], C.ident_bf, [], ["ident"], "c0")
    gb = [P.sb("ln_g", [128, D], F32), P.sb("ln_b", [128, D], F32)]
    P.dma(SP, gb[0][:], C.ln1_g_bc[layer], [], ["ln_g"], "c1")
    P.dma(SP, gb[1][:], C.ln1_b_bc[layer], [], ["ln_b"], "c2")
    router = None
    if moe:
        rbc = P.sb("rbc", [128, NE, D], F32)
        P.dma(SP, rbc[:], C.router_bc[layer // 2], [], ["rbc"], "c3")
        lg = [P.sb(f"lg{i}", [128, NE], F32) for i in range(2)]
        junk = P.sb("junk", [128, D], F32)
        router = (rbc, lg, junk)
    Wo = P.sb("Wo", [128, 8, D], BF16)
    stg = [P.sb(f"stg{i}", [128, 1024], F32) for i in range(3)]
    load_cast_w(P, Wo, C.w_out[layer], 8, D, [], "Wo", stg, 1024, [0])
    bufs = ln_bufs(P)
    ct = [P.sb(f"ct{i}", [128, 8, 512], BF16) for i in range(2)]
    ps = [P.ps(f"ps{i}", [128, 512], F32) for i in range(4)]
    pt = [P.ps(f"pt{i}", [128, 8, 128], BF16) for i in range(2)]
    P.excl.update(["ps0", "ps1", "ps2", "ps3", "pt0", "pt1"])
    catT3 = C.catT.rearrange("(c p) t -> p c t", p=128)
    res_src = C.x if layer == 0 else C.xres
    for tb in range(S // 512):
        b = tb % 2
        P.dma(SP, ct[b][:], catT3[:, :, tb * 512:(tb + 1) * 512], [], [f"ct{b}"], f"ct{b}")
        for tt in range(4):
            t = tb * 4 + tt
            pp = (t % 2) * 2
            for hf in range(2):
                for c in range(8):
                    P.mm(ps[pp + hf][:], ct[b][:, c, tt * 128:(tt + 1) * 128], Wo[:, c, hf * 512:(hf + 1) * 512],
                         c == 0, c == 7, [f"ct{b}", "Wo"], [f"ps{pp + hf}"])
            ln_epilogue(P, C, t, [ps[pp][:], ps[pp + 1][:]], [f"ps{pp}", f"ps{pp + 1}"], res_src, gb, ident, bufs,
                        pt[t % 2][:], f"pt{t % 2}", router=router)
    return P.emit()


def phase_ffn(C, layer, last):
    nc = C.nc
    P = Phase(nc, f"ff{layer}")
    moe = (layer % 2 == 1)
    li = layer // 2
    if moe:
        E, F = NE, DFE
        wg_of = lambda e: C.moe_w_gate[li, e]
        wu_of = lambda e: C.moe_w_up[li, e]
        wd_of = lambda e: C.moe_w_down[li, e]
    else:
        E, F = 1, DFF
        wg_of = lambda e: C.ffn_w_gate[li]
        wu_of = lambda e: C.ffn_w_up[li]
        wd_of = lambda e: C.ffn_w_down[li]
    nfc = F // 128
    groups = [(f0, min(4, nfc - f0)) for f0 in range(0, nfc, 4)]
    SBT = 16
    ident = P.sb("ident", [128, 128], BF16)
    P.dma(SP, ident[:], C.ident_bf, [], ["ident"], "c0")
    gb = [P.sb("ln_g", [128, D], F32), P.sb("ln_b", [128, D], F32)]
    P.dma(SP, gb[0][:], C.ln2_g_bc[layer], [], ["ln_g"], "c1")
    P.dma(SP, gb[1][:], C.ln2_b_bc[layer], [], ["ln_b"], "c2")
    acc = P.sb("acc", [128, SBT, D], F32)
    xTs = P.sb("xTs", [128, 8, SBT * 128], BF16)
    Wg = [P.sb(f"Wg{i}", [128, 8, 512], BF16) for i in range(2)]
    Wu = [P.sb(f"Wu{i}", [128, 8, 512], BF16) for i in range(2)]
    Wd = [P.sb(f"Wd{i}", [128, 4, D], BF16) for i in range(2)]
    stg = [P.sb(f"stg{i}", [128, 2048], F32) for i in range(3)]
    hT = [P.sb(f"hT{i}", [128, 4, 512], BF16) for i in range(2)]
    sg = [P.sb(f"sg{i}", [128, 512], F32) for i in range(2)]
    bufs = {}
    for nm, shape, dt in [("xb", [128, D], BF16), ("xt", [128, 8, 128], BF16), ("st", [128, 2, 6], F32),
                          ("mv", [128, 2], F32), ("rs", [128, 1], F32), ("nmr", [128, 1], F32), ("sm", [128, 40], F32)]:
        bufs[nm] = [P.sb(f"ln_{nm}{i}", shape, dt) for i in range(2)]
        bufs["k" + nm] = [f"ln_{nm}{i}" for i in range(2)]
    bufs["xr"] = [stg[0], stg[0]]
    bufs["kxr"] = ["stg0", "stg0"]
    bufs["y"] = [stg[1], stg[1]]
    bufs["ky"] = ["stg1", "stg1"]
    bufs["xo"] = [stg[2], stg[2]]
    bufs["kxo"] = ["stg2", "stg2"]
    pg = [P.ps(f"pg{i}", [128, 512], F32) for i in range(2)]
    pu = [P.ps(f"pu{i}", [128, 512], F32) for i in range(2)]
    py = [P.ps(f"py{i}", [128, 512], F32) for i in range(4)]
    P.excl.update(["pg0", "pg1", "pu0", "pu1", "py0", "py1", "py2", "py3"])
    xT3 = C.xT.rearrange("(c p) t -> p c t", p=128)
    cnt = [0]
    ngu = 0
    nh = 0
    ny = 0
    wi = 0
    import os as _os
    dbg_sb = int(_os.environ.get("FF_SB", NT // SBT))
    dbg_ng = int(_os.environ.get("FF_NG", 10 ** 6))
    dbg_noln = bool(int(_os.environ.get("FF_NOLN", "0")))
    for sbk in range(min(NT // SBT, dbg_sb)):
        t0 = sbk * SBT
        for q4 in range(4):
            P.dma(SP, xTs[:, :, q4 * 512:(q4 + 1) * 512], xT3[:, :, t0 * 128 + q4 * 512:t0 * 128 + (q4 + 1) * 512],
                  [("xT", t0 + q4 * 4 + i) for i in range(4)], [("xTs", q4)], f"xTs{q4}")
        first = [True] * SBT
        for e in range(E):
            for (f0, nf) in groups[:dbg_ng]:
                wb = wi % 2
                wi += 1
                for c in range(8):
                    for (dst, src, key) in ((Wg[wb], wg_of(e), f"Wg{wb}"), (Wu[wb], wu_of(e), f"Wu{wb}")):
                        i = cnt[0] % 3
                        cnt[0] += 1
                        P.dma(SP, stg[i][:, 0:nf * 128], src[c * 128:(c + 1) * 128, f0 * 128:(f0 + nf) * 128], [],
                              [f"stg{i}"], f"stg{i}")
                        P.copy(POOL, dst[:, c, 0:nf * 128], stg[i][:, 0:nf * 128], [f"stg{i}"], [key])
                for fc in range(0, nf, 2):
                    n2 = min(2, nf - fc)
                    i = cnt[0] % 3
                    cnt[0] += 1
                    src = wd_of(e)[(f0 + fc) * 128:(f0 + fc + n2) * 128, :].rearrange("(a p) d -> p a d", p=128)
                    P.dma(SP, stg[i][:, 0:n2 * D].rearrange("p (a d) -> p a d", a=n2), src, [], [f"stg{i}"], f"stg{i}")
                    P.copy(POOL, Wd[wb][:, fc:fc + n2, :], stg[i][:, 0:n2 * D].rearrange("p (a d) -> p a d", a=n2),
                           [f"stg{i}"], [f"Wd{wb}"])
                for q4 in range(SBT // 4):
                    hb = nh % 2
                    nh += 1
                    for fc in range(nf):
                        gi = ngu % 2
                        ngu += 1
                        for c in range(8):
                            P.mm(pg[gi][:], Wg[wb][:, c, fc * 128:(fc + 1) * 128], xTs[:, c, q4 * 512:(q4 + 1) * 512],
                                 c == 0, c == 7, [f"Wg{wb}", ("xTs", q4)], [f"pg{gi}"])
                        for c in range(8):
                            P.mm(pu[gi][:], Wu[wb][:, c, fc * 128:(fc + 1) * 128], xTs[:, c, q4 * 512:(q4 + 1) * 512],
                                 c == 0, c == 7, [f"Wu{wb}", ("xTs", q4)], [f"pu{gi}"])
                        P.act(sg[gi][:], pg[gi][:], AF.Silu, [f"pg{gi}"], [f"sg{gi}"])
                        P.tt(DVE, hT[hb][:, fc, :], sg[gi][:], pu[gi][:], ALU.mult, [f"sg{gi}", f"pu{gi}"], [(f"hT{hb}", fc)])
                    for tt in range(4):
                        tl = q4 * 4 + tt
                        yb = (ny % 2) * 2
                        ny += 1
                        for hf in range(2):
                            for fc in range(nf):
                                P.mm(py[yb + hf][:], hT[hb][:, fc, tt * 128:(tt + 1) * 128], Wd[wb][:, fc, hf * 512:(hf + 1) * 512],
                                     fc == 0, fc == nf - 1, [(f"hT{hb}", fc), f"Wd{wb}"], [f"py{yb + hf}"])
                            dst = acc[:, tl, hf * 512:(hf + 1) * 512]
                            akey = ("acc", tl, hf)
                            if moe:
                                gsc = C.gate_sb[:, t0 + tl, e:e + 1]
                                if first[tl]:
                                    P.ts(DVE, dst, py[yb + hf][:], gsc, None, ALU.mult, ALU.bypass, [f"py{yb + hf}", ("gate", t0 + tl)], [akey])
                                else:
                                    P.stt(dst, py[yb + hf][:], gsc, dst, ALU.mult, ALU.add, [f"py{yb + hf}", akey, ("gate", t0 + tl)], [akey])
                            else:
                                if first[tl]:
                                    P.copy(DVE, dst, py[yb + hf][:], [f"py{yb + hf}"], [akey])
                                else:
                                    P.tt(DVE, dst, py[yb + hf][:], dst, ALU.add, [f"py{yb + hf}", akey], [akey])
                        first[tl] = False
        for tl in range(0 if dbg_noln else SBT):
            t = t0 + tl
            ptv = py[tl % 4][:].bitcast(BF16).rearrange("p (c t) -> p c t", c=8)
            ln_epilogue(P, C, t, [acc[:, tl, 0:512], acc[:, tl, 512:1024]], [("acc", tl, 0), ("acc", tl, 1)], C.xres, gb, ident,
                        bufs, ptv, f"py{tl % 4}", out_dst=(C.out if last else None))
    return P.emit()


def bc(ap, shape):
    return ap.to_broadcast(list(shape))


def phase_dn(C, layer):
    nc = C.nc
    P = Phase(nc, f"dn{layer}")
    identb = P.sb("identb", [128, 128], BF16)
    identf = P.sb("identf", [128, 128], F32)
    onesf = P.sb("onesf", [128, 128], F32)
    trif = P.sb("trif", [128, 128], F32)
    mcaus = P.sb("mcaus", [128, 128], F32)
    mneg = P.sb("mneg", [128, 128], F32)
    cw = P.sb("cw", [128, 4, 1536], F32)
    alog = P.sb("alog", [128, 8], F32)
    dtb = P.sb("dtb", [128, 8], F32)
    wn = P.sb("wn", [128, 64], F32)
    for i, (dst, src, key) in enumerate([(identb, C.ident_bf, "identb"), (identf, C.ident_f32, "identf"), (onesf, C.ones_f32, "onesf"),
                                         (trif, C.tri_f32, "trif"), (mcaus, C.mcausT, "mcaus"), (mneg, C.mnegT, "mneg"),
                                         (alog, C.a_log_bc[layer], "alog"), (dtb, C.dt_bias_bc[layer], "dtb"),
                                         (wn, C.dn_norm_bc[layer], "wn")]):
        P.dma(SP, dst[:], src, [], [key], f"c{i}")
    P.dma(SP, cw[:], C.conv_w_bc[layer].rearrange("p (j c) -> p j c", j=4), [], ["cw"], "c_cw")

    def g8(name):
        return P.sb(name, [128, NT, 8], F32)
    x8, gg8, beta8, gc8, glb8, eg8 = [g8(n) for n in ("x8", "gg8", "beta8", "gc8", "glb8", "eg8")]
    kd8, negeg8, egl8 = x8, gg8, glb8
    P.alias = {"kd8": "x8", "negeg8": "gg8", "egl8": "glb8"}
    eglS = P.sb("eglS", [128, NT, 4], F32)
    negA = P.sb("negA", [128, 8], F32)
    pp = [P.ps(f"pp{i}", [128, 512], F32) for i in range(4)]
    rA, rB, rC, rD = [P.ps(f"r{n}", [128, 512], F32) for n in "ABCD"]
    P.excl.update(["pp0", "pp1", "pp2", "pp3", "rK0", "rK1", "rC", "rD"])
    P.act(negA[:], alog[:], AF.Exp, ["alog"], ["negA"])
    P.ts(DVE, negA[:], negA[:], -1.0, None, ALU.mult, ALU.bypass, ["negA"], ["negA"])
    for h in range(8):
        P.ts(DVE, x8[:, :, h], C.ab_sb[:, :, h], dtb[:, h:h + 1], None, ALU.add, ALU.bypass, ["dtb"], ["x8"])
    P.act(x8[:], x8[:], AF.Exp, ["x8"], ["x8"])
    P.act(x8[:], x8[:], AF.Ln, ["x8"], ["x8"], bias=1.0)
    for h in range(8):
        P.ts(DVE, gg8[:, :, h], x8[:, :, h], negA[:, h:h + 1], None, ALU.mult, ALU.bypass, ["x8", "negA"], ["gg8"])
    P.act(beta8[:], C.ab_sb[:, :, 8:16], AF.Exp, [], ["beta8"], scale=-1.0)
    P.ts(DVE, beta8[:], beta8[:], 1.0, None, ALU.add, ALU.bypass, ["beta8"], ["beta8"])
    P.op(DVE, lambda e: e.reciprocal(beta8[:], beta8[:]), ["beta8"], ["beta8"])
    ggf = gg8[:].rearrange("p t h -> p (t h)")
    P.mm(pp[0][:], trif[:], ggf, True, True, ["trif", "gg8"], ["pp0"])
    P.mm(pp[1][:], onesf[:], ggf, True, True, ["onesf", "gg8"], ["pp1"])
    P.copy(DVE, gc8[:].rearrange("p t h -> p (t h)"), pp[0][:], ["pp0"], ["gc8"])
    P.copy(DVE, glb8[:].rearrange("p t h -> p (t h)"), pp[1][:], ["pp1"], ["glb8"])
    P.act(eg8[:], gc8[:], AF.Exp, ["gc8"], ["eg8"])
    P.ts(DVE, negeg8[:], eg8[:], -1.0, None, ALU.mult, ALU.bypass, ["eg8"], ["negeg8"])
    P.tt(DVE, kd8[:], glb8[:], gc8[:], ALU.subtract, ["glb8", "gc8"], ["kd8"])
    P.act(kd8[:], kd8[:], AF.Exp, ["kd8"], ["kd8"])
    P.act(egl8[:], glb8[:], AF.Exp, ["glb8"], ["egl8"])
    for par in range(2):
        P.copy(DVE, eglS[par * 64:(par + 1) * 64, :, :], egl8[par * 64:(par + 1) * 64, :, par::2], ["egl8"], ["eglS"])

    cv = [P.sb(f"cv{i}", [128, 4, 1536], F32) for i in range(2)]
    act = [P.sb(f"act{i}", [128, 1536], F32) for i in range(2)]
    ss = [P.sb(f"ss{i}", [128, 16], F32) for i in range(2)]
    qkb = [P.sb(f"qkb{i}", [128, 1024], BF16) for i in range(2)]
    dg = [P.sb(f"dg{i}", [128, 4, 128], F32) for i in range(2)]
    dsb = [P.sb(f"dsb{i}", [128, 4, 128], F32) for i in range(2)]
    DTs = [P.sb(f"DTs{i}", [128, 4, 128], F32) for i in range(2)]
    DTc = [P.sb(f"DTc{i}", [128, 4, 128], F32) for i in range(2)]
    Mb = [[P.sb(f"M{i}_{k}", [128, 8, 128], BF16) for k in range(2)] for i in range(2)]
    MTb = [[P.sb(f"MT{i}_{k}", [128, 8, 128], BF16) for k in range(2)] for i in range(2)]
    PTf = [P.sb(f"PTf{i}", [128, 8, 128], F32) for i in range(2)]
    PTw = [P.sb(f"PTw{i}", [128, 8, 128], BF16) for i in range(2)]
    qkT = [P.sb(f"qkT{i}", [128, 8, 128], BF16) for i in range(3)]
    PTb = [P.sb(f"PTb{i}", [128, 8, 128], BF16) for i in range(3)]
    inT = [P.sb(f"inT{i}", [128, 8, 128], BF16) for i in range(3)]
    v_f = [P.sb(f"v_f{i}", [128, 512], F32) for i in range(3)]
    kdec = [P.sb(f"kdec{i}", [128, 512], BF16) for i in range(3)]
    zt = [P.sb(f"zt{i}", [128, 512], F32) for i in range(3)]
    o_f = [P.sb(f"o_f{i}", [128, 512], F32) for i in range(3)]
    Sf = P.sb("Sf", [128, 4, 64], F32)
    Sb = P.sb("Sb", [128, 4, 64], BF16)
    tmp = P.sb("tmp", [128, 8, 64], F32)
    rp = P.sb("rp", [128, 8, 64], BF16)
    vn = P.sb("vn", [128, 8, 64], BF16)
    o2 = [P.sb(f"o2{i}", [128, 512], F32) for i in range(2)]
    ssn = [P.sb(f"ssn{i}", [128, 8], F32) for i in range(2)]
    o_bf = [P.sb(f"o_bf{i}", [128, 512], BF16) for i in range(2)]
    oT = [P.sb(f"oT{i}", [128, 4, 128], BF16) for i in range(2)]
    P.op(DVE, lambda e: e.memset(Sf[:], 0.0), [], ["Sf"])
    P.op(DVE, lambda e: e.memset(Sb[:], 0.0), [], ["Sb"])
    pp_free = [0, 1, 2, 3]

    def acquire(n):
        while len(pp_free) < n:
            yield
        return [pp_free.pop(0) for _ in range(n)]

    def release(*idx):
        pp_free.extend(idx)

    def hp(h):
        return (h % 2) * 64

    def prep(t):
        pb = t % 2
        hb = t % 3
        K = lambda s: f"{s}{pb}"
        H = lambda s: f"{s}{hb}"
        src = bass.AP(tensor=C.qkvpre.tensor, offset=t * 128 * 1536, ap=[[1536, 128], [1536, 4], [1, 1536]])
        P.dma(SP, cv[pb][:], src, [("qkvpre", t)], [K("cv")], K("cv"))
        yield
        P.tt(POOL, cv[pb][:], cv[pb][:], cw[:], ALU.mult, [K("cv"), "cw"], [K("cv")])
        yield
        P.tt(DVE, cv[pb][:, 0:2, :], cv[pb][:, 0:2, :], cv[pb][:, 2:4, :], ALU.add, [K("cv")], [K("cv")])
        P.tt(DVE, act[pb][:], cv[pb][:, 0, :], cv[pb][:, 1, :], ALU.add, [K("cv")], [K("act")])
        yield
        P.act(act[pb][:], act[pb][:], AF.Silu, [K("act")], [K("act")])
        yield
        sqv = cv[pb][:, 2, 0:1024]
        P.tt(POOL, sqv, act[pb][:, 0:1024], act[pb][:, 0:1024], ALU.mult, [K("act")], [K("cv")])
        P.copy(POOL, v_f[hb][:], act[pb][:, 1024:1536], [K("act")], [H("v_f")])
        yield
        P.op(DVE, lambda e: e.tensor_reduce(ss[pb][:], sqv.rearrange("p (h d) -> p h d", h=16), AX.X, ALU.add),
             [K("cv")], [K("ss")])
        P.ts(DVE, ss[pb][:], ss[pb][:], 1e-6, None, ALU.add, ALU.bypass, [K("ss")], [K("ss")])
        yield
        P.act(ss[pb][:], ss[pb][:], AF.Ln, [K("ss")], [K("ss")])
        P.act(ss[pb][:], ss[pb][:], AF.Exp, [K("ss")], [K("ss")], scale=-0.5)
        yield
        P.ts(DVE, ss[pb][:, 0:8], ss[pb][:, 0:8], 0.125, None, ALU.mult, ALU.bypass, [K("ss")], [K("ss")])
        P.tt(DVE, qkb[pb][:].rearrange("p (h d) -> p h d", h=16), act[pb][:, 0:1024].rearrange("p (h d) -> p h d", h=16),
             bc(ss[pb][:].unsqueeze(2), [128, 16, 64]), ALU.mult, [K("act"), K("ss")], [K("qkb")])
        yield
        P.tt(POOL, kdec[hb][:].rearrange("p (h d) -> p h d", h=8), qkb[pb][:, 512:1024].rearrange("p (h d) -> p h d", h=8),
             bc(kd8[:, t, :].unsqueeze(2), [128, 8, 64]), ALU.mult, [K("qkb"), "kd8"], [H("kdec")])
        (pi,) = yield from acquire(1)
        ptv = pp[pi][:].bitcast(BF16).rearrange("p (a t) -> p a t", a=8)
        for a in range(8):
            P.tr(ptv[:, a, :], qkb[pb][:, a * 128:(a + 1) * 128], identb[:], [K("qkb"), "identb"], [f"pp{pi}"])
        yield
        P.copy(ACT, qkT[hb][:], ptv, [f"pp{pi}"], [H("qkT")])
        release(pi)
        yield
        for hg in range(2):
            iG, iQ, iR = yield from acquire(3)
            P.tt(POOL, dg[pb][:], bc(identf[:].unsqueeze(1), [128, 4, 128]),
                 bc(gc8[:, t, hg::2].unsqueeze(2), [128, 4, 128]), ALU.mult, ["identf", "gc8"], [K("dg")])
            for h4 in range(4):
                h = 2 * h4 + hg
                kT_h = qkT[hb][hp(h):hp(h) + 64, 4 + h // 2, :]
                qT_h = qkT[hb][hp(h):hp(h) + 64, h // 2, :]
                P.mm(pp[iG][:, h4 * 128:(h4 + 1) * 128], kT_h, kT_h, True, True, [H("qkT")], [f"pp{iG}"])
                P.mm(pp[iQ][:, h4 * 128:(h4 + 1) * 128], kT_h, qT_h, True, True, [H("qkT")], [f"pp{iQ}"])
            P.mm(pp[iR][:], onesf[:], dg[pb][:].rearrange("p h i -> p (h i)"), True, True,
                 ["onesf", K("dg")], [f"pp{iR}"])
            yield
            for h4 in range(4):
                h = 2 * h4 + hg
                P.ts(DVE, dsb[pb][:, h4, :], pp[iR][:, h4 * 128:(h4 + 1) * 128], gc8[:, t, h:h + 1], 0.0,
                     ALU.subtract, ALU.min, [f"pp{iR}", "gc8"], [K("dsb")])
            release(iR)
            yield
            hs = slice(hg * 4, (hg + 1) * 4)
            P.act(dsb[pb][:], dsb[pb][:], AF.Exp, [K("dsb")], [K("dsb")])
            yield
            P.tt(POOL, DTs[pb][:], dsb[pb][:], bc(mneg[:].unsqueeze(1), [128, 4, 128]), ALU.mult,
                 [K("dsb"), "mneg"], [K("DTs")])
            P.tt(POOL, DTc[pb][:], dsb[pb][:], bc(mcaus[:].unsqueeze(1), [128, 4, 128]), ALU.mult,
                 [K("dsb"), "mcaus"], [K("DTc")])
            yield
            for h4 in range(4):
                h = 2 * h4 + hg
                P.stt(MTb[pb][0][:, hg * 4 + h4, :], pp[iG][:, h4 * 128:(h4 + 1) * 128], beta8[:, t, h:h + 1],
                      DTs[pb][:, h4, :], ALU.mult, ALU.mult, [f"pp{iG}", "beta8", K("DTs")],
                      [K("MT") + f"0_{hg}"])
            P.tt(DVE, inT[hb][:, hs, :], pp[iQ][:].rearrange("p (h i) -> p h i", h=4), DTc[pb][:], ALU.mult,
                 [f"pp{iQ}", K("DTc")], [H("inT") + f"_{hg}"])
            release(iG, iQ)
            yield
        (pi,) = yield from acquire(1)
        ptv = pp[pi][:].bitcast(BF16).rearrange("p (a t) -> p a t", a=8)
        for h in range(8):
            P.tr(ptv[:, h, :], MTb[pb][0][:, h, :], identb[:], [K("MT") + f"0_{h // 4}", "identb"], [f"pp{pi}"])
        P.tt(POOL, PTf[pb][:], MTb[pb][0][:], bc(identf[:].unsqueeze(1), [128, 8, 128]), ALU.add,
             [K("MT") + "0_0", K("MT") + "0_1", "identf"], [K("PTf") + "_0", K("PTf") + "_1"])
        P.tt(POOL, PTw[pb][:], MTb[pb][0][:], bc(identf[:].unsqueeze(1), [128, 8, 128]), ALU.add,
             [K("MT") + "0_0", K("MT") + "0_1", "identf"], [K("PTw") + "_0", K("PTw") + "_1"])
        yield
        P.copy(ACT, Mb[pb][0][:, 0:4, :], ptv[:, 0:4, :], [f"pp{pi}"], [K("M") + "0_0"])
        P.copy(DVE, Mb[pb][0][:, 4:8, :], ptv[:, 4:8, :], [f"pp{pi}"], [K("M") + "0_1"])
        release(pi)
        yield
        for s in range(1, 7):
            cur, prv = s % 2, (s - 1) % 2
            if s <= 5:
                bk = yield from acquire(4)
                iMs, iTs = [bk[0], bk[2]], [bk[1], bk[3]]
            else:
                iMs = yield from acquire(2)
                iTs = [None, None]
            for hg in range(2):
                kMp, kMTp = K("M") + f"{prv}_{hg}", K("MT") + f"{prv}_{hg}"
                for h4 in range(4):
                    h = hg * 4 + h4
                    P.mm(pp[iMs[hg]][:, h4 * 128:(h4 + 1) * 128], MTb[pb][prv][:, h, :], Mb[pb][prv][:, h, :], True, True,
                         [kMp, kMTp], [f"pp{iMs[hg]}"])
                if s <= 5:
                    for h4 in range(4):
                        h = hg * 4 + h4
                        P.mm(pp[iTs[hg]][:, h4 * 128:(h4 + 1) * 128], Mb[pb][prv][:, h, :], MTb[pb][prv][:, h, :], True, True,
                             [kMp, kMTp], [f"pp{iTs[hg]}"])
            yield
            for hg in range(2):
                hs = slice(hg * 4, (hg + 1) * 4)
                kM, kMT = K("M") + f"{cur}_{hg}", K("MT") + f"{cur}_{hg}"
                P.copy(ACT, Mb[pb][cur][:, hs, :], pp[iMs[hg]][:].rearrange("p (h i) -> p h i", h=4), [f"pp{iMs[hg]}"], [kM])
                if s <= 5:
                    P.copy(DVE, MTb[pb][cur][:, hs, :], pp[iTs[hg]][:].rearrange("p (h i) -> p h i", h=4), [f"pp{iTs[hg]}"], [kMT])
                    release(iTs[hg])
                release(iMs[hg])
            yield
            iAs = yield from acquire(2)
            for hg in range(2):
                kM = K("M") + f"{cur}_{hg}"
                for h4 in range(4):
                    h = hg * 4 + h4
                    P.mm(pp[iAs[hg]][:, h4 * 128:(h4 + 1) * 128], Mb[pb][cur][:, h, :], PTw[pb][:, h, :], True, True,
                         [kM, K("PTw") + f"_{hg}"], [f"pp{iAs[hg]}"])
            yield
            for hg in range(2):
                hs = slice(hg * 4, (hg + 1) * 4)
                P.tt(DVE, PTf[pb][:, hs, :], PTf[pb][:, hs, :], pp[iAs[hg]][:].rearrange("p (h i) -> p h i", h=4), ALU.add,
                     [K("PTf") + f"_{hg}", f"pp{iAs[hg]}"], [K("PTf") + f"_{hg}"])
                release(iAs[hg])
            yield
            for hg in range(2):
                hs = slice(hg * 4, (hg + 1) * 4)
                if s < 6:
                    P.copy(ACT, PTw[pb][:, hs, :], PTf[pb][:, hs, :], [K("PTf") + f"_{hg}"], [K("PTw") + f"_{hg}"])
                else:
                    P.copy(ACT, PTb[hb][:, hs, :], PTf[pb][:, hs, :], [K("PTf") + f"_{hg}"], [H("PTb") + f"_{hg}"])
            yield

    def hi(h):
        return (h % 2) * 4 + h // 2

    def recur(t):
        hb = t % 3
        H = lambda s: f"{s}{hb}"
        rK = [rA, rB]
        rCv = rC[:].rearrange("p (h d) -> p h d", h=8)
        rDv = rD[:, 0:256].rearrange("p (a d) -> p a d", a=4)
        tmp4 = tmp[:].rearrange("p (a q) d -> p a q d", q=2)
        for h in range(8):
            par, a = h % 2, h // 2
            kT_h = qkT[hb][hp(h):hp(h) + 64, 4 + a, :]
            qT_h = qkT[hb][hp(h):hp(h) + 64, a, :]
            S_h = Sb[hp(h):hp(h) + 64, a, :]
            P.mm(rK[par][:, a * 64:(a + 1) * 64], kT_h, S_h, True, True, [H("qkT"), "Sb"], [f"rK{par}"])
            P.mm(rK[par][:, 256 + a * 64:256 + (a + 1) * 64], qT_h, S_h, True, True, [H("qkT"), "Sb"], [f"rK{par}"])
        yield
        for par in range(2):
            P.tt(DVE, tmp4[:, :, par, :], rK[par][:, 0:256].rearrange("p (a d) -> p a d", a=4),
                 bc(negeg8[:, t, par::2].unsqueeze(2), [128, 4, 64]), ALU.mult, [f"rK{par}", "negeg8"], ["tmp"])
        P.tt(DVE, rp[:], tmp[:], v_f[hb][:].rearrange("p (h d) -> p h d", h=8), ALU.add, ["tmp", H("v_f")], ["rp"])
        yield
        for h in range(8):
            P.mm(rCv[:, h, :], PTb[hb][:, hi(h), :], rp[:, h, :], True, True, [H("PTb") + f"_{h % 2}", "rp"], ["rC"])
        yield
        P.tt(DVE, vn[:], rCv, bc(beta8[:, t, :].unsqueeze(2), [128, 8, 64]), ALU.mult, ["rC", "beta8"], ["vn"])
        yield
        for h in range(8):
            P.mm(rDv[hp(h):hp(h) + 64, h // 2, :], kdec[hb][:, h * 64:(h + 1) * 64], vn[:, h, :], True, True,
                 [H("kdec"), "vn"], ["rD"])
        for h in range(8):
            P.mm(rCv[:, h, :], inT[hb][:, hi(h), :], vn[:, h, :], True, True, [H("inT") + f"_{h % 2}", "vn"], ["rC"])
        yield
        P.tt(DVE, Sf[:], Sf[:], bc(eglS[:, t, :].unsqueeze(2), [128, 4, 64]), ALU.mult, ["Sf", "eglS"], ["Sf"])
        P.tt(DVE, Sb[:], Sf[:], rDv, ALU.add, ["Sf", "rD"], ["Sb"])
        P.tt(DVE, Sf[:], Sf[:], rDv, ALU.add, ["Sf", "rD"], ["Sf"])
        for par in range(2):
            P.tt(DVE, tmp4[:, :, par, :], rK[par][:, 256:512].rearrange("p (a d) -> p a d", a=4),
                 bc(eg8[:, t, par::2].unsqueeze(2), [128, 4, 64]), ALU.mult, [f"rK{par}", "eg8"], ["tmp"])
        P.tt(DVE, o_f[hb][:].rearrange("p (h d) -> p h d", h=8), tmp[:], rCv, ALU.add, ["tmp", "rC"], [H("o_f")])
        yield

    def epi(t):
        hb = t % 3
        H = lambda s: f"{s}{hb}"
        ob = t % 2
        E = lambda s: f"{s}{ob}"
        P.dma(SP, zt[hb][:], C.zbuf[t * 128:(t + 1) * 128, :], [], [H("zt")], H("zt"))
        P.tt(POOL, o2[ob][:], o_f[hb][:], o_f[hb][:], ALU.mult, [H("o_f")], [E("o2")])
        yield
        P.act(zt[hb][:], zt[hb][:], AF.Silu, [H("zt")], [H("zt")])
        P.op(DVE, lambda e: e.tensor_reduce(ssn[ob][:], o2[ob][:].rearrange("p (h d) -> p h d", h=8), AX.X, ALU.add),
             [E("o2")], [E("ssn")])
        P.ts(DVE, ssn[ob][:], ssn[ob][:], 1.0 / 64.0, 1e-6, ALU.mult, ALU.add, [E("ssn")], [E("ssn")])
        yield
        P.act(ssn[ob][:], ssn[ob][:], AF.Ln, [E("ssn")], [E("ssn")])
        P.act(ssn[ob][:], ssn[ob][:], AF.Exp, [E("ssn")], [E("ssn")], scale=-0.5)
        yield
        P.tt(POOL, o2[ob][:].rearrange("p (h d) -> p h d", h=8), o_f[hb][:].rearrange("p (h d) -> p h d", h=8),
             bc(ssn[ob][:].unsqueeze(2), [128, 8, 64]), ALU.mult, [H("o_f"), E("ssn")], [E("o2")])
        yield
        P.tt(POOL, o2[ob][:].rearrange("p (h d) -> p h d", h=8), o2[ob][:].rearrange("p (h d) -> p h d", h=8),
             bc(wn[:].unsqueeze(1), [128, 8, 64]), ALU.mult, [E("o2"), "wn"], [E("o2")])
        yield
        P.tt(POOL, o_bf[ob][:], o2[ob][:], zt[hb][:], ALU.mult, [E("o2"), H("zt")], [E("o_bf")])
        yield
        (pi,) = yield from acquire(1)
        ptv = pp[pi][:].bitcast(BF16).rearrange("p (a t) -> p a t", a=8)
        for a in range(4):
            P.tr(ptv[:, a, :], o_bf[ob][:, a * 128:(a + 1) * 128], identb[:], [E("o_bf"), "identb"], [f"pp{pi}"])
        yield
        P.copy(ACT, oT[ob][:], ptv[:, 0:4, :], [f"pp{pi}"], [f"oT{ob}"])
        release(pi)
        dst = C.catT[0:512, t * 128:(t + 1) * 128].rearrange("(a p) t -> p a t", p=128)
        P.dma(POOL, dst, oT[ob][:], [f"oT{ob}"], [("catT_dn", t)], f"oT{ob}")
        yield

    preps = {}
    epis = []
    prep_done = set()
    next_prep = 0
    rec_t = 0
    rec_gen = None
    NTD = C.dn_tiles
    while rec_t < NTD or epis or preps:
        while next_prep < NTD and next_prep <= rec_t + 2 and len(preps) < 2:
            preps[next_prep] = prep(next_prep)
            next_prep += 1
        for tt_ in list(preps):
            try:
                C.dbg_stage = getattr(C, "dbg_stage", 0) + 1
                if C.dbg_stage > getattr(C, "dbg_maxstage", 10 ** 9):
                    raise StopIteration
                next(preps[tt_])
            except StopIteration:
                prep_done.add(tt_)
                del preps[tt_]
        if getattr(C, "dbg_norec", False) and not preps:
            break
        if rec_gen is None and rec_t < NTD and rec_t in prep_done:
            rec_gen = recur(rec_t)
        if rec_gen is not None:
            try:
                next(rec_gen)
            except StopIteration:
                rec_gen = None
                epis.append(epi(rec_t))
                rec_t += 1
        for g in list(epis):
            try:
                next(g)
            except StopIteration:
                epis.remove(g)
    if getattr(C, "dn_dump", None):
        dumps = {"d_qkT": (qkT[0], ["qkT0"]), "d_kdec": (kdec[0], ["kdec0"]), "d_vf": (v_f[0], ["v_f0"]),
                 "d_inT": (inT[0], ["inT0_0", "inT0_1"]), "d_PTb": (PTb[0], ["PTb0_0", "PTb0_1"]),
                 "d_MT0": (MTb[0][0], ["MT00_0", "MT00_1"]), "d_M0": (Mb[0][0], ["M00_0", "M00_1"]),
                 "d_gc8": (gc8, ["gc8"]), "d_beta8": (beta8, ["beta8"]), "d_eg8": (eg8, ["eg8"]), "d_Sf": (Sf, ["Sf"]),
                 "d_of": (o_f[0], ["o_f0"]), "d_gg8": (gg8, ["gg8"]), "d_kd8": (kd8, ["kd8"]), "d_glb8": (glb8, ["glb8"]),
                 "d_act": (act[0], ["act0"]), "d_qkb": (qkb[0], ["qkb0"]), "d_PTf": (PTf[0], ["PTf0_0", "PTf0_1"]),
                 "d_vn": (vn, ["vn"]), "d_rp": (rp, ["rp"])}
        for nm, (tile, keys) in dumps.items():
            if nm in C.dn_dump:
                P.dma(SP, C.dn_dump[nm], tile[:], keys, [nm], nm)
    return P.emit()


INPUT_SHAPES = {
    "x": ([S, D], F32), "w_in": ([DEPTH, D, IN_W], F32), "w_out": ([DEPTH, D, D], F32),
    "ffn_w_gate": ([2, D, DFF], F32), "ffn_w_up": ([2, D, DFF], F32), "ffn_w_down": ([2, DFF, D], F32),
    "moe_w_gate": ([2, NE, D, DFE], F32), "moe_w_up": ([2, NE, D, DFE], F32), "moe_w_down": ([2, NE, DFE, D], F32),
    "ident_bf": ([128, 128], BF16), "ident_f32": ([128, 128], F32), "ones_f32": ([128, 128], F32),
    "tri_f32": ([128, 128], F32), "mcausT": ([128, 128], F32), "mnegT": ([128, 128], F32), "maskneg": ([128, 128], F32),
    "conv_w_bc": ([DEPTH, 128, 6144], F32), "a_log_bc": ([DEPTH, 128, 8], F32), "dt_bias_bc": ([DEPTH, 128, 8], F32),
    "dn_norm_bc": ([DEPTH, 128, 64], F32), "df_lambda_bc": ([DEPTH, 128, 256], F32), "df_subln_col": ([DEPTH, 128, 1], F32),
    "ln1_g_bc": ([DEPTH, 128, D], F32), "ln1_b_bc": ([DEPTH, 128, D], F32), "ln2_g_bc": ([DEPTH, 128, D], F32),
    "ln2_b_bc": ([DEPTH, 128, D], F32), "router_bc": ([2, 128, NE, D], F32),
    "bias_tiles": ([4, 2, 128, 128], F32), "cfar": ([128, 4], F32),
}


DUMP_SHAPES = {"d_qkT": ([128, 8, 128], BF16), "d_kdec": ([128, 512], BF16), "d_vf": ([128, 512], F32),
               "d_inT": ([128, 8, 128], BF16), "d_PTb": ([128, 8, 128], BF16), "d_MT0": ([128, 8, 128], BF16),
               "d_M0": ([128, 8, 128], BF16), "d_gc8": ([128, NT, 8], F32), "d_beta8": ([128, NT, 8], F32),
               "d_eg8": ([128, NT, 8], F32), "d_Sf": ([128, 4, 64], F32), "d_of": ([128, 512], F32),
               "d_gg8": ([128, NT, 8], F32), "d_kd8": ([128, NT, 8], F32), "d_glb8": ([128, NT, 8], F32),
               "d_act": ([128, 1536], F32), "d_qkb": ([128, 1024], BF16), "d_PTf": ([128, 8, 128], F32),
               "d_vn": ([128, 8, 64], BF16), "d_rp": ([128, 8, 64], BF16)}


def build(n_layers=DEPTH, debug=(), stop_after=None, skip_inputs=(), dn_tiles=NT, only=None):
    nc = bass.Bass("TRN2", target_bir_lowering=False)
    C = Ctx()
    C.nc = nc
    C.debug = set(debug)
    C.dn_tiles = dn_tiles
    import os as _os
    C.dbg_maxstage = int(_os.environ.get('DN_MAXSTAGE', 10 ** 9))
    C.dbg_norec = bool(int(_os.environ.get('DN_NOREC', '0')))
    for name, (shape, dt) in INPUT_SHAPES.items():
        if name in skip_inputs:
            continue
        setattr(C, name, nc.dram_tensor(name, list(shape), dt, kind="ExternalInput").ap())

    def dscr(name, shape, dt):
        kind = "ExternalOutput" if name in C.debug else "Internal"
        return nc.dram_tensor(name, list(shape), dt, kind=kind).ap()

    C.out = nc.dram_tensor("out", [S, D], F32, kind="ExternalOutput").ap()
    C.xT = dscr("xT", [D, S], BF16)
    C.xres = dscr("xres", [S, D], F32)
    C.qkvpre = dscr("qkvpre", [S + 3, 1536], F32)
    C.zbuf = dscr("zbuf", [S, 512], F32)
    C.Vdf = dscr("Vdf", [S, 512], BF16)
    C.catT = dscr("catT", [D, S], BF16)
    C.QT = [dscr(f"QT{h}", [128, S], BF16) for h in range(4)]
    C.KT = [dscr(f"KT{h}", [128, S], BF16) for h in range(4)]
    C.dn_dump = {}
    for nm in C.debug:
        if nm.startswith("d_"):
            shp, dt = DUMP_SHAPES[nm]
            C.dn_dump[nm] = nc.dram_tensor(nm, list(shp), dt, kind="ExternalOutput").ap()
    gstack = ExitStack()
    C.ab_sb = gstack.enter_context(nc.sbuf_tensor("ab_sb", [128, NT, 16], F32))
    C.gate_sb = gstack.enter_context(nc.sbuf_tensor("gate_sb", [128, NT, NE], F32))
    stats = {}
    C.stats = stats
    P = Phase(nc, "z0")
    zt = P.sb("zt", [3, 1536], F32)
    P.op(DVE, lambda e: e.memset(zt[:], 0.0), [], ["zt"])
    P.dma(SP, C.qkvpre[0:3, :], zt[:], ["zt"], ["pad"], "zt")
    P.emit()
    if only is None or "x0" in only:
        stats["x0"] = phase_x0(C)
    done = False
    for layer in range(n_layers):
        for nm, fn in (("ip", phase_inproj), ("dn", phase_dn), ("at", phase_attn), ("op", phase_outproj)):
            if only is None or f"{nm}{layer}" in only:
                stats[f"{nm}{layer}"] = fn(C, layer)
            if stop_after == f"{nm}{layer}":
                done = True
                break
        if done:
            break
        if only is None or f"ff{layer}" in only:
            stats[f"ff{layer}"] = phase_ffn(C, layer, layer == n_layers - 1)
        if stop_after == f"ff{layer}":
            break
    gstack.close()
    return nc, C


def t5_bucket_np(dist):
    dist = np.asarray(dist, dtype=np.int64)
    d = np.maximum(dist, 1).astype(np.float32)
    large = 16 + (np.log(d / np.float32(16.0)) / np.float32(math.log(128 / 16)) * np.float32(16.0)).astype(np.int32)
    large = np.minimum(large, 31)
    return np.where(dist < 16, dist, large)


def host_inputs(inputs):
    f32 = np.float32
    ii = np.arange(128)
    m = {}
    m["ident_bf"] = np.eye(128, dtype=f32).astype(ml_dtypes.bfloat16)
    m["ident_f32"] = np.eye(128, dtype=f32)
    m["ones_f32"] = np.ones((128, 128), f32)
    m["tri_f32"] = (ii[:, None] <= ii[None, :]).astype(f32)
    m["mcausT"] = (ii[None, :] >= ii[:, None]).astype(f32)
    m["mnegT"] = -(ii[None, :] > ii[:, None]).astype(f32)
    m["maskneg"] = np.where(ii[None, :] >= ii[:, None], 0.0, -1e5).astype(f32)

    def bc128(a):
        a = np.asarray(a, dtype=f32)
        return np.ascontiguousarray(np.broadcast_to(a[:, None, :], (a.shape[0], 128, a.shape[1])))

    m["conv_w_bc"] = bc128(np.asarray(inputs["conv_w"]).reshape(DEPTH, 4 * 1536))
    m["a_log_bc"] = bc128(inputs["dn_a_log"])
    m["dt_bias_bc"] = bc128(inputs["dn_dt_bias"])
    m["dn_norm_bc"] = bc128(inputs["dn_norm_w"])
    m["df_lambda_bc"] = bc128(np.asarray(inputs["df_lambda"]).reshape(DEPTH, 256))
    m["df_subln_col"] = np.ascontiguousarray(np.asarray(inputs["df_subln_w"], dtype=f32).reshape(DEPTH, 128, 1))
    for k in ("ln1_g", "ln1_b", "ln2_g", "ln2_b"):
        m[k + "_bc"] = bc128(inputs[k])
    r = np.asarray(inputs["moe_router"], dtype=f32).transpose(0, 2, 1)
    m["router_bc"] = np.ascontiguousarray(np.broadcast_to(r[:, None, :, :], (2, 128, NE, D)))
    rb = np.asarray(inputs["rel_bias"], dtype=f32)
    bt = np.zeros((4, 2, 128, 128), f32)
    for rel in range(2):
        dist = rel * 128 + ii[None, :] - ii[:, None]
        bk = t5_bucket_np(np.maximum(dist, 0))
        g = rb[bk]
        g = np.where((dist >= 0)[:, :, None], g, 0.0)
        bt[:, rel] = g.transpose(2, 0, 1)
    m["bias_tiles"] = bt
    m["cfar"] = np.ascontiguousarray(np.broadcast_to(rb[31][None, :], (128, 4)))
    for k in ("w_in", "w_out", "ffn_w_gate", "ffn_w_up", "ffn_w_down", "moe_w_gate", "moe_w_up", "moe_w_down"):
        m[k] = np.ascontiguousarray(np.asarray(inputs[k], dtype=f32))
    return m


def kernel(**inputs):
    nc, C = build()
    shared = host_inputs(inputs)
    in_maps = []
    for b in range(8):
        m = dict(shared)
        m["x"] = np.ascontiguousarray(np.asarray(inputs["x"][b], dtype=np.float32))
        in_maps.append(m)
    res = run_bass_kernel_spmd(nc, in_maps, core_ids=list(range(8)))
    return np.stack([np.asarray(r["out"], dtype=np.float32) for r in res.results], axis=0)
```

```python
import math
import numpy as np
import ml_dtypes
from contextlib import ExitStack
import concourse.bass as bass
import concourse.mybir as mybir
from concourse.bass_utils import run_bass_kernel_spmd

F32 = mybir.dt.float32
BF16 = mybir.dt.bfloat16
AF = mybir.ActivationFunctionType
ALU = mybir.AluOpType
AX = mybir.AxisListType

PE, ACT, DVE, POOL, SP = "pe", "act", "dve", "pool", "sp"
ENGMAP = {PE: "tensor", ACT: "scalar", DVE: "vector", POOL: "gpsimd", SP: "sync"}
SEM_EPOCH = 30000

S = 8192
D = 1024
NT = S // 128
DEPTH = 4
IN_W = 3600
DFF = 2816
DFE = 3584
NE = 8
ALPHA = (2 * DEPTH) ** 0.25
LN_EPS = 1e-5


class Op:
    __slots__ = ("eng", "fn", "deps", "signal", "sem", "val", "is_dma", "grp", "ndep", "seq")

    def __init__(self, eng, fn, is_dma, grp):
        self.eng = eng
        self.fn = fn
        self.deps = []
        self.signal = False
        self.sem = None
        self.val = 0
        self.is_dma = is_dma
        self.grp = grp
        self.ndep = 0


class Phase:
    def __init__(self, nc, name):
        self.nc = nc
        self.name = name
        self.ops = []
        self.last_w = {}
        self.readers = {}
        self.stack = ExitStack()
        self.excl = set()
        self.alias = {}
        self.eng_seq = {}

    def sb(self, name, shape, dt):
        return self.stack.enter_context(self.nc.sbuf_tensor(f"{self.name}_{name}", list(shape), dt))

    def ps(self, name, shape, dt=F32):
        return self.stack.enter_context(self.nc.psum_tensor(f"{self.name}_{name}", list(shape), dt))

    def op(self, eng, fn, r=(), w=(), dma=False, grp=None):
        if self.alias:
            r = [self.alias.get(k, k) for k in r]
            w = [self.alias.get(k, k) for k in w]
        o = Op(eng, fn, dma, grp)
        o.seq = self.eng_seq.get(eng, 0)
        self.eng_seq[eng] = o.seq + 1
        deps = []
        seen = set()
        raw = set()
        for k in r:
            lw = self.last_w.get(k)
            if lw is not None:
                raw.add(id(lw))
                if id(lw) not in seen:
                    seen.add(id(lw))
                    deps.append(lw)
            if k in self.excl:
                for rd in self.readers.get(k, ()):
                    if rd.eng != eng and id(rd) not in seen:
                        seen.add(id(rd))
                        raw.add(id(rd))
                        deps.append(rd)
        for k in w:
            lw = self.last_w.get(k)
            if lw is not None and id(lw) not in seen:
                seen.add(id(lw))
                deps.append(lw)
            for rd in self.readers.get(k, ()):
                if id(rd) not in seen:
                    seen.add(id(rd))
                    deps.append(rd)
        for d in deps:
            if d.eng == eng and not d.is_dma and not dma:
                if eng == PE:
                    continue
                if id(d) not in raw and o.seq - d.seq > 1:
                    continue
            o.deps.append(d)
            d.ndep += 1
        for k in r:
            self.readers.setdefault(k, []).append(o)
        for k in w:
            self.last_w[k] = o
            self.readers[k] = []
        self.ops.append(o)
        return o

    def mm(self, out, lhsT, rhs, start, stop, r, w, **kw):
        return self.op(PE, lambda e: e.matmul(out, lhsT, rhs, start=start, stop=stop, **kw), r, w)

    def tr(self, out, in_, ident, r, w):
        return self.op(PE, lambda e: e.transpose(out, in_, ident), r, w)

    def act(self, out, in_, func, r, w, **kw):
        return self.op(ACT, lambda e: e.activation(out, in_, func, **kw), r, w)

    def copy(self, eng, out, in_, r, w):
        if eng == ACT:
            return self.op(ACT, lambda e: e.copy(out, in_), r, w)
        return self.op(eng, lambda e: e.tensor_copy(out, in_), r, w)

    def tt(self, eng, out, in0, in1, op, r, w):
        return self.op(eng, lambda e: e.tensor_tensor(out, in0, in1, op), r, w)

    def ts(self, eng, out, in0, s1, s2, op0, op1, r, w):
        return self.op(eng, lambda e: e.tensor_scalar(out, in0, s1, s2, op0, op1), r, w)

    def stt(self, out, in0, scalar, in1, op0, op1, r, w):
        return self.op(DVE, lambda e: e.scalar_tensor_tensor(out, in0, scalar, in1, op0, op1), r, w)

    def dma(self, q, out, in_, r, w, grp, **kw):
        return self.op(q, lambda e: e.dma_start(out, in_, **kw), r, w, dma=True, grp=grp)

    def emit(self):
        nc = self.nc
        leaf = [o for o in self.ops if o.is_dma and o.ndep == 0]
        last = {}
        for o in self.ops:
            if not o.is_dma:
                last[o.eng] = o
        leaf += list(last.values())
        if leaf:
            fin = Op(SP, lambda e: e.nop(), False, None)
            fin.deps = leaf
            self.ops.append(fin)
        for o in self.ops:
            for d in o.deps:
                d.signal = True
        sem_state = {}
        nsem = [0]

        sem_handles = []

        def new_sem():
            nsem[0] += 1
            h = nc.alloc_semaphore(name=f"{self.name}_s{nsem[0]}")
            sem_handles.append(h)
            return h

        for o in self.ops:
            if not o.signal:
                continue
            key = ("dma", o.grp) if o.is_dma else o.eng
            st = sem_state.get(key)
            inc = 16 if o.is_dma else 1
            if st is None or st[1] + inc > SEM_EPOCH:
                st = [new_sem(), 0]
                sem_state[key] = st
            st[1] += inc
            o.sem = st[0]
            o.val = st[1]
        self.n_sems = nsem[0]
        per_eng = {}
        for o in self.ops:
            per_eng.setdefault(o.eng, []).append(o)
        with nc.Block() as block:
            for ename, lst in per_eng.items():
                def body(e, lst=lst):
                    waited = {}
                    for o in lst:
                        need = {}
                        for d in o.deps:
                            k = id(d.sem)
                            if k not in need or need[k][1] < d.val:
                                need[k] = (d.sem, d.val)
                        for k, (s, v) in need.items():
                            if waited.get(k, 0) >= v:
                                continue
                            e.wait_ge(s, v)
                            waited[k] = v
                        ins = o.fn(e)
                        if o.signal:
                            ins.then_inc(o.sem, 16 if o.is_dma else 1)
                getattr(block, ENGMAP[ename])(body)
        if sem_handles:
            nc.clear_and_free_semaphores(sem_handles)
            nc.all_engine_barrier()
        nops = len(self.ops)
        self.ops = None
        self.last_w = None
        self.readers = None
        self.stack.close()
        return nops


class Ctx:
    pass


def rr(lst, state=[0]):
    state[0] += 1
    return lst[state[0] % len(lst)]


def phase_x0(C):
    nc = C.nc
    P = Phase(nc, "x0")
    ident = P.sb("ident", [128, 128], BF16)
    P.dma(SP, ident[:], C.ident_bf, [], ["ident"], "ident")
    xin = [P.sb(f"xin{i}", [128, D], F32) for i in range(2)]
    xb = [P.sb(f"xb{i}", [128, D], BF16) for i in range(2)]
    xt = [P.sb(f"xt{i}", [128, 8, 128], BF16) for i in range(2)]
    pt = [P.ps(f"pt{i}", [128, 8, 128], BF16) for i in range(2)]
    P.excl.update(["pt0", "pt1"])
    xT3 = C.xT.rearrange("(c p) t -> p c t", p=128)
    for t in range(NT):
        b = t % 2
        P.dma(SP, xin[b][:], C.x[t * 128:(t + 1) * 128, :], [], [f"xin{b}"], f"xin{b}")
        P.copy(DVE if t % 2 else POOL, xb[b][:], xin[b][:], [f"xin{b}"], [f"xb{b}"])
        for c in range(8):
            P.tr(pt[b][:, c, :], xb[b][:, c * 128:(c + 1) * 128], ident[:], [f"xb{b}", "ident"], [f"pt{b}"])
        P.copy(ACT if t % 2 else DVE, xt[b][:], pt[b][:], [f"pt{b}"], [f"xt{b}"])
        P.dma(POOL, xT3[:, :, t * 128:(t + 1) * 128], xt[b][:], [f"xt{b}"], [("xT", t)], f"xt{b}")
    return P.emit()


def load_cast_w(P, dst3, src2, nchunk, ncols, r_keys, wkey, stg, colstep, cnt):
    for c in range(nchunk):
        for c0 in range(0, ncols, colstep):
            n = min(colstep, ncols - c0)
            i = cnt[0] % len(stg)
            cnt[0] += 1
            P.dma(SP, stg[i][:, 0:n], src2[c * 128:(c + 1) * 128, c0:c0 + n], list(r_keys), [f"stg{i}"], f"stg{i}")
            eng = (DVE, POOL, ACT)[cnt[0] % 3]
            P.copy(eng, dst3[:, c, c0:c0 + n], stg[i][:, 0:n], [f"stg{i}"], [wkey])


def phase_inproj(C, layer):
    nc = C.nc
    P = Phase(nc, f"ip{layer}")
    W = P.sb("W", [128, 8, IN_W], BF16)
    stg = [P.sb(f"stg{i}", [128, 1800], F32) for i in range(3)]
    cnt = [0]
    load_cast_w(P, W, C.w_in[layer], 8, IN_W, [], "W", stg, 1800, cnt)
    xTb = [P.sb(f"xTb{i}", [128, 8, 512], BF16) for i in range(2)]
    qk_sb = [P.sb(f"qk{i}", [128, 512], BF16) for i in range(3)]
    qkv_sb = [P.sb(f"qkv{i}", [128, 1536], F32) for i in range(2)]
    z_sb = [P.sb(f"z{i}", [128, 512], F32) for i in range(2)]
    v_sb = [P.sb(f"v{i}", [128, 512], BF16) for i in range(2)]
    ps = [P.ps(f"ps{i}", [128, 512], F32) for i in range(8)]
    P.excl.update([f"ps{i}" for i in range(8)])
    xT3 = C.xT.rearrange("(c p) t -> p c t", p=128)
    pi = 0
    ev = 0
    nqk = 0
    for tb in range(S // 512):
        b = tb % 2
        P.dma(SP, xTb[b][:], xT3[:, :, tb * 512:(tb + 1) * 512], [("xT", tb * 4 + i) for i in range(4)],
              [f"xTb{b}"], f"xTb{b}")
        for g in range(8):
            col0 = 2064 + g * 128
            p = pi % 8
            pi += 1
            for c in range(8):
                P.mm(ps[p][:, :], W[:, c, col0:col0 + 128], xTb[b][:, c, :], c == 0, c == 7,
                     ["W", f"xTb{b}"], [f"ps{p}"])
            i = nqk % 3
            nqk += 1
            P.copy(ACT if ev % 2 else DVE, qk_sb[i][:], ps[p][:, :], [f"ps{p}"], [f"qk{i}"])
            ev += 1
            dst = C.QT[g] if g < 4 else C.KT[g - 4]
            P.dma(POOL, dst[:, tb * 512:(tb + 1) * 512], qk_sb[i][:], [f"qk{i}"], [("qkT", g, tb)], f"qk{i}")
        for tt in range(4):
            t = tb * 4 + tt
            tb2 = t % 2
            groups = [(0, 512, "qkv"), (512, 512, "qkv"), (1024, 512, "qkv"), (1536, 512, "z"),
                      (2048, 16, "ab"), (3088, 512, "v")]
            for (col0, n, kind) in groups:
                p = pi % 8
                pi += 1
                for c in range(8):
                    P.mm(ps[p][:, 0:n], xTb[b][:, c, tt * 128:(tt + 1) * 128], W[:, c, col0:col0 + n],
                         c == 0, c == 7, ["W", f"xTb{b}"], [f"ps{p}"])
                eng = ACT if ev % 2 else DVE
                ev += 1
                if kind == "qkv":
                    P.copy(eng, qkv_sb[tb2][:, col0:col0 + 512], ps[p][:, 0:512], [f"ps{p}"], [f"qkv{tb2}_{col0}"])
                elif kind == "z":
                    P.copy(eng, z_sb[tb2][:], ps[p][:, 0:512], [f"ps{p}"], [f"z{tb2}"])
                elif kind == "ab":
                    P.copy(eng, C.ab_sb[:, t, :], ps[p][:, 0:16], [f"ps{p}"], [("ab", t)])
                else:
                    P.copy(eng, v_sb[tb2][:], ps[p][:, 0:512], [f"ps{p}"], [f"v{tb2}"])
            P.dma(POOL, C.qkvpre[3 + t * 128:3 + (t + 1) * 128, :], qkv_sb[tb2][:],
                  [f"qkv{tb2}_0", f"qkv{tb2}_512", f"qkv{tb2}_1024"], [("qkvpre", t)], f"qkv{tb2}")
            P.dma(POOL, C.zbuf[t * 128:(t + 1) * 128, :], z_sb[tb2][:], [f"z{tb2}"], [("zbuf", t)], f"z{tb2}")
            P.dma(POOL, C.Vdf[t * 128:(t + 1) * 128, :], v_sb[tb2][:], [f"v{tb2}"], [("Vdf", t)], f"v{tb2}")
    return P.emit()


def phase_attn(C, layer):
    nc = C.nc
    P = Phase(nc, f"at{layer}")
    lambda_init = 0.8 - 0.6 * math.exp(-0.3 * layer)
    identf = P.sb("identf", [128, 128], F32)
    onesf = P.sb("onesf", [128, 128], F32)
    maskneg = P.sb("maskneg", [128, 128], F32)
    cfar = P.sb("cfar", [128, 4], F32)
    bt = P.sb("bt", [128, 8, 128], F32)
    badd = P.sb("badd", [128, 8, 128], F32)
    dl = P.sb("dl", [128, 256], F32)
    dlp = P.sb("dlp", [128, 128], F32)
    ls = P.sb("ls", [128, 2], F32)
    le = P.sb("le", [128, 2], F32)
    neglam = P.sb("neglam", [128, 1], F32)
    wcol = P.sb("wcol", [128, 1], F32)
    P.dma(SP, identf[:], C.ident_f32, [], ["identf"], "c0")
    P.dma(SP, onesf[:], C.ones_f32, [], ["onesf"], "c1")
    P.dma(SP, maskneg[:], C.maskneg, [], ["maskneg"], "c2")
    P.dma(SP, cfar[:], C.cfar, [], ["cfar"], "c3")
    P.dma(SP, bt[:], C.bias_tiles.rearrange("h r k q -> k (h r) q"), [], ["bt"], "c4")
    P.dma(SP, dl[:], C.df_lambda_bc[layer], [], ["dl"], "c5")
    P.dma(SP, wcol[:], C.df_subln_col[layer], [], ["wcol"], "c6")
    for h in range(4):
        for rel in range(2):
            i = h * 2 + rel
            P.ts(DVE, badd[:, i, :], bt[:, i, :], cfar[:, h:h + 1], 8.0, ALU.subtract, ALU.mult,
                 ["bt", "cfar"], [("badd", i)])
            if rel == 0:
                P.tt(DVE, badd[:, i, :], badd[:, i, :], maskneg[:], ALU.add, [("badd", i), "maskneg"], [("badd", i)])
    P.tt(DVE, dlp[:, 0:64], dl[:, 0:64], dl[:, 64:128], ALU.mult, ["dl"], ["dlp0"])
    P.tt(DVE, dlp[:, 64:128], dl[:, 128:192], dl[:, 192:256], ALU.mult, ["dl"], ["dlp1"])
    P.op(DVE, lambda e: e.tensor_reduce(ls[:], dlp[:].rearrange("p (a b) -> p a b", a=2), AX.X, ALU.add),
         ["dlp0", "dlp1"], ["ls"])
    P.act(le[:], ls[:], AF.Exp, ["ls"], ["le"])
    P.tt(DVE, neglam[:], le[:, 1:2], le[:, 0:1], ALU.subtract, ["le"], ["neglam"])
    P.ts(DVE, neglam[:], neglam[:], -lambda_init, None, ALU.add, ALU.bypass, ["neglam"], ["neglam"])
    P.ts(DVE, wcol[:], wcol[:], 1.0 - lambda_init, None, ALU.mult, ALU.bypass, ["wcol"], ["wcol"])

    onesb = P.sb("onesb", [128, 128], BF16)
    P.op(DVE, lambda e: e.memset(onesb[:], 1.0), [], ["onesb"])
    KTh = [P.sb(f"KTh{i}", [128, S], BF16) for i in range(2)]
    QTz = [[P.sb(f"QTz{i}_{m}", [128, S], BF16) for m in range(2)] for i in range(2)]
    for i in range(2):
        for m in range(2):
            P.op(POOL if (i + m) % 2 else DVE, lambda e, i=i, m=m: e.memset(QTz[i][m][:], 0.0), [], [f"QTh{i}"])
    Vh = [P.sb(f"Vh{i}", [128, NT, 128], BF16) for i in range(2)]
    pT = [P.sb(f"pT{i}", [128, 512], BF16) for i in range(4)]
    PSa = [P.sb(f"PSa{i}", [128, 512], F32) for i in range(2)]
    rden = [P.sb(f"rden{i}", [128, 512], F32) for i in range(2)]
    on = [P.sb(f"on{i}", [128, 512], F32) for i in range(2)]
    o_sb = P.sb("o_sb", [128, 512], F32)
    sq = P.sb("sq", [128, 512], F32)
    rstd = P.sb("rstd", [128, 512], F32)
    of = [P.sb(f"of{i}", [128, 512], BF16) for i in range(2)]
    accO = [P.ps(f"accO{i}", [128, 512], F32) for i in range(2)]
    sc = [P.ps(f"sc{i}", [128, 512], F32) for i in range(4)]
    aux = [P.ps(f"aux{i}", [128, 512], F32) for i in range(2)]
    P.excl.update(["accO0", "accO1", "sc0", "sc1", "sc2", "sc3", "aux0", "aux1"])
    V3 = C.Vdf.rearrange("(t p) c -> p t c", p=128)

    def load_head(h):
        b = h % 2
        P.dma(SP, KTh[b][:], C.KT[h], [], [f"KTh{b}"], f"KTh{b}")
        for m in range(2):
            P.dma(SP, QTz[b][m][m * 64:(m + 1) * 64, :], C.QT[h][m * 64:(m + 1) * 64, :], [], [f"QTh{b}"], f"QTh{b}_{m}")
        for i in range(8):
            P.dma(SP, Vh[b][:, i * 8:(i + 1) * 8, :], V3[:, i * 8:(i + 1) * 8, h * 128:(h + 1) * 128], [],
                  [(f"Vh{b}", i)], f"Vh{b}_{i}")

    load_head(0)
    LA = 3
    nq = 0
    gstep = 0
    for h in range(4):
        b = h % 2
        if h + 1 < 4:
            load_head(h + 1)
        steps = []
        for Qb in range(S // 512):
            nk = 4 * Qb + 4
            for kt in range(nk):
                for m in range(2):
                    steps.append((Qb, kt, m, nk))
        ns = len(steps)

        def front(si, gs):
            Qb, kt, m, nk = steps[si]
            j = kt - 4 * Qb
            c0 = max(0, j) * 128
            sI = gs % 4
            pi = gs % 4
            adds = []
            if j >= 0:
                adds.append((j * 128, h * 2 + 0))
            if j >= -1 and j + 1 <= 3:
                adds.append(((j + 1) * 128, h * 2 + 1))
            P.mm(sc[sI][:, c0:512], KTh[b][:, kt * 128:(kt + 1) * 128],
                 QTz[b][m][:, Qb * 512 + c0:Qb * 512 + 512], True, len(adds) == 0,
                 [f"KTh{b}", f"QTh{b}"], [f"sc{sI}"])
            for ai, (cc, bi) in enumerate(adds):
                P.mm(sc[sI][:, cc:cc + 128], identf[:], badd[:, bi, :], False, ai == len(adds) - 1,
                     ["identf", ("badd", bi)], [f"sc{sI}"])
            P.act(pT[pi][:, c0:512], sc[sI][:, c0:512], AF.Exp, [f"sc{sI}"], [f"pT{pi}"], scale=0.125)

        def back(si, gs):
            nonlocal nq
            Qb, kt, m, nk = steps[si]
            j = kt - 4 * Qb
            c0 = max(0, j) * 128
            pi = gs % 4
            P.mm(accO[m][:, c0:512], Vh[b][:, kt, :], pT[pi][:, c0:512], kt == 0, kt == nk - 1,
                 [(f"Vh{b}", kt // 8), f"pT{pi}"], [f"accO{m}"])
            if m == 1:
                P.mm(aux[1][:, c0:512], onesb[:], pT[pi][:, c0:512], kt == 0, kt == nk - 1,
                     ["onesb", f"pT{pi}"], ["aux1"])
            elif kt == 0:
                P.copy(DVE, PSa[m][:], pT[pi][:], [f"pT{pi}"], [f"PSa{m}"])
            else:
                P.tt(DVE, PSa[m][:, c0:512], PSa[m][:, c0:512], pT[pi][:, c0:512], ALU.add,
                     [f"PSa{m}", f"pT{pi}"], [f"PSa{m}"])
            if not (kt == nk - 1 and m == 1):
                return
            for mm_ in range(2):
                if mm_ == 0:
                    P.mm(aux[mm_][:], onesf[:], PSa[mm_][:], True, True, ["onesf", f"PSa{mm_}"], [f"aux{mm_}"])
                P.op(DVE, lambda e, mm_=mm_: e.reciprocal(rden[mm_][:], aux[mm_][:]), [f"aux{mm_}"], [f"rden{mm_}"])
                P.tt(DVE, on[mm_][:], accO[mm_][:], rden[mm_][:], ALU.mult, [f"accO{mm_}", f"rden{mm_}"], [f"on{mm_}"])
            P.stt(o_sb[:], on[1][:], neglam[:, 0:1], on[0][:], ALU.mult, ALU.add, ["on0", "on1", "neglam"], ["o_sb"])
            P.tt(POOL, sq[:], o_sb[:], o_sb[:], ALU.mult, ["o_sb"], ["sq"])
            P.mm(aux[0][:], onesf[:], sq[:], True, True, ["onesf", "sq"], ["aux0"])
            P.ts(DVE, rstd[:], aux[0][:], 1.0 / 128.0, 1e-6, ALU.mult, ALU.add, ["aux0"], ["rstd"])
            P.act(rstd[:], rstd[:], AF.Ln, ["rstd"], ["rstd"])
            P.act(rstd[:], rstd[:], AF.Exp, ["rstd"], ["rstd"], scale=-0.5)
            P.tt(DVE, o_sb[:], o_sb[:], rstd[:], ALU.mult, ["o_sb", "rstd"], ["o_sb"])
            ob = nq % 2
            nq += 1
            P.ts(DVE, of[ob][:], o_sb[:], wcol[:, 0:1], None, ALU.mult, ALU.bypass, ["o_sb", "wcol"], [f"of{ob}"])
            P.dma(POOL, C.catT[512 + h * 128:512 + (h + 1) * 128, Qb * 512:(Qb + 1) * 512], of[ob][:],
                  [f"of{ob}"], [("catT_df", h, Qb)], f"of{ob}")

        for idx in range(ns + LA):
            if idx < ns:
                front(idx, gstep + idx)
            if idx - LA >= 0:
                back(idx - LA, gstep + idx - LA)
        gstep += ns
    return P.emit()


def ln_epilogue(P, C, t, src_halves, src_keys, res_src, gb, ident, bufs, pt, pt_key, out_dst=None, router=None):
    i = t % 2
    xr, y, xo, xb, xt = bufs["xr"][i], bufs["y"][i], bufs["xo"][i], bufs["xb"][i], bufs["xt"][i]
    kxr, ky, kxo, kxb, kxt = bufs["kxr"][i], bufs["ky"][i], bufs["kxo"][i], bufs["kxb"][i], bufs["kxt"][i]
    st, mv, rs, nmr = bufs["st"][i], bufs["mv"][i], bufs["rs"][i], bufs["nmr"][i]
    ks = f"lnsmall{i}"
    P.dma(SP, xr[:, 0:D], res_src[t * 128:(t + 1) * 128, :], [("xres", t)], [kxr], f"ln_{kxr}")
    for hf in range(2):
        P.stt(y[:, hf * 512:(hf + 1) * 512], xr[:, hf * 512:(hf + 1) * 512], ALPHA, src_halves[hf], ALU.mult, ALU.add,
              [kxr, src_keys[hf]], [ky])
        P.op(DVE, lambda e, hf=hf: e.bn_stats(st[:, hf, :], y[:, hf * 512:(hf + 1) * 512]), [ky], [ks + f"st{hf}"])
    P.op(DVE, lambda e: e.bn_aggr(mv[:], st[:].rearrange("p a b -> p (a b)")), [ks + "st0", ks + "st1"], [ks + "mv"])
    P.ts(DVE, rs[:], mv[:, 1:2], LN_EPS, None, ALU.add, ALU.bypass, [ks + "mv"], [ks + "rs"])
    P.act(rs[:], rs[:], AF.Ln, [ks + "rs"], [ks + "rs"])
    P.act(rs[:], rs[:], AF.Exp, [ks + "rs"], [ks + "rs"], scale=-0.5)
    P.ts(DVE, nmr[:], mv[:, 0:1], rs[:, 0:1], -1.0, ALU.mult, ALU.mult, [ks + "mv", ks + "rs"], [ks + "nmr"])
    P.ts(POOL, y[:, 0:D], y[:, 0:D], rs[:, 0:1], nmr[:, 0:1], ALU.mult, ALU.add, [ky, ks + "rs", ks + "nmr"],
         [ky])
    P.tt(POOL, y[:, 0:D], y[:, 0:D], gb[0][:], ALU.mult, [ky, "ln_g"], [ky])
    P.tt(DVE, xo[:, 0:D], y[:, 0:D], gb[1][:], ALU.add, [ky, "ln_b"], [kxo])
    if out_dst is not None:
        P.dma(POOL, out_dst[t * 128:(t + 1) * 128, :], xo[:, 0:D], [kxo], [("out", t)], f"ln_{kxo}")
        return
    P.dma(POOL, C.xres[t * 128:(t + 1) * 128, :], xo[:, 0:D], [kxo], [("xres", t)], f"ln_{kxo}")
    P.copy(ACT, xb[:, 0:D], xo[:, 0:D], [kxo], [kxb])
    for c in range(8):
        P.tr(pt[:, c, :], xb[:, c * 128:(c + 1) * 128], ident[:], [kxb, "ident"], [pt_key])
    P.copy(ACT, xt[:], pt, [pt_key], [kxt])
    xT3 = C.xT.rearrange("(c p) t -> p c t", p=128)
    P.dma(POOL, xT3[:, :, t * 128:(t + 1) * 128], xt[:], [kxt], [("xT", t)], f"ln_{kxt}")
    if router is not None:
        rbc, lg, junk = router
        for e in range(NE):
            P.op(DVE, lambda en, e=e: en.scalar_tensor_tensor(junk[:], xo[:, 0:D], 1.0, rbc[:, e, :], ALU.mult, ALU.mult,
                                                               accum_out=lg[i][:, e:e + 1]),
                 [kxo, "rbc"], [f"lg{i}_{e}", "junk"])
        lgk = [f"lg{i}_{e}" for e in range(NE)]
        sm = bufs["sm"][i]
        kk = f"sm{i}"
        P.op(DVE, lambda en: en.tensor_reduce(sm[:, 0:1], lg[i][:], AX.X, ALU.max), lgk, [kk + "m1"])
        P.ts(DVE, sm[:, 8:16], lg[i][:], sm[:, 0:1], None, ALU.is_equal, ALU.bypass, lgk + [kk + "m1"], [kk + "k1"])
        P.stt(sm[:, 24:32], sm[:, 8:16], -1e30, lg[i][:], ALU.mult, ALU.add, [kk + "k1"] + lgk, [kk + "l2"])
        P.op(DVE, lambda en: en.tensor_reduce(sm[:, 1:2], sm[:, 24:32], AX.X, ALU.max), [kk + "l2"], [kk + "m2"])
        P.ts(DVE, sm[:, 16:24], sm[:, 24:32], sm[:, 1:2], None, ALU.is_equal, ALU.bypass, [kk + "l2", kk + "m2"], [kk + "k2"])
        P.tt(DVE, sm[:, 2:3], sm[:, 1:2], sm[:, 0:1], ALU.subtract, [kk + "m1", kk + "m2"], [kk + "d"])
        P.act(sm[:, 2:3], sm[:, 2:3], AF.Exp, [kk + "d"], [kk + "d"])
        P.ts(DVE, sm[:, 3:4], sm[:, 2:3], 1.0, None, ALU.add, ALU.bypass, [kk + "d"], [kk + "g1"])
        P.op(DVE, lambda en: en.reciprocal(sm[:, 3:4], sm[:, 3:4]), [kk + "g1"], [kk + "g1"])
        P.tt(DVE, sm[:, 4:5], sm[:, 2:3], sm[:, 3:4], ALU.mult, [kk + "d", kk + "g1"], [kk + "g2"])
        P.ts(DVE, sm[:, 8:16], sm[:, 8:16], sm[:, 3:4], None, ALU.mult, ALU.bypass, [kk + "k1", kk + "g1"], [kk + "k1"])
        P.stt(C.gate_sb[:, t, :], sm[:, 16:24], sm[:, 4:5], sm[:, 8:16], ALU.mult, ALU.add,
              [kk + "k2", kk + "g2", kk + "k1"], [("gate", t)])


def ln_bufs(P):
    b = {}
    for nm, shape, dt in [("xr", [128, D], F32), ("y", [128, D], F32), ("xo", [128, D], F32), ("xb", [128, D], BF16),
                          ("xt", [128, 8, 128], BF16), ("st", [128, 2, 6], F32), ("mv", [128, 2], F32),
                          ("rs", [128, 1], F32), ("nmr", [128, 1], F32), ("sm", [128, 40], F32)]:
        b[nm] = [P.sb(f"ln_{nm}{i}", shape, dt) for i in range(2)]
        b["k" + nm] = [f"ln_{nm}{i}" for i in range(2)]
    return b


def phase_outproj(C, layer):
    nc = C.nc
    P = Phase(nc, f"op{layer}")
    moe = (layer % 2 == 1)
    ident = P.sb("ident", [128, 128], BF16)
    P.dma(SP, ident[:], C.ident_bf, [], ["ident"], "c0")
    gb = [P.sb("ln_g", [128, D], F32), P.sb("ln_b", [128, D], F32)]
    P.dma(SP, gb[0][:], C.ln1_g_bc[layer], [], ["ln_g"], "c1")
    P.dma(SP, gb[1][:], C.ln1_b_bc[layer], [], ["ln_b"], "c2")
    router = None
    if moe:
        rbc = P.sb("rbc", [128, NE, D], F32)
        P.dma(SP, rbc[:], C.router_bc[layer // 2], [], ["rbc"], "c3")
        lg = [P.sb(f"lg{i}", [128, NE], F32) for i in range(2)]
        junk = P.sb("junk", [128, D], F32)
        router = (rbc, lg, junk)
    Wo = P.sb("Wo", [128, 8, D], BF16)
    stg = [P.sb(f"stg{i}", [128, 1024], F32) for i in range(3)]
    load_cast_w(P, Wo, C.w_out[layer], 8, D, [], "Wo", stg, 1024, [0])
    bufs = ln_bufs(P)
    ct = [P.sb(f"ct{i}", [128, 8, 512], BF16) for i in range(2)]
    ps = [P.ps(f"ps{i}", [128, 512], F32) for i in range(4)]
    pt = [P.ps(f"pt{i}", [128, 8, 128], BF16) for i in range(2)]
    P.excl.update(["ps0", "ps1", "ps2", "ps3", "pt0", "pt1"])
    catT3 = C.catT.rearrange("(c p) t -> p c t", p=128)
    res_src = C.x if layer == 0 else C.xres
    for tb in range(S // 512):
        b = tb % 2
        P.dma(SP, ct[b][:], catT3[:, :, tb * 512:(tb + 1) * 512], [], [f"ct{b}"], f"ct{b}")
        for tt in range(4):
            t = tb * 4 + tt
            pp = (t % 2) * 2
            for hf in range(2):
                for c in range(8):
                    P.mm(ps[pp + hf][:], ct[b][:, c, tt * 128:(tt + 1) * 128], Wo[:, c, hf * 512:(hf + 1) * 512],
                         c == 0, c == 7, [f"ct{b}", "Wo"], [f"ps{pp + hf}"])
            ln_epilogue(P, C, t, [ps[pp][:], ps[pp + 1][:]], [f"ps{pp}", f"ps{pp + 1}"], res_src, gb, ident, bufs,
                        pt[t % 2][:], f"pt{t % 2}", router=router)
    return P.emit()


def phase_ffn(C, layer, last):
    nc = C.nc
    P = Phase(nc, f"ff{layer}")
    moe = (layer % 2 == 1)
    li = layer // 2
    if moe:
        E, F = NE, DFE
        wg_of = lambda e: C.moe_w_gate[li, e]
        wu_of = lambda e: C.moe_w_up[li, e]
        wd_of = lambda e: C.moe_w_down[li, e]
    else:
        E, F = 1, DFF
        wg_of = lambda e: C.ffn_w_gate[li]
        wu_of = lambda e: C.ffn_w_up[li]
        wd_of = lambda e: C.ffn_w_down[li]
    nfc = F // 128
    groups = [(f0, min(4, nfc - f0)) for f0 in range(0, nfc, 4)]
    SBT = 16
    ident = P.sb("ident", [128, 128], BF16)
    P.dma(SP, ident[:], C.ident_bf, [], ["ident"], "c0")
    gb = [P.sb("ln_g", [128, D], F32), P.sb("ln_b", [128, D], F32)]
    P.dma(SP, gb[0][:], C.ln2_g_bc[layer], [], ["ln_g"], "c1")
    P.dma(SP, gb[1][:], C.ln2_b_bc[layer], [], ["ln_b"], "c2")
    acc = P.sb("acc", [128, SBT, D], F32)
    xTs = P.sb("xTs", [128, 8, SBT * 128], BF16)
    Wg = [P.sb(f"Wg{i}", [128, 8, 512], BF16) for i in range(2)]
    Wu = [P.sb(f"Wu{i}", [128, 8, 512], BF16) for i in range(2)]
    Wd = [P.sb(f"Wd{i}", [128, 4, D], BF16) for i in range(2)]
    stg = [P.sb(f"stg{i}", [128, 2048], F32) for i in range(3)]
    hT = [P.sb(f"hT{i}", [128, 4, 512], BF16) for i in range(2)]
    sg = [P.sb(f"sg{i}", [128, 512], F32) for i in range(2)]
    bufs = {}
    for nm, shape, dt in [("xb", [128, D], BF16), ("xt", [128, 8, 128], BF16), ("st", [128, 2, 6], F32),
                          ("mv", [128, 2], F32), ("rs", [128, 1], F32), ("nmr", [128, 1], F32), ("sm", [128, 40], F32)]:
        bufs[nm] = [P.sb(f"ln_{nm}{i}", shape, dt) for i in range(2)]
        bufs["k" + nm] = [f"ln_{nm}{i}" for i in range(2)]
    bufs["xr"] = [stg[0], stg[0]]
    bufs["kxr"] = ["stg0", "stg0"]
    bufs["y"] = [stg[1], stg[1]]
    bufs["ky"] = ["stg1", "stg1"]
    bufs["xo"] = [stg[2], stg[2]]
    bufs["kxo"] = ["stg2", "stg2"]
    pg = [P.ps(f"pg{i}", [128, 512], F32) for i in range(2)]
    pu = [P.ps(f"pu{i}", [128, 512], F32) for i in range(2)]
    py = [P.ps(f"py{i}", [128, 512], F32) for i in range(4)]
    P.excl.update(["pg0", "pg1", "pu0", "pu1", "py0", "py1", "py2", "py3"])
    xT3 = C.xT.rearrange("(c p) t -> p c t", p=128)
    cnt = [0]
    ngu = 0
    nh = 0
    ny = 0
    wi = 0
    import os as _os
    dbg_sb = int(_os.environ.get("FF_SB", NT // SBT))
    dbg_ng = int(_os.environ.get("FF_NG", 10 ** 6))
    dbg_noln = bool(int(_os.environ.get("FF_NOLN", "0")))
    for sbk in range(min(NT // SBT, dbg_sb)):
        t0 = sbk * SBT
        for q4 in range(4):
            P.dma(SP, xTs[:, :, q4 * 512:(q4 + 1) * 512], xT3[:, :, t0 * 128 + q4 * 512:t0 * 128 + (q4 + 1) * 512],
                  [("xT", t0 + q4 * 4 + i) for i in range(4)], [("xTs", q4)], f"xTs{q4}")
        first = [True] * SBT
        for e in range(E):
            for (f0, nf) in groups[:dbg_ng]:
                wb = wi % 2
                wi += 1
                for c in range(8):
                    for (dst, src, key) in ((Wg[wb], wg_of(e), f"Wg{wb}"), (Wu[wb], wu_of(e), f"Wu{wb}")):
                        i = cnt[0] % 3
                        cnt[0] += 1
                        P.dma(SP, stg[i][:, 0:nf * 128], src[c * 128:(c + 1) * 128, f0 * 128:(f0 + nf) * 128], [],
                              [f"stg{i}"], f"stg{i}")
                        P.copy(POOL, dst[:, c, 0:nf * 128], stg[i][:, 0:nf * 128], [f"stg{i}"], [key])
                for fc in range(0, nf, 2):
                    n2 = min(2, nf - fc)
                    i = cnt[0] % 3
                    cnt[0] += 1
                    src = wd_of(e)[(f0 + fc) * 128:(f0 + fc + n2) * 128, :].rearrange("(a p) d -> p a d", p=128)
                    P.dma(SP, stg[i][:, 0:n2 * D].rearrange("p (a d) -> p a d", a=n2), src, [], [f"stg{i}"], f"stg{i}")
                    P.copy(POOL, Wd[wb][:, fc:fc + n2, :], stg[i][:, 0:n2 * D].rearrange("p (a d) -> p a d", a=n2),
                           [f"stg{i}"], [f"Wd{wb}"])
                for q4 in range(SBT // 4):
                    hb = nh % 2
                    nh += 1
                    for fc in range(nf):
                        gi = ngu % 2
                        ngu += 1
                        for c in range(8):
                            P.mm(pg[gi][:], Wg[wb][:, c, fc * 128:(fc + 1) * 128], xTs[:, c, q4 * 512:(q4 + 1) * 512],
                                 c == 0, c == 7, [f"Wg{wb}", ("xTs", q4)], [f"pg{gi}"])
                        for c in range(8):
                            P.mm(pu[gi][:], Wu[wb][:, c, fc * 128:(fc + 1) * 128], xTs[:, c, q4 * 512:(q4 + 1) * 512],
                                 c == 0, c == 7, [f"Wu{wb}", ("xTs", q4)], [f"pu{gi}"])
                        P.act(sg[gi][:], pg[gi][:], AF.Silu, [f"pg{gi}"], [f"sg{gi}"])
                        P.tt(DVE, hT[hb][:, fc, :], sg[gi][:], pu[gi][:], ALU.mult, [f"sg{gi}", f"pu{gi}"], [(f"hT{hb}", fc)])
                    for tt in range(4):
                        tl = q4 * 4 + tt
                        yb = (ny % 2) * 2
                        ny += 1
                        for hf in range(2):
                            for fc in range(nf):
                                P.mm(py[yb + hf][:], hT[hb][:, fc, tt * 128:(tt + 1) * 128], Wd[wb][:, fc, hf * 512:(hf + 1) * 512],
                                     fc == 0, fc == nf - 1, [(f"hT{hb}", fc), f"Wd{wb}"], [f"py{yb + hf}"])
                            dst = acc[:, tl, hf * 512:(hf + 1) * 512]
                            akey = ("acc", tl, hf)
                            if moe:
                                gsc = C.gate_sb[:, t0 + tl, e:e + 1]
                                if first[tl]:
                                    P.ts(DVE, dst, py[yb + hf][:], gsc, None, ALU.mult, ALU.bypass, [f"py{yb + hf}", ("gate", t0 + tl)], [akey])
                                else:
                                    P.stt(dst, py[yb + hf][:], gsc, dst, ALU.mult, ALU.add, [f"py{yb + hf}", akey, ("gate", t0 + tl)], [akey])
                            else:
                                if first[tl]:
                                    P.copy(DVE, dst, py[yb + hf][:], [f"py{yb + hf}"], [akey])
                                else:
                                    P.tt(DVE, dst, py[yb + hf][:], dst, ALU.add, [f"py{yb + hf}", akey], [akey])
                        first[tl] = False
        for tl in range(0 if dbg_noln else SBT):
            t = t0 + tl
            ptv = py[tl % 4][:].bitcast(BF16).rearrange("p (c t) -> p c t", c=8)
            ln_epilogue(P, C, t, [acc[:, tl, 0:512], acc[:, tl, 512:1024]], [("acc", tl, 0), ("acc", tl, 1)], C.xres, gb, ident,
                        bufs, ptv, f"py{tl % 4}", out_dst=(C.out if last else None))
    return P.emit()


def bc(ap, shape):
    return ap.to_broadcast(list(shape))


def phase_dn(C, layer):
    nc = C.nc
    P = Phase(nc, f"dn{layer}")
    identb = P.sb("identb", [128, 128], BF16)
    identf = P.sb("identf", [128, 128], F32)
    onesf = P.sb("onesf", [128, 128], F32)
    trif = P.sb("trif", [128, 128], F32)
    mcaus = P.sb("mcaus", [128, 128], F32)
    mneg = P.sb("mneg", [128, 128], F32)
    cw = P.sb("cw", [128, 4, 1536], F32)
    alog = P.sb("alog", [128, 8], F32)
    dtb = P.sb("dtb", [128, 8], F32)
    wn = P.sb("wn", [128, 64], F32)
    for i, (dst, src, key) in enumerate([(identb, C.ident_bf, "identb"), (identf, C.ident_f32, "identf"), (onesf, C.ones_f32, "onesf"),
                                         (trif, C.tri_f32, "trif"), (mcaus, C.mcausT, "mcaus"), (mneg, C.mnegT, "mneg"),
                                         (alog, C.a_log_bc[layer], "alog"), (dtb, C.dt_bias_bc[layer], "dtb"),
                                         (wn, C.dn_norm_bc[layer], "wn")]):
        P.dma(SP, dst[:], src, [], [key], f"c{i}")
    P.dma(SP, cw[:], C.conv_w_bc[layer].rearrange("p (j c) -> p j c", j=4), [], ["cw"], "c_cw")

    def g8(name):
        return P.sb(name, [128, NT, 8], F32)
    x8, gg8, beta8, gc8, glb8, eg8 = [g8(n) for n in ("x8", "gg8", "beta8", "gc8", "glb8", "eg8")]
    kd8, negeg8, egl8 = x8, gg8, glb8
    P.alias = {"kd8": "x8", "negeg8": "gg8", "egl8": "glb8"}
    eglS = P.sb("eglS", [128, NT, 4], F32)
    negA = P.sb("negA", [128, 8], F32)
    pp = [P.ps(f"pp{i}", [128, 512], F32) for i in range(4)]
    rA, rB, rC, rD = [P.ps(f"r{n}", [128, 512], F32) for n in "ABCD"]
    P.excl.update(["pp0", "pp1", "pp2", "pp3", "rK0", "rK1", "rC", "rD"])
    P.act(negA[:], alog[:], AF.Exp, ["alog"], ["negA"])
    P.ts(DVE, negA[:], negA[:], -1.0, None, ALU.mult, ALU.bypass, ["negA"], ["negA"])
    for h in range(8):
        P.ts(DVE, x8[:, :, h], C.ab_sb[:, :, h], dtb[:, h:h + 1], None, ALU.add, ALU.bypass, ["dtb"], ["x8"])
    P.act(x8[:], x8[:], AF.Exp, ["x8"], ["x8"])
    P.act(x8[:], x8[:], AF.Ln, ["x8"], ["x8"], bias=1.0)
    for h in range(8):
        P.ts(DVE, gg8[:, :, h], x8[:, :, h], negA[:, h:h + 1], None, ALU.mult, ALU.bypass, ["x8", "negA"], ["gg8"])
    P.act(beta8[:], C.ab_sb[:, :, 8:16], AF.Exp, [], ["beta8"], scale=-1.0)
    P.ts(DVE, beta8[:], beta8[:], 1.0, None, ALU.add, ALU.bypass, ["beta8"], ["beta8"])
    P.op(DVE, lambda e: e.reciprocal(beta8[:], beta8[:]), ["beta8"], ["beta8"])
    ggf = gg8[:].rearrange("p t h -> p (t h)")
    P.mm(pp[0][:], trif[:], ggf, True, True, ["trif", "gg8"], ["pp0"])
    P.mm(pp[1][:], onesf[:], ggf, True, True, ["onesf", "gg8"], ["pp1"])
    P.copy(DVE, gc8[:].rearrange("p t h -> p (t h)"), pp[0][:], ["pp0"], ["gc8"])
    P.copy(DVE, glb8[:].rearrange("p t h -> p (t h)"), pp[1][:], ["pp1"], ["glb8"])
    P.act(eg8[:], gc8[:], AF.Exp, ["gc8"], ["eg8"])
    P.ts(DVE, negeg8[:], eg8[:], -1.0, None, ALU.mult, ALU.bypass, ["eg8"], ["negeg8"])
    P.tt(DVE, kd8[:], glb8[:], gc8[:], ALU.subtract, ["glb8", "gc8"], ["kd8"])
    P.act(kd8[:], kd8[:], AF.Exp, ["kd8"], ["kd8"])
    P.act(egl8[:], glb8[:], AF.Exp, ["glb8"], ["egl8"])
    for par in range(2):
        P.copy(DVE, eglS[par * 64:(par + 1) * 64, :, :], egl8[par * 64:(par + 1) * 64, :, par::2], ["egl8"], ["eglS"])

    cv = [P.sb(f"cv{i}", [128, 4, 1536], F32) for i in range(2)]
    act = [P.sb(f"act{i}", [128, 1536], F32) for i in range(2)]
    ss = [P.sb(f"ss{i}", [128, 16], F32) for i in range(2)]
    qkb = [P.sb(f"qkb{i}", [128, 1024], BF16) for i in range(2)]
    dg = [P.sb(f"dg{i}", [128, 4, 128], F32) for i in range(2)]
    dsb = [P.sb(f"dsb{i}", [128, 4, 128], F32) for i in range(2)]
    DTs = [P.sb(f"DTs{i}", [128, 4, 128], F32) for i in range(2)]
    DTc = [P.sb(f"DTc{i}", [128, 4, 128], F32) for i in range(2)]
    Mb = [[P.sb(f"M{i}_{k}", [128, 8, 128], BF16) for k in range(2)] for i in range(2)]
    MTb = [[P.sb(f"MT{i}_{k}", [128, 8, 128], BF16) for k in range(2)] for i in range(2)]
    PTf = [P.sb(f"PTf{i}", [128, 8, 128], F32) for i in range(2)]
    PTw = [P.sb(f"PTw{i}", [128, 8, 128], BF16) for i in range(2)]
    qkT = [P.sb(f"qkT{i}", [128, 8, 128], BF16) for i in range(3)]
    PTb = [P.sb(f"PTb{i}", [128, 8, 128], BF16) for i in range(3)]
    inT = [P.sb(f"inT{i}", [128, 8, 128], BF16) for i in range(3)]
    v_f = [P.sb(f"v_f{i}", [128, 512], F32) for i in range(3)]
    kdec = [P.sb(f"kdec{i}", [128, 512], BF16) for i in range(3)]
    zt = [P.sb(f"zt{i}", [128, 512], F32) for i in range(3)]
    o_f = [P.sb(f"o_f{i}", [128, 512], F32) for i in range(3)]
    Sf = P.sb("Sf", [128, 4, 64], F32)
    Sb = P.sb("Sb", [128, 4, 64], BF16)
    tmp = P.sb("tmp", [128, 8, 64], F32)
    rp = P.sb("rp", [128, 8, 64], BF16)
    vn = P.sb("vn", [128, 8, 64], BF16)
    o2 = [P.sb(f"o2{i}", [128, 512], F32) for i in range(2)]
    ssn = [P.sb(f"ssn{i}", [128, 8], F32) for i in range(2)]
    o_bf = [P.sb(f"o_bf{i}", [128, 512], BF16) for i in range(2)]
    oT = [P.sb(f"oT{i}", [128, 4, 128], BF16) for i in range(2)]
    P.op(DVE, lambda e: e.memset(Sf[:], 0.0), [], ["Sf"])
    P.op(DVE, lambda e: e.memset(Sb[:], 0.0), [], ["Sb"])
    pp_free = [0, 1, 2, 3]

    def acquire(n):
        while len(pp_free) < n:
            yield
        return [pp_free.pop(0) for _ in range(n)]

    def release(*idx):
        pp_free.extend(idx)

    def hp(h):
        return (h % 2) * 64

    def prep(t):
        pb = t % 2
        hb = t % 3
        K = lambda s: f"{s}{pb}"
        H = lambda s: f"{s}{hb}"
        src = bass.AP(tensor=C.qkvpre.tensor, offset=t * 128 * 1536, ap=[[1536, 128], [1536, 4], [1, 1536]])
        P.dma(SP, cv[pb][:], src, [("qkvpre", t)], [K("cv")], K("cv"))
        yield
        P.tt(POOL, cv[pb][:], cv[pb][:], cw[:], ALU.mult, [K("cv"), "cw"], [K("cv")])
        yield
        P.tt(DVE, cv[pb][:, 0:2, :], cv[pb][:, 0:2, :], cv[pb][:, 2:4, :], ALU.add, [K("cv")], [K("cv")])
        P.tt(DVE, act[pb][:], cv[pb][:, 0, :], cv[pb][:, 1, :], ALU.add, [K("cv")], [K("act")])
        yield
        P.act(act[pb][:], act[pb][:], AF.Silu, [K("act")], [K("act")])
        yield
        sqv = cv[pb][:, 2, 0:1024]
        P.tt(POOL, sqv, act[pb][:, 0:1024], act[pb][:, 0:1024], ALU.mult, [K("act")], [K("cv")])
        P.copy(POOL, v_f[hb][:], act[pb][:, 1024:1536], [K("act")], [H("v_f")])
        yield
        P.op(DVE, lambda e: e.tensor_reduce(ss[pb][:], sqv.rearrange("p (h d) -> p h d", h=16), AX.X, ALU.add),
             [K("cv")], [K("ss")])
        P.ts(DVE, ss[pb][:], ss[pb][:], 1e-6, None, ALU.add, ALU.bypass, [K("ss")], [K("ss")])
        yield
        P.act(ss[pb][:], ss[pb][:], AF.Ln, [K("ss")], [K("ss")])
        P.act(ss[pb][:], ss[pb][:], AF.Exp, [K("ss")], [K("ss")], scale=-0.5)
        yield
        P.ts(DVE, ss[pb][:, 0:8], ss[pb][:, 0:8], 0.125, None, ALU.mult, ALU.bypass, [K("ss")], [K("ss")])
        P.tt(DVE, qkb[pb][:].rearrange("p (h d) -> p h d", h=16), act[pb][:, 0:1024].rearrange("p (h d) -> p h d", h=16),
             bc(ss[pb][:].unsqueeze(2), [128, 16, 64]), ALU.mult, [K("act"), K("ss")], [K("qkb")])
        yield
        P.tt(POOL, kdec[hb][:].rearrange("p (h d) -> p h d", h=8), qkb[pb][:, 512:1024].rearrange("p (h d) -> p h d", h=8),
             bc(kd8[:, t, :].unsqueeze(2), [128, 8, 64]), ALU.mult, [K("qkb"), "kd8"], [H("kdec")])
        (pi,) = yield from acquire(1)
        ptv = pp[pi][:].bitcast(BF16).rearrange("p (a t) -> p a t", a=8)
        for a in range(8):
            P.tr(ptv[:, a, :], qkb[pb][:, a * 128:(a + 1) * 128], identb[:], [K("qkb"), "identb"], [f"pp{pi}"])
        yield
        P.copy(ACT, qkT[hb][:], ptv, [f"pp{pi}"], [H("qkT")])
        release(pi)
        yield
        for hg in range(2):
            iG, iQ, iR = yield from acquire(3)
            P.tt(POOL, dg[pb][:], bc(identf[:].unsqueeze(1), [128, 4, 128]),
                 bc(gc8[:, t, hg::2].unsqueeze(2), [128, 4, 128]), ALU.mult, ["identf", "gc8"], [K("dg")])
            for h4 in range(4):
                h = 2 * h4 + hg
                kT_h = qkT[hb][hp(h):hp(h) + 64, 4 + h // 2, :]
                qT_h = qkT[hb][hp(h):hp(h) + 64, h // 2, :]
                P.mm(pp[iG][:, h4 * 128:(h4 + 1) * 128], kT_h, kT_h, True, True, [H("qkT")], [f"pp{iG}"])
                P.mm(pp[iQ][:, h4 * 128:(h4 + 1) * 128], kT_h, qT_h, True, True, [H("qkT")], [f"pp{iQ}"])
            P.mm(pp[iR][:], onesf[:], dg[pb][:].rearrange("p h i -> p (h i)"), True, True,
                 ["onesf", K("dg")], [f"pp{iR}"])
            yield
            for h4 in range(4):
                h = 2 * h4 + hg
                P.ts(DVE, dsb[pb][:, h4, :], pp[iR][:, h4 * 128:(h4 + 1) * 128], gc8[:, t, h:h + 1], 0.0,
                     ALU.subtract, ALU.min, [f"pp{iR}", "gc8"], [K("dsb")])
            release(iR)
            yield
            hs = slice(hg * 4, (hg + 1) * 4)
            P.act(dsb[pb][:], dsb[pb][:], AF.Exp, [K("dsb")], [K("dsb")])
            yield
            P.tt(POOL, DTs[pb][:], dsb[pb][:], bc(mneg[:].unsqueeze(1), [128, 4, 128]), ALU.mult,
                 [K("dsb"), "mneg"], [K("DTs")])
            P.tt(POOL, DTc[pb][:], dsb[pb][:], bc(mcaus[:].unsqueeze(1), [128, 4, 128]), ALU.mult,
                 [K("dsb"), "mcaus"], [K("DTc")])
            yield
            for h4 in range(4):
                h = 2 * h4 + hg
                P.stt(MTb[pb][0][:, hg * 4 + h4, :], pp[iG][:, h4 * 128:(h4 + 1) * 128], beta8[:, t, h:h + 1],
                      DTs[pb][:, h4, :], ALU.mult, ALU.mult, [f"pp{iG}", "beta8", K("DTs")],
                      [K("MT") + f"0_{hg}"])
            P.tt(DVE, inT[hb][:, hs, :], pp[iQ][:].rearrange("p (h i) -> p h i", h=4), DTc[pb][:], ALU.mult,
                 [f"pp{iQ}", K("DTc")], [H("inT") + f"_{hg}"])
            release(iG, iQ)
            yield
        (pi,) = yield from acquire(1)
        ptv = pp[pi][:].bitcast(BF16).rearrange("p (a t) -> p a t", a=8)
        for h in range(8):
            P.tr(ptv[:, h, :], MTb[pb][0][:, h, :], identb[:], [K("MT") + f"0_{h // 4}", "identb"], [f"pp{pi}"])
        P.tt(POOL, PTf[pb][:], MTb[pb][0][:], bc(identf[:].unsqueeze(1), [128, 8, 128]), ALU.add,
             [K("MT") + "0_0", K("MT") + "0_1", "identf"], [K("PTf") + "_0", K("PTf") + "_1"])
        P.tt(POOL, PTw[pb][:], MTb[pb][0][:], bc(identf[:].unsqueeze(1), [128, 8, 128]), ALU.add,
             [K("MT") + "0_0", K("MT") + "0_1", "identf"], [K("PTw") + "_0", K("PTw") + "_1"])
        yield
        P.copy(ACT, Mb[pb][0][:, 0:4, :], ptv[:, 0:4, :], [f"pp{pi}"], [K("M") + "0_0"])
        P.copy(DVE, Mb[pb][0][:, 4:8, :], ptv[:, 4:8, :], [f"pp{pi}"], [K("M") + "0_1"])
        release(pi)
        yield
        for s in range(1, 7):
            cur, prv = s % 2, (s - 1) % 2
            for hg in range(2):
                hs = slice(hg * 4, (hg + 1) * 4)
                kM, kMT = K("M") + f"{cur}_{hg}", K("MT") + f"{cur}_{hg}"
                kMp, kMTp = K("M") + f"{prv}_{hg}", K("MT") + f"{prv}_{hg}"
                if s <= 5:
                    iM, iT = yield from acquire(2)
                else:
                    (iM,) = yield from acquire(1)
                for h4 in range(4):
                    h = hg * 4 + h4
                    P.mm(pp[iM][:, h4 * 128:(h4 + 1) * 128], MTb[pb][prv][:, h, :], Mb[pb][prv][:, h, :], True, True,
                         [kMp, kMTp], [f"pp{iM}"])
                if s <= 5:
                    for h4 in range(4):
                        h = hg * 4 + h4
                        P.mm(pp[iT][:, h4 * 128:(h4 + 1) * 128], Mb[pb][prv][:, h, :], MTb[pb][prv][:, h, :], True, True,
                             [kMp, kMTp], [f"pp{iT}"])
                yield
                P.copy(ACT, Mb[pb][cur][:, hs, :], pp[iM][:].rearrange("p (h i) -> p h i", h=4), [f"pp{iM}"], [kM])
                if s <= 5:
                    P.copy(DVE, MTb[pb][cur][:, hs, :], pp[iT][:].rearrange("p (h i) -> p h i", h=4), [f"pp{iT}"], [kMT])
                    release(iT)
                release(iM)
                yield
                (iA,) = yield from acquire(1)
                for h4 in range(4):
                    h = hg * 4 + h4
                    P.mm(pp[iA][:, h4 * 128:(h4 + 1) * 128], Mb[pb][cur][:, h, :], PTw[pb][:, h, :], True, True,
                         [kM, K("PTw") + f"_{hg}"], [f"pp{iA}"])
                yield
                P.tt(DVE, PTf[pb][:, hs, :], PTf[pb][:, hs, :], pp[iA][:].rearrange("p (h i) -> p h i", h=4), ALU.add,
                     [K("PTf") + f"_{hg}", f"pp{iA}"], [K("PTf") + f"_{hg}"])
                release(iA)
                yield
                if s < 6:
                    P.copy(ACT, PTw[pb][:, hs, :], PTf[pb][:, hs, :], [K("PTf") + f"_{hg}"], [K("PTw") + f"_{hg}"])
                else:
                    P.copy(ACT, PTb[hb][:, hs, :], PTf[pb][:, hs, :], [K("PTf") + f"_{hg}"], [H("PTb") + f"_{hg}"])
                yield

    def hi(h):
        return (h % 2) * 4 + h // 2

    def recur(t):
        hb = t % 3
        H = lambda s: f"{s}{hb}"
        rK = [rA, rB]
        rCv = rC[:].rearrange("p (h d) -> p h d", h=8)
        rDv = rD[:, 0:256].rearrange("p (a d) -> p a d", a=4)
        tmp4 = tmp[:].rearrange("p (a q) d -> p a q d", q=2)
        for h in range(8):
            par, a = h % 2, h // 2
            kT_h = qkT[hb][hp(h):hp(h) + 64, 4 + a, :]
            qT_h = qkT[hb][hp(h):hp(h) + 64, a, :]
            S_h = Sb[hp(h):hp(h) + 64, a, :]
            P.mm(rK[par][:, a * 64:(a + 1) * 64], kT_h, S_h, True, True, [H("qkT"), "Sb"], [f"rK{par}"])
            P.mm(rK[par][:, 256 + a * 64:256 + (a + 1) * 64], qT_h, S_h, True, True, [H("qkT"), "Sb"], [f"rK{par}"])
        yield
        for par in range(2):
            P.tt(DVE, tmp4[:, :, par, :], rK[par][:, 0:256].rearrange("p (a d) -> p a d", a=4),
                 bc(negeg8[:, t, par::2].unsqueeze(2), [128, 4, 64]), ALU.mult, [f"rK{par}", "negeg8"], ["tmp"])
        P.tt(DVE, rp[:], tmp[:], v_f[hb][:].rearrange("p (h d) -> p h d", h=8), ALU.add, ["tmp", H("v_f")], ["rp"])
        yield
        for h in range(8):
            P.mm(rCv[:, h, :], PTb[hb][:, hi(h), :], rp[:, h, :], True, True, [H("PTb") + f"_{h % 2}", "rp"], ["rC"])
        yield
        P.tt(DVE, vn[:], rCv, bc(beta8[:, t, :].unsqueeze(2), [128, 8, 64]), ALU.mult, ["rC", "beta8"], ["vn"])
        yield
        for h in range(8):
            P.mm(rDv[hp(h):hp(h) + 64, h // 2, :], kdec[hb][:, h * 64:(h + 1) * 64], vn[:, h, :], True, True,
                 [H("kdec"), "vn"], ["rD"])
        for h in range(8):
            P.mm(rCv[:, h, :], inT[hb][:, hi(h), :], vn[:, h, :], True, True, [H("inT") + f"_{h % 2}", "vn"], ["rC"])
        yield
        P.tt(DVE, Sf[:], Sf[:], bc(eglS[:, t, :].unsqueeze(2), [128, 4, 64]), ALU.mult, ["Sf", "eglS"], ["Sf"])
        P.tt(DVE, Sb[:], Sf[:], rDv, ALU.add, ["Sf", "rD"], ["Sb"])
        P.tt(DVE, Sf[:], Sf[:], rDv, ALU.add, ["Sf", "rD"], ["Sf"])
        for par in range(2):
            P.tt(DVE, tmp4[:, :, par, :], rK[par][:, 256:512].rearrange("p (a d) -> p a d", a=4),
                 bc(eg8[:, t, par::2].unsqueeze(2), [128, 4, 64]), ALU.mult, [f"rK{par}", "eg8"], ["tmp"])
        P.tt(DVE, o_f[hb][:].rearrange("p (h d) -> p h d", h=8), tmp[:], rCv, ALU.add, ["tmp", "rC"], [H("o_f")])
        yield

    def epi(t):
        hb = t % 3
        H = lambda s: f"{s}{hb}"
        ob = t % 2
        E = lambda s: f"{s}{ob}"
        P.dma(SP, zt[hb][:], C.zbuf[t * 128:(t + 1) * 128, :], [], [H("zt")], H("zt"))
        P.tt(POOL, o2[ob][:], o_f[hb][:], o_f[hb][:], ALU.mult, [H("o_f")], [E("o2")])
        yield
        P.act(zt[hb][:], zt[hb][:], AF.Silu, [H("zt")], [H("zt")])
        P.op(DVE, lambda e: e.tensor_reduce(ssn[ob][:], o2[ob][:].rearrange("p (h d) -> p h d", h=8), AX.X, ALU.add),
             [E("o2")], [E("ssn")])
        P.ts(DVE, ssn[ob][:], ssn[ob][:], 1.0 / 64.0, 1e-6, ALU.mult, ALU.add, [E("ssn")], [E("ssn")])
        yield
        P.act(ssn[ob][:], ssn[ob][:], AF.Ln, [E("ssn")], [E("ssn")])
        P.act(ssn[ob][:], ssn[ob][:], AF.Exp, [E("ssn")], [E("ssn")], scale=-0.5)
        yield
        P.tt(POOL, o2[ob][:].rearrange("p (h d) -> p h d", h=8), o_f[hb][:].rearrange("p (h d) -> p h d", h=8),
             bc(ssn[ob][:].unsqueeze(2), [128, 8, 64]), ALU.mult, [H("o_f"), E("ssn")], [E("o2")])
        yield
        P.tt(POOL, o2[ob][:].rearrange("p (h d) -> p h d", h=8), o2[ob][:].rearrange("p (h d) -> p h d", h=8),
             bc(wn[:].unsqueeze(1), [128, 8, 64]), ALU.mult, [E("o2"), "wn"], [E("o2")])
        yield
        P.tt(POOL, o_bf[ob][:], o2[ob][:], zt[hb][:], ALU.mult, [E("o2"), H("zt")], [E("o_bf")])
        yield
        (pi,) = yield from acquire(1)
        ptv = pp[pi][:].bitcast(BF16).rearrange("p (a t) -> p a t", a=8)
        for a in range(4):
            P.tr(ptv[:, a, :], o_bf[ob][:, a * 128:(a + 1) * 128], identb[:], [E("o_bf"), "identb"], [f"pp{pi}"])
        yield
        P.copy(ACT, oT[ob][:], ptv[:, 0:4, :], [f"pp{pi}"], [f"oT{ob}"])
        release(pi)
        dst = C.catT[0:512, t * 128:(t + 1) * 128].rearrange("(a p) t -> p a t", p=128)
        P.dma(POOL, dst, oT[ob][:], [f"oT{ob}"], [("catT_dn", t)], f"oT{ob}")
        yield

    preps = {}
    epis = []
    prep_done = set()
    next_prep = 0
    rec_t = 0
    rec_gen = None
    NTD = C.dn_tiles
    while rec_t < NTD or epis or preps:
        while next_prep < NTD and next_prep <= rec_t + 2 and len(preps) < 2:
            preps[next_prep] = prep(next_prep)
            next_prep += 1
        for tt_ in list(preps):
            try:
                C.dbg_stage = getattr(C, "dbg_stage", 0) + 1
                if C.dbg_stage > getattr(C, "dbg_maxstage", 10 ** 9):
                    raise StopIteration
                next(preps[tt_])
            except StopIteration:
                prep_done.add(tt_)
                del preps[tt_]
        if getattr(C, "dbg_norec", False) and not preps:
            break
        if rec_gen is None and rec_t < NTD and rec_t in prep_done:
            rec_gen = recur(rec_t)
        if rec_gen is not None:
            try:
                next(rec_gen)
            except StopIteration:
                rec_gen = None
                epis.append(epi(rec_t))
                rec_t += 1
        for g in list(epis):
            try:
                next(g)
            except StopIteration:
                epis.remove(g)
    if getattr(C, "dn_dump", None):
        dumps = {"d_qkT": (qkT[0], ["qkT0"]), "d_kdec": (kdec[0], ["kdec0"]), "d_vf": (v_f[0], ["v_f0"]),
                 "d_inT": (inT[0], ["inT0_0", "inT0_1"]), "d_PTb": (PTb[0], ["PTb0_0", "PTb0_1"]),
                 "d_MT0": (MTb[0][0], ["MT00_0", "MT00_1"]), "d_M0": (Mb[0][0], ["M00_0", "M00_1"]),
                 "d_gc8": (gc8, ["gc8"]), "d_beta8": (beta8, ["beta8"]), "d_eg8": (eg8, ["eg8"]), "d_Sf": (Sf, ["Sf"]),
                 "d_of": (o_f[0], ["o_f0"]), "d_gg8": (gg8, ["gg8"]), "d_kd8": (kd8, ["kd8"]), "d_glb8": (glb8, ["glb8"]),
                 "d_act": (act[0], ["act0"]), "d_qkb": (qkb[0], ["qkb0"]), "d_PTf": (PTf[0], ["PTf0_0", "PTf0_1"]),
                 "d_vn": (vn, ["vn"]), "d_rp": (rp, ["rp"])}
        for nm, (tile, keys) in dumps.items():
            if nm in C.dn_dump:
                P.dma(SP, C.dn_dump[nm], tile[:], keys, [nm], nm)
    return P.emit()


INPUT_SHAPES = {
    "x": ([S, D], F32), "w_in": ([DEPTH, D, IN_W], F32), "w_out": ([DEPTH, D, D], F32),
    "ffn_w_gate": ([2, D, DFF], F32), "ffn_w_up": ([2, D, DFF], F32), "ffn_w_down": ([2, DFF, D], F32),
    "moe_w_gate": ([2, NE, D, DFE], F32), "moe_w_up": ([2, NE, D, DFE], F32), "moe_w_down": ([2, NE, DFE, D], F32),
    "ident_bf": ([128, 128], BF16), "ident_f32": ([128, 128], F32), "ones_f32": ([128, 128], F32),
    "tri_f32": ([128, 128], F32), "mcausT": ([128, 128], F32), "mnegT": ([128, 128], F32), "maskneg": ([128, 128], F32),
    "conv_w_bc": ([DEPTH, 128, 6144], F32), "a_log_bc": ([DEPTH, 128, 8], F32), "dt_bias_bc": ([DEPTH, 128, 8], F32),
    "dn_norm_bc": ([DEPTH, 128, 64], F32), "df_lambda_bc": ([DEPTH, 128, 256], F32), "df_subln_col": ([DEPTH, 128, 1], F32),
    "ln1_g_bc": ([DEPTH, 128, D], F32), "ln1_b_bc": ([DEPTH, 128, D], F32), "ln2_g_bc": ([DEPTH, 128, D], F32),
    "ln2_b_bc": ([DEPTH, 128, D], F32), "router_bc": ([2, 128, NE, D], F32),
    "bias_tiles": ([4, 2, 128, 128], F32), "cfar": ([128, 4], F32),
}


DUMP_SHAPES = {"d_qkT": ([128, 8, 128], BF16), "d_kdec": ([128, 512], BF16), "d_vf": ([128, 512], F32),
               "d_inT": ([128, 8, 128], BF16), "d_PTb": ([128, 8, 128], BF16), "d_MT0": ([128, 8, 128], BF16),
               "d_M0": ([128, 8, 128], BF16), "d_gc8": ([128, NT, 8], F32), "d_beta8": ([128, NT, 8], F32),
               "d_eg8": ([128, NT, 8], F32), "d_Sf": ([128, 4, 64], F32), "d_of": ([128, 512], F32),
               "d_gg8": ([128, NT, 8], F32), "d_kd8": ([128, NT, 8], F32), "d_glb8": ([128, NT, 8], F32),
               "d_act": ([128, 1536], F32), "d_qkb": ([128, 1024], BF16), "d_PTf": ([128, 8, 128], F32),
               "d_vn": ([128, 8, 64], BF16), "d_rp": ([128, 8, 64], BF16)}


def build(n_layers=DEPTH, debug=(), stop_after=None, skip_inputs=(), dn_tiles=NT, only=None):
    nc = bass.Bass("TRN2", target_bir_lowering=False)
    C = Ctx()
    C.nc = nc
    C.debug = set(debug)
    C.dn_tiles = dn_tiles
    import os as _os
    C.dbg_maxstage = int(_os.environ.get('DN_MAXSTAGE', 10 ** 9))
    C.dbg_norec = bool(int(_os.environ.get('DN_NOREC', '0')))
    for name, (shape, dt) in INPUT_SHAPES.items():
        if name in skip_inputs:
            continue
        setattr(C, name, nc.dram_tensor(name, list(shape), dt, kind="ExternalInput").ap())

    def dscr(name, shape, dt):
        kind = "ExternalOutput" if name in C.debug else "Internal"
        return nc.dram_tensor(name, list(shape), dt, kind=kind).ap()

    C.out = nc.dram_tensor("out", [S, D], F32, kind="ExternalOutput").ap()
    C.xT = dscr("xT", [D, S], BF16)
    C.xres = dscr("xres", [S, D], F32)
    C.qkvpre = dscr("qkvpre", [S + 3, 1536], F32)
    C.zbuf = dscr("zbuf", [S, 512], F32)
    C.Vdf = dscr("Vdf", [S, 512], BF16)
    C.catT = dscr("catT", [D, S], BF16)
    C.QT = [dscr(f"QT{h}", [128, S], BF16) for h in range(4)]
    C.KT = [dscr(f"KT{h}", [128, S], BF16) for h in range(4)]
    C.dn_dump = {}
    for nm in C.debug:
        if nm.startswith("d_"):
            shp, dt = DUMP_SHAPES[nm]
            C.dn_dump[nm] = nc.dram_tensor(nm, list(shp), dt, kind="ExternalOutput").ap()
    gstack = ExitStack()
    C.ab_sb = gstack.enter_context(nc.sbuf_tensor("ab_sb", [128, NT, 16], F32))
    C.gate_sb = gstack.enter_context(nc.sbuf_tensor("gate_sb", [128, NT, NE], F32))
    stats = {}
    C.stats = stats
    P = Phase(nc, "z0")
    zt = P.sb("zt", [3, 1536], F32)
    P.op(DVE, lambda e: e.memset(zt[:], 0.0), [], ["zt"])
    P.dma(SP, C.qkvpre[0:3, :], zt[:], ["zt"], ["pad"], "zt")
    P.emit()
    if only is None or "x0" in only:
        stats["x0"] = phase_x0(C)
    done = False
    for layer in range(n_layers):
        for nm, fn in (("ip", phase_inproj), ("dn", phase_dn), ("at", phase_attn), ("op", phase_outproj)):
            if only is None or f"{nm}{layer}" in only:
                stats[f"{nm}{layer}"] = fn(C, layer)
            if stop_after == f"{nm}{layer}":
                done = True
                break
        if done:
            break
        if only is None or f"ff{layer}" in only:
            stats[f"ff{layer}"] = phase_ffn(C, layer, layer == n_layers - 1)
        if stop_after == f"ff{layer}":
            break
    gstack.close()
    return nc, C


def t5_bucket_np(dist):
    dist = np.asarray(dist, dtype=np.int64)
    d = np.maximum(dist, 1).astype(np.float32)
    large = 16 + (np.log(d / np.float32(16.0)) / np.float32(math.log(128 / 16)) * np.float32(16.0)).astype(np.int32)
    large = np.minimum(large, 31)
    return np.where(dist < 16, dist, large)


def host_inputs(inputs):
    f32 = np.float32
    ii = np.arange(128)
    m = {}
    m["ident_bf"] = np.eye(128, dtype=f32).astype(ml_dtypes.bfloat16)
    m["ident_f32"] = np.eye(128, dtype=f32)
    m["ones_f32"] = np.ones((128, 128), f32)
    m["tri_f32"] = (ii[:, None] <= ii[None, :]).astype(f32)
    m["mcausT"] = (ii[None, :] >= ii[:, None]).astype(f32)
    m["mnegT"] = -(ii[None, :] > ii[:, None]).astype(f32)
    m["maskneg"] = np.where(ii[None, :] >= ii[:, None], 0.0, -1e5).astype(f32)

    def bc128(a):
        a = np.asarray(a, dtype=f32)
        return np.ascontiguousarray(np.broadcast_to(a[:, None, :], (a.shape[0], 128, a.shape[1])))

    m["conv_w_bc"] = bc128(np.asarray(inputs["conv_w"]).reshape(DEPTH, 4 * 1536))
    m["a_log_bc"] = bc128(inputs["dn_a_log"])
    m["dt_bias_bc"] = bc128(inputs["dn_dt_bias"])
    m["dn_norm_bc"] = bc128(inputs["dn_norm_w"])
    m["df_lambda_bc"] = bc128(np.asarray(inputs["df_lambda"]).reshape(DEPTH, 256))
    m["df_subln_col"] = np.ascontiguousarray(np.asarray(inputs["df_subln_w"], dtype=f32).reshape(DEPTH, 128, 1))
    for k in ("ln1_g", "ln1_b", "ln2_g", "ln2_b"):
        m[k + "_bc"] = bc128(inputs[k])
    r = np.asarray(inputs["moe_router"], dtype=f32).transpose(0, 2, 1)
    m["router_bc"] = np.ascontiguousarray(np.broadcast_to(r[:, None, :, :], (2, 128, NE, D)))
    rb = np.asarray(inputs["rel_bias"], dtype=f32)
    bt = np.zeros((4, 2, 128, 128), f32)
    for rel in range(2):
        dist = rel * 128 + ii[None, :] - ii[:, None]
        bk = t5_bucket_np(np.maximum(dist, 0))
        g = rb[bk]
        g = np.where((dist >= 0)[:, :, None], g, 0.0)
        bt[:, rel] = g.transpose(2, 0, 1)
    m["bias_tiles"] = bt
    m["cfar"] = np.ascontiguousarray(np.broadcast_to(rb[31][None, :], (128, 4)))
    for k in ("w_in", "w_out", "ffn_w_gate", "ffn_w_up", "ffn_w_down", "moe_w_gate", "moe_w_up", "moe_w_down"):
        m[k] = np.ascontiguousarray(np.asarray(inputs[k], dtype=f32))
    return m


def kernel(**inputs):
    nc, C = build()
    shared = host_inputs(inputs)
    in_maps = []
    for b in range(8):
        m = dict(shared)
        m["x"] = np.ascontiguousarray(np.asarray(inputs["x"][b], dtype=np.float32))
        in_maps.append(m)
    res = run_bass_kernel_spmd(nc, in_maps, core_ids=list(range(8)))
    return np.stack([np.asarray(r["out"], dtype=np.float32) for r in res.results], axis=0)
```
